# Optimizing a Trainium2 kernel written in Bass

```python
import jax
import jax.numpy as jnp
from jax import lax
import numpy as np

D_MODEL = 1024
BATCH = 8
SEQ = 2048
DEPTH = 4

GRID_W = 64
CTX_LEN = 256
CHUNK = 64
N_MIXERS = 2
RET_HEADS = 4
RET_QK = D_MODEL
RET_V = 2 * D_MODEL
RET_DK = RET_QK // RET_HEADS
RET_DV = RET_V // RET_HEADS
RET_IN = 2 * RET_QK + 2 * RET_V
GLA_HEADS = 4
GLA_K = D_MODEL // 2
GLA_V = D_MODEL
GLA_DK = GLA_K // GLA_HEADS
GLA_DV = GLA_V // GLA_HEADS
GLA_GATE_RANK = 16
GLA_TAU = 16.0
GLA_IN = 2 * GLA_K + 2 * GLA_V + 2 * GLA_GATE_RANK
D_FF = ((8 * D_MODEL // 3 + 255) // 256) * 256
N_EXPERTS = 8
TOP_K = 2
D_FF_EXPERT = 7 * D_MODEL // 2
ROPE_BASE = 10000.0
EPS = 1e-6
N_RET = (DEPTH + 1) // 2
N_GLA = DEPTH // 2

kernel_name = 'hybrid_retention_gla_moe_prefix_dit'


def rmsnorm(x, g):
    xf = x.astype(jnp.float32)
    y = xf * lax.rsqrt(jnp.mean(xf * xf, axis=-1, keepdims=True) + EPS)
    return (y * g.astype(jnp.float32)).astype(x.dtype)


def flip(t):
    return jnp.flip(t, axis=1)


def to_chunks(t):
    b, l, h, d = t.shape
    return t.reshape(b, l // CHUNK, CHUNK, h, d).transpose(1, 0, 3, 2, 4)


def from_chunks(t):
    n, b, h, c, d = t.shape
    return t.transpose(1, 0, 3, 2, 4).reshape(b, n * c, h, d)


def rope_1d(t, pos):
    half = t.shape[-1] // 2
    freqs = ROPE_BASE ** (-jnp.arange(half, dtype=jnp.float32) / half)
    ang = pos[:, None] * freqs[None, :]
    cos = jnp.cos(ang)[None, :, None, :].astype(t.dtype)
    sin = jnp.sin(ang)[None, :, None, :].astype(t.dtype)
    t1, t2 = t[..., :half], t[..., half:]
    return jnp.concatenate([t1 * cos - t2 * sin, t1 * sin + t2 * cos], axis=-1)


def rope_axial(t):
    length = t.shape[1]
    rows = length // GRID_W
    row = jnp.broadcast_to(jnp.arange(rows, dtype=jnp.float32)[:, None], (rows, GRID_W)).reshape(length)
    col = jnp.broadcast_to(jnp.arange(GRID_W, dtype=jnp.float32)[None, :], (rows, GRID_W)).reshape(length)
    half = t.shape[-1] // 2
    return jnp.concatenate([rope_1d(t[..., :half], row), rope_1d(t[..., half:], col)], axis=-1)


def retention_scan(q, k, v, log_gamma, state0):
    pos = jnp.arange(CHUNK, dtype=jnp.float32)
    lg = log_gamma.astype(jnp.float32)
    diff = pos[:, None] - pos[None, :]
    dmat = jnp.where(diff >= 0, jnp.exp(lg[:, None, None] * jnp.maximum(diff, 0.0)), 0.0)
    q_decay = jnp.exp(lg[:, None] * (pos + 1.0))[:, :, None]
    k_decay = jnp.exp(lg[:, None] * (CHUNK - 1.0 - pos))[:, :, None]
    chunk_decay = jnp.exp(lg * CHUNK)[:, None, None]

    def step(s, inp):
        qc, kc, vc = inp
        scores = jnp.einsum('bhid,bhjd->bhij', qc, kc) * dmat
        o = jnp.einsum('bhij,bhjv->bhiv', scores, vc) + jnp.einsum('bhid,bhdv->bhiv', qc * q_decay, s)
        s = s * chunk_decay + jnp.einsum('bhjd,bhjv->bhdv', kc * k_decay, vc)
        return s, o

    s, o = lax.scan(step, state0, (to_chunks(q), to_chunks(k), to_chunks(v)))
    return from_chunks(o), s


def retention_state(k, v, log_gamma):
    length = k.shape[1]
    t = jnp.arange(length, dtype=jnp.float32)
    w = jnp.exp(log_gamma.astype(jnp.float32)[None, :] * (length - 1.0 - t)[:, None])
    return jnp.einsum('blhd,blhv->bhdv', k * w[None, :, :, None], v).astype(jnp.float32)


def gla_scan(q, k, v, log_a, state0):
    causal = jnp.tril(jnp.ones((CHUNK, CHUNK), dtype=bool))[:, :, None]

    def step(s, inp):
        qc, kc, vc, gc = inp
        b = jnp.cumsum(gc, axis=2)
        rel = jnp.where(causal, b[:, :, :, None, :] - b[:, :, None, :, :], -jnp.inf)
        scores = jnp.einsum('bhjd,bhid,bhjid->bhji', qc, kc, jnp.exp(rel))
        o = jnp.einsum('bhji,bhiv->bhjv', scores, vc) + jnp.einsum('bhjd,bhdv->bhjv', qc * jnp.exp(b), s)
        b_last = b[:, :, -1:, :]
        s = s * jnp.exp(b_last)[:, :, 0, :, None] + jnp.einsum('bhid,bhiv->bhdv', kc * jnp.exp(b_last - b), vc)
        return s, o

    s, o = lax.scan(step, state0, (to_chunks(q), to_chunks(k), to_chunks(v), to_chunks(log_a)))
    return from_chunks(o), s


def gla_state(k, v, log_a):
    b = jnp.cumsum(log_a, axis=1)
    return jnp.einsum('blhd,blhv->bhdv', k * jnp.exp(b[:, -1:] - b), v).astype(jnp.float32)


def retention_mixer(h_ctx, h_lat, w_in, log_decay, gn_w, gn_b, w_out, with_ctx_out):
    batch = h_lat.shape[0]
    log_gamma = -jnp.exp(log_decay.astype(jnp.float32))
    qs = RET_QK + RET_V

    def query_side(p):
        b, l, _ = p.shape
        return p[..., :RET_QK].reshape(b, l, RET_HEADS, RET_DK), p[..., RET_QK:]

    def state_side(p):
        b, l, _ = p.shape
        k = p[..., :RET_QK].reshape(b, l, RET_HEADS, RET_DK) * (RET_DK ** -0.5)
        return k, p[..., RET_QK:].reshape(b, l, RET_HEADS, RET_DV)

    def finish(o, g):
        b, l = o.shape[:2]
        mu = jnp.mean(o, axis=-1, keepdims=True)
        var = jnp.mean(jnp.square(o - mu), axis=-1, keepdims=True)
        o = ((o - mu) * lax.rsqrt(var + EPS)).reshape(b, l, RET_V)
        o = (o * gn_w.astype(jnp.float32) + gn_b.astype(jnp.float32)).astype(g.dtype)
        return (o * jax.nn.silu(g)) @ w_out

    zeros = jnp.zeros((batch, RET_HEADS, RET_DK, RET_DV), jnp.float32)
    p_lat = h_lat @ w_in
    ql, gl = query_side(p_lat[..., :qs])
    kl, vl = state_side(p_lat[..., qs:])
    ql, kl = rope_axial(ql), rope_axial(kl)
    if with_ctx_out:
        p_ctx = h_ctx @ w_in
        qc, gc = query_side(p_ctx[..., :qs])
        kc, vc = state_side(p_ctx[..., qs:])
        oc_f, s_f = retention_scan(qc, kc, vc, log_gamma[0], zeros)
        oc_b, s_b = retention_scan(flip(qc), flip(kc), flip(vc), log_gamma[1], zeros)
        out_ctx = finish(oc_f + flip(oc_b), gc)
    else:
        kc, vc = state_side(h_ctx @ w_in[:, qs:])
        s_f = retention_state(kc, vc, log_gamma[0])
        s_b = retention_state(flip(kc), flip(vc), log_gamma[1])
        out_ctx = None
    ol_f, _ = retention_scan(ql, kl, vl, log_gamma[0], s_f)
    ol_b, _ = retention_scan(flip(ql), flip(kl), flip(vl), log_gamma[1], s_b)
    out_lat = finish(ol_f + flip(ol_b), gl)
    return out_lat, out_ctx


def gla_log_gate(a, w_up, b):
    z = (a @ w_up + b).astype(jnp.float32)
    return (jax.nn.log_sigmoid(z) / GLA_TAU).reshape(a.shape[0], a.shape[1], GLA_HEADS, GLA_DK)


def gla_mixer(h_ctx, h_lat, w_in, w_gate_up, b_gate, norm_g, w_out, with_ctx_out):
    batch = h_lat.shape[0]
    qs = GLA_K + GLA_V

    def query_side(p):
        b, l, _ = p.shape
        q = p[..., :GLA_K].reshape(b, l, GLA_HEADS, GLA_DK) * (GLA_DK ** -0.5)
        return q, p[..., GLA_K:]

    def state_side(p):
        b, l, _ = p.shape
        k = p[..., :GLA_K].reshape(b, l, GLA_HEADS, GLA_DK)
        v = p[..., GLA_K:GLA_K + GLA_V].reshape(b, l, GLA_HEADS, GLA_DV)
        a_f = p[..., GLA_K + GLA_V:GLA_K + GLA_V + GLA_GATE_RANK]
        a_b = p[..., GLA_K + GLA_V + GLA_GATE_RANK:]
        return k, v, gla_log_gate(a_f, w_gate_up[0], b_gate[0]), gla_log_gate(a_b, w_gate_up[1], b_gate[1])

    def finish(o, r):
        b, l = o.shape[:2]
        o = o * lax.rsqrt(jnp.mean(o * o, axis=-1, keepdims=True) + EPS)
        o = (o.reshape(b, l, GLA_V) * norm_g.astype(jnp.float32)).astype(r.dtype)
        return (o * jax.nn.silu(r)) @ w_out

    zeros = jnp.zeros((batch, GLA_HEADS, GLA_DK, GLA_DV), jnp.float32)
    p_lat = h_lat @ w_in
    ql, rl = query_side(p_lat[..., :qs])
    kl, vl, laf_l, lab_l = state_side(p_lat[..., qs:])
    if with_ctx_out:
        p_ctx = h_ctx @ w_in
        qc, rc = query_side(p_ctx[..., :qs])
        kc, vc, laf_c, lab_c = state_side(p_ctx[..., qs:])
        oc_f, s_f = gla_scan(qc, kc, vc, laf_c, zeros)
        oc_b, s_b = gla_scan(flip(qc), flip(kc), flip(vc), flip(lab_c), zeros)
        out_ctx = finish(oc_f + flip(oc_b), rc)
    else:
        kc, vc, laf_c, lab_c = state_side(h_ctx @ w_in[:, qs:])
        s_f = gla_state(kc, vc, laf_c)
        s_b = gla_state(flip(kc), flip(vc), flip(lab_c))
        out_ctx = None
    ol_f, _ = gla_scan(ql, kl, vl, laf_l, s_f)
    ol_b, _ = gla_scan(flip(ql), flip(kl), flip(vl), flip(lab_l), s_b)
    out_lat = finish(ol_f + flip(ol_b), rl)
    return out_lat, out_ctx


def swiglu(h, w_gate, w_up, w_down):
    return (jax.nn.silu(h @ w_gate) * (h @ w_up)) @ w_down


def moe_swiglu(h, w_router, w_gate, w_up, w_down):
    logits = (h @ w_router).astype(jnp.float32)
    top_vals, top_idx = lax.top_k(logits, TOP_K)
    top_w = jax.nn.softmax(top_vals, axis=-1)
    gates = jnp.sum(jax.nn.one_hot(top_idx, N_EXPERTS, dtype=jnp.float32) * top_w[..., None], axis=-2)
    out = jnp.zeros_like(h)
    for e in range(N_EXPERTS):
        out = out + gates[..., e:e + 1].astype(h.dtype) * swiglu(h, w_gate[e], w_up[e], w_down[e])
    return out


def setup_inputs(seed: int = 0) -> dict:
    key = jax.random.key(seed)
    keys = iter(jax.random.split(key, 32))
    f32 = jnp.float32
    d = D_MODEL

    def nrm(shape, scale):
        return jax.random.normal(next(keys), shape, f32) * scale

    base_decay = jnp.log(-jnp.log1p(-(2.0 ** (-5.0 - jnp.arange(RET_HEADS, dtype=f32)))))
    return {
        'x': nrm((BATCH, SEQ, d), 1.0),
        'c': nrm((BATCH, d), 1.0),
        'ctx': nrm((BATCH, CTX_LEN, d), 1.0),
        'c_ctx': nrm((d,), 1.0),
        'ada_w': nrm((DEPTH, d, 6 * d), 0.5 * d ** -0.5),
        'ada_b': nrm((DEPTH, 6 * d), 0.02),
        'norm_mix_g': 1.0 + nrm((DEPTH, d), 0.02),
        'norm_ffn_g': 1.0 + nrm((DEPTH, d), 0.02),
        'final_g': 1.0 + nrm((d,), 0.02),
        'ret_w_in': nrm((N_RET, d, RET_IN), d ** -0.5),
        'ret_log_decay': base_decay[None, None, :] + nrm((N_RET, 2, RET_HEADS), 0.1),
        'ret_gn_w': 1.0 + nrm((N_RET, RET_V), 0.02),
        'ret_gn_b': nrm((N_RET, RET_V), 0.02),
        'ret_w_out': nrm((N_RET, RET_V, d), RET_V ** -0.5),
        'gla_w_in': nrm((N_GLA, d, GLA_IN), d ** -0.5),
        'gla_w_gate_up': nrm((N_GLA, 2, GLA_GATE_RANK, GLA_K), GLA_GATE_RANK ** -0.5),
        'gla_b_gate': nrm((N_GLA, 2, GLA_K), 0.1),
        'gla_norm_g': 1.0 + nrm((N_GLA, GLA_V), 0.02),
        'gla_w_out': nrm((N_GLA, GLA_V, d), GLA_V ** -0.5),
        'ffn_w_gate': nrm((N_RET, d, D_FF), d ** -0.5),
        'ffn_w_up': nrm((N_RET, d, D_FF), d ** -0.5),
        'ffn_w_down': nrm((N_RET, D_FF, d), D_FF ** -0.5),
        'moe_w_router': nrm((N_GLA, d, N_EXPERTS), d ** -0.5),
        'moe_w_gate': nrm((N_GLA, N_EXPERTS, d, D_FF_EXPERT), d ** -0.5),
        'moe_w_up': nrm((N_GLA, N_EXPERTS, d, D_FF_EXPERT), d ** -0.5),
        'moe_w_down': nrm((N_GLA, N_EXPERTS, D_FF_EXPERT, d), D_FF_EXPERT ** -0.5),
    }


def reference(x, c, ctx, c_ctx, ada_w, ada_b, norm_mix_g, norm_ffn_g, final_g,
              ret_w_in, ret_log_decay, ret_gn_w, ret_gn_b, ret_w_out,
              gla_w_in, gla_w_gate_up, gla_b_gate, gla_norm_g, gla_w_out,
              ffn_w_gate, ffn_w_up, ffn_w_down,
              moe_w_router, moe_w_gate, moe_w_up, moe_w_down):
    lat, cx = x, ctx
    silu_c = jax.nn.silu(c)
    silu_cc = jax.nn.silu(c_ctx)
    for i in range(DEPTH):
        last = i == DEPTH - 1
        j = i // N_MIXERS
        mod_l = (silu_c @ ada_w[i] + ada_b[i])[:, None, :]
        mod_c = (silu_cc @ ada_w[i] + ada_b[i])[None, None, :]
        sh1_l, sc1_l, g1_l, sh2_l, sc2_l, g2_l = jnp.split(mod_l, 6, axis=-1)
        sh1_c, sc1_c, g1_c, sh2_c, sc2_c, g2_c = jnp.split(mod_c, 6, axis=-1)

        h_l = rmsnorm(lat, norm_mix_g[i]) * (1.0 + sc1_l) + sh1_l
        h_c = rmsnorm(cx, norm_mix_g[i]) * (1.0 + sc1_c) + sh1_c
        if i % N_MIXERS == 0:
            mix_l, mix_c = retention_mixer(h_c, h_l, ret_w_in[j], ret_log_decay[j], ret_gn_w[j],
                                           ret_gn_b[j], ret_w_out[j], not last)
        else:
            mix_l, mix_c = gla_mixer(h_c, h_l, gla_w_in[j], gla_w_gate_up[j], gla_b_gate[j],
                                     gla_norm_g[j], gla_w_out[j], not last)
        lat = lat + g1_l * mix_l.astype(lat.dtype)
        if not last:
            cx = cx + g1_c * mix_c.astype(cx.dtype)

        h_l = rmsnorm(lat, norm_ffn_g[i]) * (1.0 + sc2_l) + sh2_l
        if last:
            n_c = 0
            h_all = h_l
        else:
            n_c = cx.shape[1]
            h_c = rmsnorm(cx, norm_ffn_g[i]) * (1.0 + sc2_c) + sh2_c
            h_all = jnp.concatenate([h_c, h_l], axis=1)
        if i % 2 == 0:
            f = swiglu(h_all, ffn_w_gate[j], ffn_w_up[j], ffn_w_down[j])
        else:
            f = moe_swiglu(h_all, moe_w_router[j], moe_w_gate[j], moe_w_up[j], moe_w_down[j])
        lat = lat + g2_l * f[:, n_c:].astype(lat.dtype)
        if not last:
            cx = cx + g2_c * f[:, :n_c].astype(cx.dtype)
    return rmsnorm(lat, final_g)
```

```python
import contextlib
import numpy as np
import concourse.bass as bass
import concourse.mybir as mybir
from concourse.bass_utils import run_bass_kernel_spmd

F32 = mybir.dt.float32
BF16 = mybir.dt.bfloat16
AF = mybir.ActivationFunctionType
ALU = mybir.AluOpType
AX = mybir.AxisListType

ENGS = ("pe", "act", "dve", "pool", "sp")
NSLOT = 8
QSLOT = {"pool": 5, "sp": 8, "act": 8, "pe": 8, "dve": 8}
SAME_ENGINE_SYNC = True
import os as _os
OPT = set(_os.environ.get("KOPT", "war,prefetch").split(","))

D = 1024
KC = 8
NCTX = 256
NLAT = 2048
T = NCTX + NLAT
NCH = T // 128
EPS = 1e-6
TILES = [(0, 256, 1)] + [(256 + 512 * i, 512, 0) for i in range(4)]
TG = {t[0]: i for i, t in enumerate(TILES)}
D_FF = 2816
D_FFE = 3584
NE = 8


class Buf:
    __slots__ = ("name", "w", "r", "rd")

    def __init__(self, name=""):
        self.name = name
        self.w = None
        self.r = {}
        self.rd = []


class Ins:
    __slots__ = ("eng", "fn", "deps", "sig", "sem", "semval", "dma")

    def __init__(self, eng, fn, dma):
        self.eng = eng
        self.fn = fn
        self.deps = []
        self.sig = False
        self.sem = None
        self.semval = 0
        self.dma = dma


class Prog:
    def __init__(self, nc):
        self.nc = nc
        self.q = {e: [] for e in ENGS}
        self.dma_cnt = {e: 0 for e in ENGS}
        self.slot_last = {e: [None] * NSLOT for e in ENGS}
        self.slot_uses = {e: [0] * NSLOT for e in ENGS}

    def op(self, eng, fn, reads=(), writes=(), dma=False):
        ins = Ins(eng, fn, dma)
        deps = {}

        def add(d, war):
            if d is None:
                return
            if d.eng == eng and not d.dma and not dma:
                if eng in ("pe", "sp"):
                    return
                if not SAME_ENGINE_SYNC:
                    return
                if war and "war" not in OPT:
                    return
            deps[id(d)] = d

        for b in reads:
            add(b.w, False)
        for b in writes:
            add(b.w, False)
            for d in b.r.values():
                add(d, True)
            for d in b.rd:
                add(d, True)
        if dma:
            k = self.dma_cnt[eng] % QSLOT[eng]
            self.dma_cnt[eng] += 1
            prev = self.slot_last[eng][k]
            if prev is not None:
                deps[id(prev)] = prev
            self.slot_last[eng][k] = ins
            self.slot_uses[eng][k] += 1
            ins.sem = ("dma", eng, k)
            ins.semval = 16 * self.slot_uses[eng][k]
            ins.sig = True
        ins.deps = list(deps.values())
        for d in ins.deps:
            d.sig = True
        for b in reads:
            if dma:
                b.rd.append(ins)
            else:
                b.r[eng] = ins
        for b in writes:
            b.w = ins
            b.r = {}
            b.rd = []
        self.q[eng].append(ins)
        return ins

    def barrier(self):
        lasts = []
        for e in ENGS:
            for ins in reversed(self.q[e]):
                if not ins.dma and ins.fn is not None:
                    lasts.append(ins)
                    break
            for k in range(NSLOT):
                if self.slot_last[e][k] is not None:
                    lasts.append(self.slot_last[e][k])
        for e in ENGS:
            ins = Ins(e, None, False)
            ins.deps = list(lasts)
            for d in ins.deps:
                d.sig = True
            self.q[e].append(ins)

    def emit(self):
        nc = self.nc
        with contextlib.ExitStack() as st:
            esem = {e: st.enter_context(nc.semaphore("s_" + e)) for e in ENGS}
            dsem = {}
            for e in ENGS:
                for k in range(NSLOT):
                    if self.slot_uses[e][k]:
                        dsem[("dma", e, k)] = st.enter_context(nc.semaphore("d_%s%d" % (e, k)))
            for e in ENGS:
                c = 0
                for ins in self.q[e]:
                    if ins.dma or ins.fn is None:
                        continue
                    if ins.sig:
                        c += 1
                        ins.sem = ("eng", e)
                        ins.semval = c

            def semof(key):
                return esem[key[1]] if key[0] == "eng" else dsem[key]

            block = st.enter_context(nc.Block())

            def run(ename, eng):
                waited = {}
                for ins in self.q[ename]:
                    for d in ins.deps:
                        if waited.get(d.sem, 0) >= d.semval:
                            continue
                        eng.wait_ge(semof(d.sem), d.semval)
                        waited[d.sem] = d.semval
                    if ins.fn is None:
                        continue
                    r = ins.fn(eng)
                    if ins.sig:
                        r.then_inc(semof(ins.sem), 16 if ins.dma else 1)

            @block.tensor
            def _(eng):
                run("pe", eng)

            @block.scalar
            def _(eng):
                run("act", eng)

            @block.vector
            def _(eng):
                run("dve", eng)

            @block.gpsimd
            def _(eng):
                run("pool", eng)

            @block.sync
            def _(eng):
                run("sp", eng)


class Arena:
    def __init__(self, big, nbytes):
        self.big = big
        self.nbytes = nbytes
        self.off = 0

    def alloc(self, shape, dt):
        size = 4 if dt == F32 else 2
        n = int(np.prod(shape))
        nb = (n * size + 63) // 64 * 64
        assert self.off + nb <= self.nbytes, ("SBUF arena overflow", self.off, nb, self.nbytes)
        ap = self.big[:, self.off // 2:(self.off + n * size) // 2]
        self.off += nb
        if dt == F32:
            ap = ap.bitcast(F32)
        if len(shape) == 2:
            ap = ap.rearrange("p (a b) -> p a b", a=shape[0])
        elif len(shape) == 3:
            ap = ap.rearrange("p (a b c) -> p a b c", a=shape[0], b=shape[1])
        elif len(shape) == 4:
            ap = ap.rearrange("p (a b c d) -> p a b c d", a=shape[0], b=shape[1], c=shape[2])
        return ap

    def mark(self):
        return self.off

    def release(self, m):
        self.off = m


C_ID, C_DPOS, C_DNEG, C_LO, C_UP, C_I1, C_IB, C_PF, C_PB, C_ROPE = 0, 128, 256, 384, 512, 640, 768, 896, 897, 898
C_ROPEK = 898 + 192
C_N = 898 + 384


def host_consts():
    c = np.zeros((128, C_N), np.float32)
    p = np.arange(128)[:, None].astype(np.float32)
    i = np.arange(128)[None, :].astype(np.float32)
    c[:, C_ID:C_ID + 128] = np.eye(128)
    c[:, C_DPOS:C_DPOS + 128] = np.maximum(i - p, 0)
    c[:, C_DNEG:C_DNEG + 128] = np.maximum(p - i, 0)
    c[:, C_LO:C_LO + 128] = (i >= p)
    c[:, C_UP:C_UP + 128] = (i <= p)
    c[:, C_I1:C_I1 + 128] = i + 1
    c[:, C_IB:C_IB + 128] = 128 - i
    c[:, C_PF] = 127 - p[:, 0]
    c[:, C_PB] = p[:, 0]
    half = 64
    freqs = (10000.0 ** (-np.arange(half, dtype=np.float32) / half)).astype(np.float32)
    fr = np.concatenate([freqs, freqs])[:, None]
    rows = np.arange(32, dtype=np.float32)[None, :]
    cols = np.arange(64, dtype=np.float32)[None, :]
    c[:, C_ROPE:C_ROPE + 32] = np.cos(rows * fr)
    sgn = np.where(np.arange(128) < 64, -1.0, 1.0).astype(np.float32)[:, None]
    c[:, C_ROPE + 32:C_ROPE + 64] = np.sin(rows * fr) * sgn
    c[:, C_ROPE + 64:C_ROPE + 128] = np.cos(cols * fr)
    c[:, C_ROPE + 128:C_ROPE + 192] = np.sin(cols * fr) * sgn
    c[:, C_ROPEK:C_ROPEK + 192] = c[:, C_ROPE:C_ROPE + 192] * np.float32(0.0625)
    return c


def build(n_layers=4, taps=()):
    nc = bass.Bass("TRN2", target_bir_lowering=False)
    P = Prog(nc)

    def din(name, shape, dt=F32):
        return nc.dram_tensor(name, list(shape), dt, kind="ExternalInput").ap()

    xT = din("xT", [D, T])
    cc = din("cc", [128, 2, 8])
    cst_d = din("cst", [128, C_N])
    ada_w = din("ada_w", [4, D, 6 * D])
    ada_b = din("ada_b_fm", [128, 4, 48])
    ngm_d = din("ngm_fm", [128, 4, 8])
    ngf_d = din("ngf_fm", [128, 4, 8])
    fg_d = din("fg_fm", [128, 8])
    ret_w_in = din("ret_w_in", [2, D, 6144])
    ret_ld = din("ret_ld", [2, 8])
    ret_gnw = din("ret_gnw_fm", [128, 2, 16])
    ret_gnb = din("ret_gnb_fm", [128, 2, 16])
    ret_w_out = din("ret_w_out", [2, 2048, D])
    gla_w_in = din("gla_w_in", [2, D, 3104])
    gla_wgu = din("gla_w_gate_up", [2, 2, 16, 512])
    gla_bg = din("gla_bg_fm", [128, 2, 2, 4])
    gla_ng = din("gla_ng_fm", [128, 2, 8])
    gla_w_out = din("gla_w_out", [2, D, D])
    ffn_wg = din("ffn_w_gate", [2, D, D_FF])
    ffn_wu = din("ffn_w_up", [2, D, D_FF])
    ffn_wd = din("ffn_w_down", [2, D_FF, D])
    moe_wr = din("moe_w_router", [2, D, NE])
    moe_wg = din("moe_w_gate", [2, NE, D, D_FFE])
    moe_wu = din("moe_w_up", [2, NE, D, D_FFE])
    moe_wd = din("moe_w_down", [2, NE, D_FFE, D])

    outT = nc.dram_tensor("outT", [D, NLAT], F32, kind="ExternalOutput").ap()
    x_dram = nc.dram_tensor("x_scr", [D, T], F32, kind="Internal").ap()
    yT_dram = nc.dram_tensor("yT_scr", [2048, T], BF16, kind="Internal").ap()
    sb_dram = nc.dram_tensor("sb_scr", [NCH, 128, 1024], BF16, kind="Internal").ap()
    tap_out = {}
    for (name, shape) in taps:
        tap_out[name] = nc.dram_tensor("tap_" + name, list(shape), F32, kind="ExternalOutput").ap()

    ARENA_BYTES = 207 * 1024
    with contextlib.ExitStack() as st:
        big = st.enter_context(nc.sbuf_tensor("big", [128, ARENA_BYTES // 2], BF16))
        psum = st.enter_context(nc.psum_tensor("psum", [128, 8, 512], F32))
        A = Arena(big, ARENA_BYTES)
        PB = [Buf("bank%d" % i) for i in range(8)]
        bank = [psum[:, i, :] for i in range(8)]
        B_xd = Buf("x_dram")
        B_yd = Buf("yT_dram")
        B_sbd = [Buf("sbd%d" % i) for i in range(NCH)]
        B_out = Buf("out")

        def dma(eng, out, in_, reads, writes):
            return P.op(eng, lambda e: e.dma_start(out=out, in_=in_), reads, writes, dma=True)

        def mm(out, lhsT, rhs, start, stop, reads, writes):
            return P.op("pe", lambda e: e.matmul(out, lhsT, rhs, start=start, stop=stop), reads, writes)

        def tr(out, in_, ident, reads, writes):
            return P.op("pe", lambda e: e.transpose(out, in_, ident), reads, writes)

        def act(out, in_, func, reads, writes, **kw):
            return P.op("act", lambda e: e.activation(out=out, in_=in_, func=func, **kw), reads, writes)

        def tt(eng, out, in0, in1, op, reads, writes):
            return P.op(eng, lambda e: e.tensor_tensor(out=out, in0=in0, in1=in1, op=op), reads, writes)

        def ts(eng, out, in0, s1, s2, op0, op1, reads, writes):
            if s2 is None:
                return P.op(eng, lambda e: e.tensor_scalar(out=out, in0=in0, scalar1=s1, scalar2=None, op0=op0), reads, writes)
            return P.op(eng, lambda e: e.tensor_scalar(out=out, in0=in0, scalar1=s1, scalar2=s2, op0=op0, op1=op1), reads, writes)

        def stt(out, in0, scalar, in1, op0, op1, reads, writes):
            return P.op("dve", lambda e: e.scalar_tensor_tensor(out=out, in0=in0, scalar=scalar, in1=in1, op0=op0, op1=op1), reads, writes)

        def cp(eng, out, in_, reads, writes):
            if eng == "act":
                return P.op("act", lambda e: e.copy(out=out, in_=in_), reads, writes)
            return P.op(eng, lambda e: e.tensor_copy(out=out, in_=in_), reads, writes)

        def tap(name, src, reads):
            if name in tap_out:
                dma("sp", tap_out[name], src, reads, [B_out])

        cst = A.alloc([C_N], F32); B_cst = Buf("cst")
        identb = A.alloc([128], BF16)
        onesb = A.alloc([128], BF16)
        onesf = A.alloc([128], F32)
        epsc = A.alloc([1], F32)
        modt = A.alloc([4, 48, 2], F32); B_mod = Buf("mod")
        gm1 = A.alloc([4, 8, 2], F32)
        gm2 = A.alloc([4, 8, 2], F32)
        adab = A.alloc([4, 48], F32)
        ngm = A.alloc([4, 8], F32)
        ngf = A.alloc([4, 8], F32)
        fgt = A.alloc([8], F32)
        B_small = Buf("small")
        dma("sp", cst, cst_d, [], [B_cst])
        dma("sp", adab, ada_b, [], [B_small])
        dma("sp", ngm, ngm_d, [], [B_small])
        dma("sp", ngf, ngf_d, [], [B_small])
        dma("sp", fgt, fg_d, [], [B_small])
        cp("dve", identb, cst[:, C_ID:C_ID + 128], [B_cst], [B_small])
        P.op("dve", lambda e: e.memset(onesb, 1.0), [], [B_small])
        P.op("dve", lambda e: e.memset(onesf, 1.0), [], [B_small])
        P.op("dve", lambda e: e.memset(epsc, EPS), [], [B_small])
        P.barrier()
        base_mark = A.mark()

        def stage_adaln():
            m = A.mark()
            cs = A.alloc([2, 8], F32)
            csb = A.alloc([8, 2], BF16)
            wsl = [A.alloc([8, 1536], BF16) for _ in range(2)]
            Bw = [Buf("aw0"), Buf("aw1")]
            Bc = Buf("cs")
            tmpm = A.alloc([8, 2], F32)
            dma("sp", cs, cc, [], [Bc])
            act(csb, cs.rearrange("p j c -> p c j"), AF.Silu, [Bc], [Bc])
            for i in range(n_layers):
                pb = i % 2
                for cb in range(4):
                    s = (i * 4 + cb) % 2
                    dma("pool", wsl[s], ada_w[i].rearrange("(kc p) n -> p kc n", p=128)[:, :, cb * 1536:(cb + 1) * 1536], [], [Bw[s]])
                    for j in range(12):
                        col = (cb * 12 + j) * 2
                        for kc in range(KC):
                            mm(bank[pb][:, col:col + 2], wsl[s][:, kc, j * 128:(j + 1) * 128], csb[:, kc, :], kc == 0, kc == KC - 1, [Bw[s], Bc], [PB[pb]])
                tt("dve", modt[:, i], bank[pb][:, 0:96].rearrange("p (q j) -> p q j", j=2), adab[:, i, :].unsqueeze(2).to_broadcast([128, 48, 2]), ALU.add, [PB[pb], B_small], [B_mod])
                for (gm, ng, off) in ((gm1, ngm, 8), (gm2, ngf, 32)):
                    ts("dve", tmpm, modt[:, i, off:off + 8, :], 1.0, None, ALU.add, None, [B_mod], [Bc])
                    tt("dve", gm[:, i], tmpm, ng[:, i, :].unsqueeze(2).to_broadcast([128, 8, 2]), ALU.mult, [Bc, B_small], [B_mod])
            P.barrier()
            A.release(m)

        def stage_norm(layer, gm, sh_off, hT, Bh, x_sb=None, Bx=None, xsrc=None, Bxs=None, tiles=TILES, hf_cb=None):
            m = A.mark()
            stg = None
            if x_sb is None:
                stg = [A.alloc([8, 512], F32) for _ in range(2)]
                Bst = [Buf(), Buf()]
            sq = [A.alloc([8, 512], BF16) for _ in range(2)]
            Bsq = [Buf(), Buf()]
            rs = [A.alloc([512], F32) for _ in range(2)]
            Brs = [Buf(), Buf()]
            tmp = [A.alloc([8, 512], F32) for _ in range(2)]
            Btmp = [Buf(), Buf()]
            if hf_cb is not None:
                hf = [A.alloc([8, 512], F32) for _ in range(2)]
                Bhf = [Buf(), Buf()]
            info = {}

            def front(ti):
                (t0, n, j) = tiles[ti]
                s = ti % 2
                if x_sb is None:
                    dma("sp", stg[s][:, :, 0:n], xsrc.rearrange("(c p) t -> p c t", p=128)[:, :, t0:t0 + n], [Bxs], [Bst[s]])
                    xt = stg[s][:, :, 0:n]
                    Bxl = [Bst[s]]
                else:
                    xt = x_sb[:, :, t0:t0 + n]
                    Bxl = Bx[TG[t0]]
                info[ti] = (xt, Bxl)
                act(sq[s][:, :, 0:n], xt, AF.Square, Bxl, [Bsq[s]])
                pb = 6 + s
                for c in range(KC):
                    mm(bank[pb][:, 0:n], onesb, sq[s][:, c, 0:n], c == 0, c == KC - 1, [Bsq[s], B_small], [PB[pb]])

            def frontB(ti):
                (t0, n, j) = tiles[ti]
                s = ti % 2
                pb = 6 + s
                act(rs[s][:, 0:n], bank[pb][:, 0:n], AF.Ln, [PB[pb]], [Brs[s]], scale=1.0 / D, bias=epsc[:, 0:1])
                act(rs[s][:, 0:n], rs[s][:, 0:n], AF.Exp, [Brs[s]], [Brs[s]], scale=-0.5)

            def back(ti):
                (t0, n, j) = tiles[ti]
                s = ti % 2
                (xt, Bxl) = info[ti]
                tt("dve", tmp[s][:, :, 0:n], xt, rs[s][:, 0:n].unsqueeze(1).to_broadcast([128, 8, n]), ALU.mult, Bxl + [Brs[s]], [Btmp[s]])
                for c in range(KC):
                    sc_ap = gm[:, layer, c, j:j + 1]
                    bi_ap = modt[:, layer, sh_off + c, j:j + 1]
                    if hf_cb is None:
                        if c < 4:
                            act(hT[:, c, t0:t0 + n], tmp[s][:, c, 0:n], AF.Identity, [Btmp[s], B_mod], [Bh[ti]], scale=sc_ap, bias=bi_ap)
                        else:
                            ts("pool", hT[:, c, t0:t0 + n], tmp[s][:, c, 0:n], sc_ap, bi_ap, ALU.mult, ALU.add, [Btmp[s], B_mod], [Bh[ti]])
                    else:
                        act(hT[:, c, t0:t0 + n], tmp[s][:, c, 0:n], AF.Identity, [Btmp[s], B_mod], [Bh[ti]], scale=sc_ap, bias=bi_ap)
                        ts("dve" if c < 4 else "pool", hf[s][:, c, 0:n], tmp[s][:, c, 0:n], sc_ap, bi_ap, ALU.mult, ALU.add, [Btmp[s], B_mod], [Bhf[s]])
                if hf_cb is not None:
                    hf_cb(ti, t0, n, hf[s], Bhf[s])

            front(0)
            frontB(0)
            for ti in range(len(tiles)):
                if ti + 1 < len(tiles):
                    front(ti + 1)
                back(ti)
                if ti + 1 < len(tiles):
                    frontB(ti + 1)
            P.barrier()
            A.release(m)

        def stage_retention(layer, hT, Bh):
            jl = layer // 2
            m = A.mark()
            ld = A.alloc([8], F32); lg = A.alloc([8], F32); cdec = A.alloc([8], F32)
            maskT = A.alloc([4, 128], F32)
            decf = A.alloc([4, 128], F32); decb = A.alloc([4, 128], F32)
            kdecf = A.alloc([4], F32); kdecb = A.alloc([4], F32)
            mt = A.alloc([128], F32)
            gnw = A.alloc([16], F32); gnb = A.alloc([16], F32)
            Bd = Buf("dec")
            dma("sp", ld, ret_ld[jl:jl + 1, :].partition_broadcast(128), [], [Bd])
            dma("sp", gnw, ret_gnw[:, jl, :], [], [Bd])
            dma("sp", gnb, ret_gnb[:, jl, :], [], [Bd])
            act(lg, ld, AF.Exp, [Bd], [Bd])
            ts("dve", lg, lg, -1.0, None, ALU.mult, None, [Bd], [Bd])
            ts("dve", cdec, lg, 128.0, None, ALU.mult, None, [Bd], [Bd])
            act(cdec, cdec, AF.Exp, [Bd], [Bd])
            for hd in range(4):
                f, b = hd, 4 + hd
                act(maskT[:, hd], cst[:, C_DPOS:C_DPOS + 128], AF.Exp, [Bd, B_cst], [Bd], scale=lg[:, f:f + 1])
                tt("dve", maskT[:, hd], maskT[:, hd], cst[:, C_LO:C_LO + 128], ALU.mult, [Bd], [Bd])
                act(mt, cst[:, C_DNEG:C_DNEG + 128], AF.Exp, [Bd, B_cst], [Bd], scale=lg[:, b:b + 1])
                tt("dve", mt, mt, cst[:, C_UP:C_UP + 128], ALU.mult, [Bd], [Bd])
                tt("dve", maskT[:, hd], maskT[:, hd], mt, ALU.add, [Bd], [Bd])
                act(decf[:, hd], cst[:, C_I1:C_I1 + 128], AF.Exp, [Bd, B_cst], [Bd], scale=lg[:, f:f + 1])
                act(decb[:, hd], cst[:, C_IB:C_IB + 128], AF.Exp, [Bd, B_cst], [Bd], scale=lg[:, b:b + 1])
                act(kdecf[:, hd:hd + 1], cst[:, C_PF:C_PF + 1], AF.Exp, [Bd, B_cst], [Bd], scale=lg[:, f:f + 1])
                act(kdecb[:, hd:hd + 1], cst[:, C_PB:C_PB + 1], AF.Exp, [Bd, B_cst], [Bd], scale=lg[:, b:b + 1])
            wq = A.alloc([8, 256], BF16); wqr = A.alloc([8, 256], BF16)
            wk = A.alloc([8, 256], BF16); wkr = A.alloc([8, 256], BF16)
            wv = A.alloc([8, 512], BF16); wg = A.alloc([8, 512], BF16)
            Bwq, Bwk, Bwv, Bwg = Buf("wq"), Buf("wk"), Buf("wv"), Buf("wg")
            qT = A.alloc([2, T], BF16)
            qfc = [A.alloc([2, 128], BF16) for _ in range(2)]; qbc = [A.alloc([2, 128], BF16) for _ in range(2)]; Bqc = [Buf(), Buf()]
            kT = A.alloc([2, T], BF16)
            kdf = A.alloc([NCH, 256], BF16); kdb = A.alloc([NCH, 256], BF16)
            v = A.alloc([NCH, 512], BF16)
            sg = A.alloc([4, T], BF16)
            Bq, Bk, Bkd, Bv, Bsg = Buf("q"), Buf("k"), Buf("kd"), Buf("v"), Buf("sg")
            t1 = [A.alloc([512], F32)] * 2
            t2 = [A.alloc([512], F32)] * 2
            qr = [A.alloc([512], F32) for _ in range(2)]
            Bt1, Bt2, Bqr = [Buf()] * 2, [Buf()] * 2, [Buf(), Buf()]
            Sf = A.alloc([2, 512], F32); Sb = A.alloc([2, 512], F32)
            Sfb = [A.alloc([2, 512], BF16) for _ in range(2)]; Sbb = [A.alloc([2, 512], BF16) for _ in range(2)]
            Sbin = [A.alloc([2, 512], BF16) for _ in range(2)]
            BSf, BSb, BSfb = Buf("Sf"), Buf("Sb"), [Buf("Sfb0"), Buf("Sfb1")]
            BSbb, BSbin = [Buf(), Buf()], [Buf(), Buf()]
            PT = [A.alloc([128], BF16) for _ in range(2)]; BPT = [Buf(), Buf()]
            osb = [A.alloc([512], F32) for _ in range(2)]; Bosb = [Buf(), Buf()]
            junk = t1[0]; Bjunk = Bt1[0]
            stat = [A.alloc([8], F32) for _ in range(2)]; Bstat = [Buf(), Buf()]
            onb = [A.alloc([512], BF16) for _ in range(2)]; Bonb = [Buf(), Buf()]
            tmpn = [A.alloc([4, 128], F32) for _ in range(2)]; Btn = [Buf(), Buf()]; Btnv = [[Buf() for _ in range(4)] for _ in range(2)]
            ystg = [A.alloc([4, 512], BF16) for _ in range(2)]; Bys = [Buf(), Buf()]
            w_in = ret_w_in[jl].rearrange("(kc p) n -> p kc n", p=128)
            psT = [psum[:, 4 + i, :].bitcast(BF16) for i in range(2)]
            psT2 = [psum[:, 6 + i, :].bitcast(BF16) for i in range(2)]

            def rot_weights(w, wr_, Bw):
                w4 = w.rearrange("p k (b h x) -> p (k b) h x", b=2, h=2)
                r4 = wr_.rearrange("p k (b h x) -> p (k b) h x", b=2, h=2)
                cp("dve", r4[:, :, 0, :], w4[:, :, 1, :], [Bw], [Bw])
                cp("dve", r4[:, :, 1, :], w4[:, :, 0, :], [Bw], [Bw])

            def rope_evac(dst_list, b_main, b_rot, dc, t0, n, j, s, Bdst, hd, with_decay, RB=C_ROPE, csc=1.0):
                if j == 0:
                    r0 = (t0 - NCTX) // 64
                    nr = n // 64
                    if dc == 0:
                        cosv = cst[:, RB + r0:RB + r0 + nr].unsqueeze(2).to_broadcast([128, nr, 64])
                        sinv = cst[:, RB + 32 + r0:RB + 32 + r0 + nr].unsqueeze(2).to_broadcast([128, nr, 64])
                    else:
                        cosv = cst[:, RB + 64:RB + 128].unsqueeze(1).to_broadcast([128, nr, 64])
                        sinv = cst[:, RB + 128:RB + 192].unsqueeze(1).to_broadcast([128, nr, 64])
                    v3 = lambda ap: ap.rearrange("p (r c) -> p r c", c=64)
                    tt("dve", v3(t1[s][:, 0:n]), v3(bank[b_main][:, 0:n]), cosv, ALU.mult, [PB[b_main], B_cst], [Bt1[s]])
                    tt("dve", v3(t2[s][:, 0:n]), v3(bank[b_rot][:, 0:n]), sinv, ALU.mult, [PB[b_rot], B_cst], [Bt2[s]])
                    tt("pool", qr[s][:, 0:n], t1[s][:, 0:n], t2[s][:, 0:n], ALU.add, [Bt1[s], Bt2[s]], [Bqr[s]])
                else:
                    act(qr[s][:, 0:n], bank[b_main][:, 0:n], AF.Copy, [PB[b_main]], [Bqr[s]], scale=csc)
                cp("act", dst_list[0][:, dc, t0:t0 + n], qr[s][:, 0:n], [Bqr[s]], [Bdst])
                if with_decay:
                    nck = n // 128
                    q3 = qr[s][:, 0:n].rearrange("p (c i) -> p c i", i=128)
                    tt("pool", dst_list[1][:, dc, t0:t0 + n].rearrange("p (c i) -> p c i", i=128), q3,
                       decf[:, hd].unsqueeze(1).to_broadcast([128, nck, 128]), ALU.mult, [Bqr[s], Bd], [Bdst])
                    tt("dve", dst_list[2][:, dc, t0:t0 + n].rearrange("p (c i) -> p c i", i=128), q3,
                       decb[:, hd].unsqueeze(1).to_broadcast([128, nck, 128]), ALU.mult, [Bqr[s], Bd], [Bdst])

            def load_head_weights(hd):
                dma("pool", wq, w_in[:, :, hd * 256:(hd + 1) * 256], [], [Bwq])
                rot_weights(wq, wqr, Bwq)
                dma("pool", wk, w_in[:, :, 3072 + hd * 256:3072 + (hd + 1) * 256], [], [Bwk])
                rot_weights(wk, wkr, Bwk)
                dma("pool", wv, w_in[:, :, 4096 + hd * 512:4096 + (hd + 1) * 512], [], [Bwv])
                dma("pool", wg, w_in[:, :, 1024 + hd * 512:1024 + (hd + 1) * 512], [], [Bwg])

            load_head_weights(0)
            for hd in range(4):
                if hd > 0 and "prefetch" not in OPT:
                    load_head_weights(hd)
                cnt = 0
                for ti, (t0, n, j) in enumerate(TILES):
                    for (w, wr_, Bw, dsts, Bdst, wd, RB, csc) in ((wq, wqr, Bwq, (qT,), Bq, False, C_ROPE, 1.0), (wk, wkr, Bwk, (kT,), Bk, False, C_ROPEK, 0.0625)):
                        for dc in range(2):
                            bm, br = 0 + (cnt % 2) * 2, 1 + (cnt % 2) * 2
                            for kc in range(KC):
                                mm(bank[bm][:, 0:n], w[:, kc, dc * 128:(dc + 1) * 128], hT[:, kc, t0:t0 + n], kc == 0, kc == KC - 1, [Bw, Bh[ti]], [PB[bm]])
                            if j == 0:
                                for kc in range(KC):
                                    mm(bank[br][:, 0:n], wr_[:, kc, dc * 128:(dc + 1) * 128], hT[:, kc, t0:t0 + n], kc == 0, kc == KC - 1, [Bw, Bh[ti]], [PB[br]])
                            rope_evac(dsts, bm, br, dc, t0, n, j, cnt % 2, Bdst, hd, wd, RB, csc)
                            cnt += 1
                    for vc in range(4):
                        pb = 6 + (vc % 2)
                        for kc in range(KC):
                            mm(bank[pb][:, 0:n], wg[:, kc, vc * 128:(vc + 1) * 128], hT[:, kc, t0:t0 + n], kc == 0, kc == KC - 1, [Bwg, Bh[ti]], [PB[pb]])
                        act(sg[:, vc, t0:t0 + n], bank[pb][:, 0:n], AF.Silu, [PB[pb]], [Bsg])
                    for sub in range(n // 128):
                        c = t0 // 128 + sub
                        pb = 6 + (sub % 2)
                        for kc in range(KC):
                            mm(bank[pb], hT[:, kc, c * 128:(c + 1) * 128], wv[:, kc, :], kc == 0, kc == KC - 1, [Bwv, Bh[ti]], [PB[pb]])
                        cp("act", v[:, c, :], bank[pb], [PB[pb]], [Bv])
                if hd < 3 and "prefetch" in OPT:
                    load_head_weights(hd + 1)
                for c in range(NCH):
                    s = c % 2
                    for dc in range(2):
                        tr(psT[s][:, dc * 128:(dc + 1) * 128], kT[:, dc, c * 128:(c + 1) * 128], identb, [Bk, B_small], [PB[4 + s]])
                    ts("dve", kdf[:, c, :], psT[s][:, 0:256], kdecf[:, hd:hd + 1], None, ALU.mult, None, [PB[4 + s], Bd], [Bkd])
                    ts("dve", kdb[:, c, :], psT[s][:, 0:256], kdecb[:, hd:hd + 1], None, ALU.mult, None, [PB[4 + s], Bd], [Bkd])
                BSbd = [Buf(), Buf()]
                P.op("dve", lambda e: e.memset(Sb, 0.0), [], [BSb])
                orderB = [1, 0] + list(range(NCH - 1, 1, -1))
                for idx, c in enumerate(orderB):
                    s = idx % 2
                    for dc in range(2):
                        cp("act", Sbb[s][:, dc, :], Sb[:, dc, :], [BSb, BSbd[dc]], [BSbb[s]])
                    dma("sp", sb_dram[c].rearrange("p (a b) -> p a b", a=2), Sbb[s], [BSbb[s]], [B_sbd[c]])
                    for dc in range(2):
                        pb = 0 + dc
                        mm(bank[pb], kdb[:, c, dc * 128:(dc + 1) * 128], v[:, c, :], True, True, [Bkd, Bv], [PB[pb]])
                        stt(Sb[:, dc, :], Sb[:, dc, :], cdec[:, 4 + hd:5 + hd], bank[pb], ALU.mult, ALU.add, [BSb, BSbd[dc], PB[pb], Bd], [BSbd[dc]])
                P.op("dve", lambda e: e.memset(Sf, 0.0), [], [BSf])
                P.op("pool", lambda e: e.memset(Sfb[0], 0.0), [], [BSfb[0]])
                gstate = [0]

                def stA(c, hd=hd):
                    s = c % 2
                    ck = slice(c * 128, (c + 1) * 128)
                    dma("sp", Sbin[s], sb_dram[c].rearrange("p (a b) -> p a b", a=2), [B_sbd[c]], [BSbin[s]])
                    for dc in range(2):
                        mm(bank[2][:, 0:128], kT[:, dc, ck], qT[:, dc, ck], dc == 0, dc == 1, [Bk, Bq], [PB[2]])
                    for dc in range(2):
                        mm(bank[dc], kdf[:, c, dc * 128:(dc + 1) * 128], v[:, c, :], True, True, [Bkd, Bv], [PB[dc]])
                    tt("dve", PT[s], bank[2][:, 0:128], maskT[:, hd], ALU.mult, [PB[2], Bd], [BPT[s]])
                    tt("pool", qfc[s], qT[:, :, ck], decf[:, hd].unsqueeze(1).to_broadcast([128, 2, 128]), ALU.mult, [Bq, Bd], [Bqc[s]])
                    tt("pool", qbc[s], qT[:, :, ck], decb[:, hd].unsqueeze(1).to_broadcast([128, 2, 128]), ALU.mult, [Bq, Bd], [Bqc[s]])
                    mm(bank[3], PT[s], v[:, c, :], True, False, [BPT[s], Bv], [PB[3]])
                    for dc in range(2):
                        mm(bank[3], qfc[s][:, dc, :], Sfb[s][:, dc, :], False, False, [Bqc[s], BSfb[s]], [PB[3]])
                    for dc in range(2):
                        mm(bank[3], qbc[s][:, dc, :], Sbin[s][:, dc, :], False, dc == 1, [Bqc[s], BSbin[s]], [PB[3]])
                    for dc in range(2):
                        stt(Sf[:, dc, :], Sf[:, dc, :], cdec[:, hd:hd + 1], bank[dc], ALU.mult, ALU.add, [BSf, PB[dc], Bd], [BSf])
                    cp("dve", Sfb[1 - s], Sf, [BSf], [BSfb[1 - s]])
                    act(osb[s], bank[3], AF.Identity, [PB[3]], [Bosb[s], Bstat[s]], accum_out=stat[s][:, 0:1])
                    act(junk, osb[s], AF.Square, [Bosb[s]], [Bjunk, Bstat[s]], accum_out=stat[s][:, 1:2])

                def stB(c, hd=hd):
                    s = c % 2
                    sv = stat[s]
                    ts("dve", sv[:, 2:3], sv[:, 0:1], 1.0 / 512, None, ALU.mult, None, [Bstat[s]], [Bstat[s]])
                    tt("dve", sv[:, 3:4], sv[:, 2:3], sv[:, 2:3], ALU.mult, [Bstat[s]], [Bstat[s]])
                    stt(sv[:, 4:5], sv[:, 1:2], 1.0 / 512, sv[:, 3:4], ALU.mult, ALU.subtract, [Bstat[s]], [Bstat[s]])
                    ts("dve", sv[:, 4:5], sv[:, 4:5], EPS, None, ALU.add, None, [Bstat[s]], [Bstat[s]])
                    act(sv[:, 4:5], sv[:, 4:5], AF.Sqrt, [Bstat[s]], [Bstat[s]])
                    P.op("dve", lambda e, sv=sv: e.reciprocal(out=sv[:, 5:6], in_=sv[:, 4:5]), [Bstat[s]], [Bstat[s]])
                    stt(sv[:, 6:7], sv[:, 2:3], -1.0, sv[:, 5:6], ALU.mult, ALU.mult, [Bstat[s]], [Bstat[s]])
                    act(onb[s], osb[s], AF.Identity, [Bosb[s], Bstat[s]], [Bonb[s]], scale=sv[:, 5:6], bias=sv[:, 6:7])
                    for vc in range(4):
                        if vc < 2:
                            tr(psT[s][:, vc * 128:(vc + 1) * 128], onb[s][:, vc * 128:(vc + 1) * 128], identb, [Bonb[s], B_small], [PB[4 + s]])
                        else:
                            tr(psT2[s][:, vc * 128:(vc + 1) * 128], onb[s][:, vc * 128:(vc + 1) * 128], identb, [Bonb[s], B_small], [PB[6 + s]])

                def stC(c, hd=hd):
                    s = c % 2
                    ck = slice(c * 128, (c + 1) * 128)
                    for vc in range(4):
                        if vc < 2:
                            act(tmpn[s][:, vc, :], psT[s][:, vc * 128:(vc + 1) * 128], AF.Identity, [PB[4 + s], Bd], [Btnv[s][vc]],
                                scale=gnw[:, hd * 4 + vc:hd * 4 + vc + 1], bias=gnb[:, hd * 4 + vc:hd * 4 + vc + 1])
                        else:
                            ts("dve", tmpn[s][:, vc, :], psT2[s][:, vc * 128:(vc + 1) * 128], gnw[:, hd * 4 + vc:hd * 4 + vc + 1], gnb[:, hd * 4 + vc:hd * 4 + vc + 1],
                               ALU.mult, ALU.add, [PB[6 + s], Bd], [Btnv[s][vc]])
                    if c < 2:
                        g0, gn = 0, 2
                    else:
                        g0, gn = 2 + ((c - 2) // 4) * 4, 4
                    gs = gstate[0] % 2
                    ci = c - g0
                    tt("pool", ystg[gs][:, :, ci * 128:(ci + 1) * 128], tmpn[s], sg[:, :, ck], ALU.mult, Btnv[s] + [Bsg], [Bys[gs]])
                    if ci == gn - 1:
                        dma("sp", yT_dram.rearrange("(h vc p) t -> p h vc t", p=128, vc=4)[:, hd, :, g0 * 128:(g0 + gn) * 128],
                            ystg[gs][:, :, 0:gn * 128], [Bys[gs]], [B_yd])
                        gstate[0] += 1

                for it in range(NCH + 2):
                    if it < NCH:
                        stA(it)
                    if 0 <= it - 1 < NCH:
                        stB(it - 1)
                    if 0 <= it - 2 < NCH:
                        stC(it - 2)
            P.barrier()
            A.release(m)

        def stage_outproj(layer, x_sb, Bx, w_out_ap, kco, tiles):
            m = A.mark()
            wo = A.alloc([kco, 1024], BF16); Bwo = Buf("wo")
            yt = [A.alloc([kco, 512], BF16) for _ in range(2)]; Byt = [Buf(), Buf()]
            dma("pool", wo, w_out_ap.rearrange("(kc p) n -> p kc n", p=128), [], [Bwo])
            cnt = 0
            for ti, (t0, n, j) in enumerate(tiles):
                s = ti % 2
                dma("sp", yt[s][:, :, 0:n], yT_dram.rearrange("(kc p) t -> p kc t", p=128)[:, 0:kco, t0:t0 + n], [B_yd], [Byt[s]])
                for dcn in range(8):
                    pb = cnt % 4
                    cnt += 1
                    for kc in range(kco):
                        mm(bank[pb][:, 0:n], wo[:, kc, dcn * 128:(dcn + 1) * 128], yt[s][:, kc, 0:n], kc == 0, kc == kco - 1, [Bwo, Byt[s]], [PB[pb]])
                    stt(x_sb[:, dcn, t0:t0 + n], bank[pb][:, 0:n], modt[:, layer, 16 + dcn, j:j + 1], x_sb[:, dcn, t0:t0 + n], ALU.mult, ALU.add, [PB[pb], B_mod, Bx[TG[t0]][dcn]], [Bx[TG[t0]][dcn]])
            P.barrier()
            A.release(m)

        def ffn_expert(layer, x_sb, Bx, hT, Bh, wg_ap, wu_ap, wd_ap, F, tiles, slots, Ge=None, BGe=None):
            (wgs, wus, wds, Bws, sgt, Bsgt, actT, Bact, tmp2, Btmp2, cnts) = slots
            NW = len(wgs)
            w_g = wg_ap.rearrange("(kc p) n -> p kc n", p=128)
            w_u = wu_ap.rearrange("(kc p) n -> p kc n", p=128)
            w_d = wd_ap.rearrange("(fc p) n -> p fc n", p=128)
            for grp in range(F // 256):
                ws = cnts[0] % NW
                cnts[0] += 1
                dma("pool", wgs[ws], w_g[:, :, grp * 256:(grp + 1) * 256], [], [Bws[ws]])
                dma("pool", wus[ws], w_u[:, :, grp * 256:(grp + 1) * 256], [], [Bws[ws]])
                dma("pool", wds[ws], w_d[:, grp * 2:(grp + 1) * 2, :], [], [Bws[ws]])
                for ti, (t0, n, j) in enumerate(tiles):
                    a_s = cnts[1] % 2
                    cnts[1] += 1
                    for fc in range(2):
                        pg = 0 + fc
                        pu = 2 + fc
                        for kc in range(KC):
                            mm(bank[pg][:, 0:n], wgs[ws][:, kc, fc * 128:(fc + 1) * 128], hT[:, kc, t0:t0 + n], kc == 0, kc == KC - 1, [Bws[ws], Bh[ti]], [PB[pg]])
                        for kc in range(KC):
                            mm(bank[pu][:, 0:n], wus[ws][:, kc, fc * 128:(fc + 1) * 128], hT[:, kc, t0:t0 + n], kc == 0, kc == KC - 1, [Bws[ws], Bh[ti]], [PB[pu]])
                        act(sgt[fc][:, 0:n], bank[pg][:, 0:n], AF.Silu, [PB[pg]], [Bsgt[fc]])
                        if Ge is None:
                            tt("dve", actT[a_s][:, fc, 0:n], bank[pu][:, 0:n], sgt[fc][:, 0:n], ALU.mult, [PB[pu], Bsgt[fc]], [Bact[a_s][fc]])
                        else:
                            tt("dve", tmp2[fc][:, 0:n], bank[pu][:, 0:n], sgt[fc][:, 0:n], ALU.mult, [PB[pu], Bsgt[fc]], [Btmp2[fc]])
                            tt("dve", actT[a_s][:, fc, 0:n], tmp2[fc][:, 0:n], Ge[:, t0:t0 + n], ALU.mult, [Btmp2[fc], BGe], [Bact[a_s][fc]])
                    if cnts[3] is not None:
                        cnts[3]()

                    def down(ws=ws, a_s=a_s, t0=t0, n=n, j=j):
                        for dcn in range(8):
                            pb = 4 + (cnts[2] % 4)
                            cnts[2] += 1
                            for fc in range(2):
                                mm(bank[pb][:, 0:n], wds[ws][:, fc, dcn * 128:(dcn + 1) * 128], actT[a_s][:, fc, 0:n], fc == 0, fc == 1, [Bws[ws], Bact[a_s][fc]], [PB[pb]])
                            bx = Bx[TG[t0]][dcn]
                            stt(x_sb[:, dcn, t0:t0 + n], bank[pb][:, 0:n], modt[:, layer, 40 + dcn, j:j + 1], x_sb[:, dcn, t0:t0 + n], ALU.mult, ALU.add, [PB[pb], B_mod, bx], [bx])
                    cnts[3] = down

        def ffn_flush(slots):
            cnts = slots[-1]
            if cnts[3] is not None:
                cnts[3]()
                cnts[3] = None

        def alloc_ffn_slots(moe):
            wgs = [A.alloc([8, 256], BF16) for _ in range(3)]
            wus = [A.alloc([8, 256], BF16) for _ in range(3)]
            wds = [A.alloc([2, 1024], BF16) for _ in range(3)]
            Bws = [Buf(), Buf(), Buf()]
            sgt = [A.alloc([512], BF16) for _ in range(2)]; Bsgt = [Buf(), Buf()]
            actT = [A.alloc([2, 512], BF16) for _ in range(2)]; Bact = [[Buf(), Buf()], [Buf(), Buf()]]
            tmp2 = [A.alloc([512], F32) for _ in range(2)] if moe else None
            Btmp2 = [Buf(), Buf()]
            return (wgs, wus, wds, Bws, sgt, Bsgt, actT, Bact, tmp2, Btmp2, [0, 0, 0, None])

        def stage_gla(layer, hT, Bh, last):
            jl = layer // 2
            m = A.mark()
            w_in = gla_w_in[jl].rearrange("(kc p) n -> p kc n", p=128)
            bg = A.alloc([2, 4], F32); nbg = A.alloc([2, 4], F32); ng = A.alloc([8], F32)
            Bc = Buf("glac")
            dma("sp", bg, gla_bg[:, jl], [], [Bc])
            dma("sp", ng, gla_ng[:, jl, :], [], [Bc])
            ts("dve", nbg, bg, -1.0, None, ALU.mult, None, [Bc], [Bc])
            wa = A.alloc([8, 32], BF16); Bwa = Buf("wa")
            wup = A.alloc([2, 512], BF16)
            aT = A.alloc([2, T], BF16); BaT = Buf("aT")
            dma("pool", wa, w_in[:, :, 3072:3104], [], [Bwa])
            dma("pool", wup[0:16], gla_wgu[jl].rearrange("d r f -> r d f"), [], [Bwa])
            cnt = 0
            for ti, (t0, n, j) in enumerate(TILES):
                for dr in range(2):
                    pb = cnt % 2
                    cnt += 1
                    for kc in range(KC):
                        mm(bank[pb][0:16, 0:n], wa[:, kc, dr * 16:(dr + 1) * 16], hT[:, kc, t0:t0 + n], kc == 0, kc == KC - 1, [Bwa, Bh[ti]], [PB[pb]])
                    cp("act", aT[0:16, dr, t0:t0 + n], bank[pb][0:16, 0:n], [PB[pb]], [BaT])
            wq = A.alloc([8, 128], BF16); wk = A.alloc([8, 128], BF16)
            wv = A.alloc([8, 256], BF16); wr_ = A.alloc([8, 256], BF16)
            Bwq, Bwk, Bwv, Bwr = Buf(), Buf(), Buf(), Buf()
            qs = A.alloc([T], F32); ks = A.alloc([T], F32); Bqs, Bks = Buf(), Buf()
            la = A.alloc([T], F32); Bla = Buf()
            Pp = A.alloc([T + 1], F32); BPp = Buf()
            fq = A.alloc([T], F32); fk = A.alloc([T], F32); fs = A.alloc([T], F32); Bfq, Bfk, Bfs = Buf(), Buf(), Buf()
            ex = [A.alloc([512], F32) for _ in range(2)]; Bex = [Buf(), Buf()]
            qt = [A.alloc([T], BF16) for _ in range(2)]; kt = [A.alloc([T], BF16) for _ in range(2)]
            kp = A.alloc([T], BF16)
            Bqt, Bkt, Bkp = [Buf(), Buf()], [Buf(), Buf()], Buf()
            kptm = [A.alloc([NCH, 128], BF16) for _ in range(2)]; Bkptm = [Buf(), Buf()]
            ed = [A.alloc([NCH], F32) for _ in range(2)]; Bed = [Buf(), Buf()]
            v = A.alloc([NCH, 256], BF16); Bv = Buf()
            sr = A.alloc([2, T], BF16); Bsr = Buf()
            Sf = A.alloc([256], F32); Sb = A.alloc([256], F32); BSf, BSb = Buf(), Buf()
            Sfb = [A.alloc([256], BF16) for _ in range(2)]; BSfb = [Buf(), Buf()]
            Sbb = [A.alloc([256], BF16) for _ in range(2)]; BSbb = [Buf(), Buf()]
            Sbin = [A.alloc([256], BF16) for _ in range(2)]; BSbin = [Buf(), Buf()]
            PTf = [A.alloc([128], BF16) for _ in range(2)]; PTb = [A.alloc([128], BF16) for _ in range(2)]; BPT = [Buf(), Buf()]
            junk = A.alloc([256], F32); Bjunk = Buf()
            stat = [A.alloc([4], F32) for _ in range(2)]; Bstat = [Buf(), Buf()]
            onb = [A.alloc([256], BF16) for _ in range(2)]; Bonb = [Buf(), Buf()]
            tmpn = [A.alloc([2, 128], F32) for _ in range(2)]; Btn = [Buf(), Buf()]
            ystg = [A.alloc([2, 512], BF16) for _ in range(2)]; Bys = [Buf(), Buf()]
            psT = [psum[:, 4 + i, :].bitcast(BF16) for i in range(2)]
            P.op("dve", lambda e: e.memset(Pp[:, 0:1], 0.0), [], [BPp])
            QB = float(np.log(128.0 ** -0.5))
            pv = lambda ap: ap.rearrange("p (c i) -> p c i", i=128)
            gstate = [0]
            def gla_load(hd):
                dma("pool", wq, w_in[:, :, hd * 128:(hd + 1) * 128], [], [Bwq])
                dma("pool", wk, w_in[:, :, 1536 + hd * 128:1536 + (hd + 1) * 128], [], [Bwk])
                dma("pool", wv, w_in[:, :, 2048 + hd * 256:2048 + (hd + 1) * 256], [], [Bwv])
                dma("pool", wr_, w_in[:, :, 512 + hd * 256:512 + (hd + 1) * 256], [], [Bwr])

            def gla_gates(dr, hd):
                for ti, (t0, n, j) in enumerate(TILES):
                    s = ti % 2
                    pb = 2 + s
                    mm(bank[pb][:, 0:n], wup[0:16, dr, hd * 128:(hd + 1) * 128], aT[0:16, dr, t0:t0 + n], True, True, [Bwa, BaT], [PB[pb]])
                    act(ex[s][:, 0:n], bank[pb][:, 0:n], AF.Exp, [PB[pb], Bc], [Bex[s]], scale=-1.0, bias=nbg[:, dr, hd:hd + 1])
                    act(la[:, t0:t0 + n], ex[s][:, 0:n], AF.Ln, [Bex[s]], [Bla], bias=1.0)
                P.op("dve", lambda e: e.tensor_tensor_scan(out=Pp[:, 1:T + 1], data0=la, data1=la, initial=0.0, op0=ALU.add, op1=ALU.add), [Bla], [BPp])
                c0v = pv(Pp[:, 0:T])[:, :, 0:1].to_broadcast([128, NCH, 128])
                cev = pv(Pp[:, 1:T + 1])[:, :, 127:128].to_broadcast([128, NCH, 128])
                if dr == 0:
                    tt("dve", pv(fq), pv(Pp[:, 1:T + 1]), c0v, ALU.subtract, [BPp], [Bfq])
                    tt("dve", pv(fs), cev, pv(Pp[:, 1:T + 1]), ALU.subtract, [BPp], [Bfs])
                else:
                    tt("dve", pv(fq), cev, pv(Pp[:, 0:T]), ALU.subtract, [BPp], [Bfq])
                    tt("dve", pv(fs), pv(Pp[:, 0:T]), c0v, ALU.subtract, [BPp], [Bfs])
                tt("dve", ed[dr], pv(Pp[:, 1:T + 1])[:, :, 127], pv(Pp[:, 0:T])[:, :, 0], ALU.subtract, [BPp], [Bed[dr]])
                act(ed[dr], ed[dr], AF.Exp, [Bed[dr]], [Bed[dr]], scale=-1.0 / 32)
                act(fk, fq, AF.Exp, [Bfq], [Bfk], scale=1.0 / 32)
                act(fq, fq, AF.Exp, [Bfq], [Bfq], scale=-1.0 / 32, bias=QB)
                act(fs, fs, AF.Exp, [Bfs], [Bfs], scale=-1.0 / 32)

            def gla_products(dr):
                tt("pool", qt[dr], qs, fq, ALU.mult, [Bqs, Bfq], [Bqt[dr]])
                tt("dve", kt[dr], ks, fk, ALU.mult, [Bks, Bfk], [Bkt[dr]])
                tt("pool", kp, ks, fs, ALU.mult, [Bks, Bfs], [Bkp])
                for c in range(NCH):
                    s = c % 2
                    tr(psT[s][:, 0:128], kp[:, c * 128:(c + 1) * 128], identb, [Bkp, B_small], [PB[4 + s]])
                    cp("act", kptm[dr][:, c, :], psT[s][:, 0:128], [PB[4 + s]], [Bkptm[dr]])

            gla_load(0)
            for hd in range(4):
                gla_gates(0, hd)
                for ti, (t0, n, j) in enumerate(TILES):
                    for (w, Bw, dst, Bdst, pb) in ((wq, Bwq, qs, Bqs, 0), (wk, Bwk, ks, Bks, 1)):
                        for kc in range(KC):
                            mm(bank[pb][:, 0:n], w[:, kc, :], hT[:, kc, t0:t0 + n], kc == 0, kc == KC - 1, [Bw, Bh[ti]], [PB[pb]])
                        cp("act", dst[:, t0:t0 + n], bank[pb][:, 0:n], [PB[pb]], [Bdst])
                    for vc in range(2):
                        pb = 2 + vc
                        for kc in range(KC):
                            mm(bank[pb][:, 0:n], wr_[:, kc, vc * 128:(vc + 1) * 128], hT[:, kc, t0:t0 + n], kc == 0, kc == KC - 1, [Bwr, Bh[ti]], [PB[pb]])
                        act(sr[:, vc, t0:t0 + n], bank[pb][:, 0:n], AF.Silu, [PB[pb]], [Bsr])
                    for sub in range(n // 128):
                        c = t0 // 128 + sub
                        pb = 6 + (sub % 2)
                        for kc in range(KC):
                            mm(bank[pb][:, 0:256], hT[:, kc, c * 128:(c + 1) * 128], wv[:, kc, :], kc == 0, kc == KC - 1, [Bwv, Bh[ti]], [PB[pb]])
                        cp("dve", v[:, c, :], bank[pb][:, 0:256], [PB[pb]], [Bv])
                if hd < 3:
                    gla_load(hd + 1)
                gla_products(0)
                gla_gates(1, hd)
                gla_products(1)
                P.op("dve", lambda e: e.memset(Sb, 0.0), [], [BSb])
                orderB = [1, 0] + list(range(NCH - 1, 1, -1))
                for idx, c in enumerate(orderB):
                    s = idx % 2
                    cp("act", Sbb[s], Sb, [BSb], [BSbb[s]])
                    dma("sp", sb_dram[c][:, 0:256], Sbb[s], [BSbb[s]], [B_sbd[c]])
                    mm(bank[0][:, 0:256], kptm[1][:, c, :], v[:, c, :], True, True, [Bkptm[1], Bv], [PB[0]])
                    stt(Sb, Sb, ed[1][:, c:c + 1], bank[0][:, 0:256], ALU.mult, ALU.add, [BSb, PB[0], Bed[1]], [BSb])
                P.op("dve", lambda e: e.memset(Sf, 0.0), [], [BSf])
                P.op("pool", lambda e: e.memset(Sfb[0], 0.0), [], [BSfb[0]])
                OB = (3, 1)

                def gA(c, hd=hd):
                    s = c % 2
                    ck = slice(c * 128, (c + 1) * 128)
                    ob = OB[s]
                    need_out = not (last and c < 2)
                    mm(bank[0][:, 0:256], kptm[0][:, c, :], v[:, c, :], True, True, [Bkptm[0], Bv], [PB[0]])
                    if need_out:
                        dma("sp", Sbin[s], sb_dram[c][:, 0:256], [B_sbd[c]], [BSbin[s]])
                        mm(bank[2][:, 0:128], kt[0][:, ck], qt[0][:, ck], True, True, [Bkt[0], Bqt[0]], [PB[2]])
                        mm(bank[2][:, 128:256], kt[1][:, ck], qt[1][:, ck], True, True, [Bkt[1], Bqt[1]], [PB[2]])
                        tt("dve", PTf[s], bank[2][:, 0:128], cst[:, C_LO:C_LO + 128], ALU.mult, [PB[2], B_cst], [BPT[s]])
                        tt("dve", PTb[s], bank[2][:, 128:256], cst[:, C_UP:C_UP + 128], ALU.mult, [PB[2], B_cst], [BPT[s]])
                        mm(bank[ob][:, 0:256], PTf[s], v[:, c, :], True, False, [BPT[s], Bv], [PB[ob]])
                        mm(bank[ob][:, 0:256], PTb[s], v[:, c, :], False, False, [BPT[s], Bv], [PB[ob]])
                        mm(bank[ob][:, 0:256], qt[0][:, ck], Sfb[s], False, False, [Bqt[0], BSfb[s]], [PB[ob]])
                        mm(bank[ob][:, 0:256], qt[1][:, ck], Sbin[s], False, True, [Bqt[1], BSbin[s]], [PB[ob]])
                    stt(Sf, Sf, ed[0][:, c:c + 1], bank[0][:, 0:256], ALU.mult, ALU.add, [BSf, PB[0], Bed[0]], [BSf])
                    cp("dve", Sfb[1 - s], Sf, [BSf], [BSfb[1 - s]])
                    if need_out:
                        act(junk, bank[ob][:, 0:256], AF.Square, [PB[ob]], [Bjunk, Bstat[s]], accum_out=stat[s][:, 0:1])

                def gB(c, hd=hd):
                    s = c % 2
                    ob = OB[s]
                    if last and c < 2:
                        return
                    sv = stat[s]
                    ts("dve", sv[:, 1:2], sv[:, 0:1], 1.0 / 256, EPS, ALU.mult, ALU.add, [Bstat[s]], [Bstat[s]])
                    act(sv[:, 1:2], sv[:, 1:2], AF.Sqrt, [Bstat[s]], [Bstat[s]])
                    P.op("dve", lambda e, sv=sv: e.reciprocal(out=sv[:, 2:3], in_=sv[:, 1:2]), [Bstat[s]], [Bstat[s]])
                    act(onb[s], bank[ob][:, 0:256], AF.Copy, [PB[ob], Bstat[s]], [Bonb[s]], scale=sv[:, 2:3])
                    for vc in range(2):
                        tr(psT[s][:, vc * 128:(vc + 1) * 128], onb[s][:, vc * 128:(vc + 1) * 128], identb, [Bonb[s], B_small], [PB[4 + s]])

                def gC(c, hd=hd):
                    s = c % 2
                    ck = slice(c * 128, (c + 1) * 128)
                    if last and c < 2:
                        return
                    for vc in range(2):
                        ts("dve", tmpn[s][:, vc, :], psT[s][:, vc * 128:(vc + 1) * 128], ng[:, hd * 2 + vc:hd * 2 + vc + 1], None, ALU.mult, None, [PB[4 + s], Bc], [Btn[s]])
                    if c < 2:
                        g0, gn = 0, 2
                    else:
                        g0, gn = 2 + ((c - 2) // 4) * 4, 4
                    gs = gstate[0] % 2
                    ci = c - g0
                    tt("pool", ystg[gs][:, :, ci * 128:(ci + 1) * 128], tmpn[s], sr[:, :, ck], ALU.mult, [Btn[s], Bsr], [Bys[gs]])
                    if ci == gn - 1:
                        dma("sp", yT_dram[0:1024].rearrange("(h vc p) t -> p h vc t", p=128, vc=2)[:, hd, :, g0 * 128:(g0 + gn) * 128],
                            ystg[gs][:, :, 0:gn * 128], [Bys[gs]], [B_yd])
                        gstate[0] += 1

                for it in range(NCH + 2):
                    if it < NCH:
                        gA(it)
                    if 0 <= it - 1 < NCH:
                        gB(it - 1)
                    if 0 <= it - 2 < NCH:
                        gC(it - 2)
            P.barrier()
            A.release(m)

        def stage_moe(layer, x_sb, Bx, hT, Bh, tiles):
            jl = layer // 2
            m = A.mark()
            wr = A.alloc([8, 8], F32); Bwr = Buf("wr")
            dma("sp", wr, moe_wr[jl].rearrange("(kc p) e -> p kc e", p=128), [], [Bwr])

            def hf_cb(ti, t0, n, hf, Bhf):
                for sub in range(n // 128):
                    c = t0 // 128 + sub
                    for kc in range(KC):
                        mm(bank[5][:, c * 8:(c + 1) * 8], hf[:, kc, sub * 128:(sub + 1) * 128], wr[:, kc, :], kc == 0, kc == KC - 1, [Bhf, Bwr], [PB[5]])

            stage_norm(layer, gm2, 24, hT, Bh, x_sb=x_sb, Bx=Bx, tiles=tiles, hf_cb=hf_cb)
            c_lo = tiles[0][0] // 128
            ncu = NCH - c_lo
            L = A.alloc([NCH, 8], F32); L2 = A.alloc([NCH, 8], F32); eq1 = A.alloc([NCH, 8], F32); eq2 = A.alloc([NCH, 8], F32)
            gates = A.alloc([NCH, 8], F32)
            m1 = A.alloc([NCH], F32); m2 = A.alloc([NCH], F32); w1 = A.alloc([NCH], F32); w2 = A.alloc([NCH], F32)
            Bg = Buf("gate")
            sl = slice(c_lo, NCH)
            cp("dve", L[:, sl], bank[5][:, c_lo * 8:NCH * 8].rearrange("p (c e) -> p c e", e=8), [PB[5]], [Bg])
            P.op("dve", lambda e: e.tensor_reduce(out=m1[:, sl], in_=L[:, sl], axis=AX.X, op=ALU.max), [Bg], [Bg])
            tt("dve", eq1[:, sl], L[:, sl], m1[:, sl].unsqueeze(2).to_broadcast([128, ncu, 8]), ALU.is_equal, [Bg], [Bg])
            stt(L2[:, sl], eq1[:, sl], -1e30, L[:, sl], ALU.mult, ALU.add, [Bg], [Bg])
            P.op("dve", lambda e: e.tensor_reduce(out=m2[:, sl], in_=L2[:, sl], axis=AX.X, op=ALU.max), [Bg], [Bg])
            tt("dve", eq2[:, sl], L2[:, sl], m2[:, sl].unsqueeze(2).to_broadcast([128, ncu, 8]), ALU.is_equal, [Bg], [Bg])
            tt("dve", w2[:, sl], m2[:, sl], m1[:, sl], ALU.subtract, [Bg], [Bg])
            act(w2[:, sl], w2[:, sl], AF.Exp, [Bg], [Bg])
            ts("dve", w1[:, sl], w2[:, sl], 1.0, None, ALU.add, None, [Bg], [Bg])
            P.op("dve", lambda e: e.reciprocal(out=w1[:, sl], in_=w1[:, sl]), [Bg], [Bg])
            tt("dve", w2[:, sl], w2[:, sl], w1[:, sl], ALU.mult, [Bg], [Bg])
            tt("dve", gates[:, sl], eq1[:, sl], w1[:, sl].unsqueeze(2).to_broadcast([128, ncu, 8]), ALU.mult, [Bg], [Bg])
            tt("dve", eq2[:, sl], eq2[:, sl], w2[:, sl].unsqueeze(2).to_broadcast([128, ncu, 8]), ALU.mult, [Bg], [Bg])
            tt("dve", gates[:, sl], gates[:, sl], eq2[:, sl], ALU.add, [Bg], [Bg])
            Ge = [A.alloc([T], F32) for _ in range(2)]; BGe = [Buf(), Buf()]
            diag = [A.alloc([128], F32) for _ in range(2)]; Bdg = [Buf(), Buf()]
            slots = alloc_ffn_slots(True)
            dcnt = 0
            for e_ in range(NE):
                gs = e_ % 2
                for (t0, n, j) in tiles:
                    for sub in range(n // 128):
                        c = t0 // 128 + sub
                        ds_ = dcnt % 2
                        dcnt += 1
                        ts("dve", diag[ds_], cst[:, C_ID:C_ID + 128], gates[:, c, e_:e_ + 1], None, ALU.mult, None, [Bg, B_cst], [Bdg[ds_]])
                        mm(bank[4][:, sub * 128:(sub + 1) * 128], onesf, diag[ds_], True, True, [Bdg[ds_], B_small], [PB[4]])
                    cp("act", Ge[gs][:, t0:t0 + n], bank[4][:, 0:n], [PB[4]], [BGe[gs]])
                ffn_expert(layer, x_sb, Bx, hT, Bh, moe_wg[jl, e_], moe_wu[jl, e_], moe_wd[jl, e_], D_FFE, tiles, slots, Ge=Ge[gs], BGe=BGe[gs])
            ffn_flush(slots)
            P.barrier()
            A.release(m)

        def stage_final(x_sb, Bx):
            m = A.mark()
            sq = [A.alloc([8, 512], BF16) for _ in range(2)]; Bsq = [Buf(), Buf()]
            rs = [A.alloc([512], F32) for _ in range(2)]; Brs = [Buf(), Buf()]
            tmp = [A.alloc([8, 512], F32) for _ in range(2)]; Btmp = [Buf(), Buf()]
            for ti, (t0, n, j) in enumerate(TILES[1:]):
                s = ti % 2
                xt = x_sb[:, :, t0:t0 + n]
                act(sq[s], xt, AF.Square, Bx[TG[t0]], [Bsq[s]])
                pb = 6 + s
                for c in range(KC):
                    mm(bank[pb], onesb, sq[s][:, c, :], c == 0, c == KC - 1, [Bsq[s], B_small], [PB[pb]])
                ts("dve", rs[s], bank[pb], 1.0 / D, EPS, ALU.mult, ALU.add, [PB[pb]], [Brs[s]])
                act(rs[s], rs[s], AF.Sqrt, [Brs[s]], [Brs[s]])
                P.op("dve", lambda e, s=s: e.reciprocal(out=rs[s], in_=rs[s]), [Brs[s]], [Brs[s]])
                tt("dve", tmp[s], xt, rs[s].unsqueeze(1).to_broadcast([128, 8, 512]), ALU.mult, Bx[TG[t0]] + [Brs[s]], [Btmp[s]])
                tt("pool", tmp[s], tmp[s], fgt.unsqueeze(2).to_broadcast([128, 8, 512]), ALU.mult, [Btmp[s], B_small], [Btmp[s]])
                dma("sp", outT.rearrange("(c p) t -> p c t", p=128)[:, :, t0 - NCTX:t0 - NCTX + n], tmp[s], [Btmp[s]], [B_out])
            P.barrier()
            A.release(m)

        stage_adaln()
        for layer in range(n_layers):
            last = layer == 3
            jl = layer // 2
            is_ret = layer % 2 == 0
            xsrc = xT if layer == 0 else x_dram
            Bxs = Buf("xT") if layer == 0 else B_xd
            m0 = A.mark()
            hT = A.alloc([8, T], BF16)
            Bh = [Buf("h%d" % i) for i in range(5)]
            stage_norm(layer, gm1, 0, hT, Bh, xsrc=xsrc, Bxs=Bxs)
            if is_ret:
                stage_retention(layer, hT, Bh)
                kco, w_out_ap = 16, ret_w_out[jl]
            else:
                stage_gla(layer, hT, Bh, last)
                kco, w_out_ap = 8, gla_w_out[jl]
            A.release(m0)
            tiles = TILES[1:] if last else TILES
            x_sb = A.alloc([8, T], F32); Bx = [[Buf("x%d_%d" % (a, b_)) for b_ in range(8)] for a in range(5)]
            for tg, (t0, n, j) in enumerate(TILES):
                dma("sp", x_sb[:, :, t0:t0 + n], xsrc.rearrange("(c p) t -> p c t", p=128)[:, :, t0:t0 + n], [Bxs], Bx[tg])
            stage_outproj(layer, x_sb, Bx, w_out_ap, kco, tiles)
            hT = A.alloc([8, T], BF16)
            Bh = [Buf("h%d" % i) for i in range(5)]
            if is_ret:
                stage_norm(layer, gm2, 24, hT, Bh[5 - len(tiles):], x_sb=x_sb, Bx=Bx, tiles=tiles)
                m1 = A.mark()
                slots = alloc_ffn_slots(False)
                ffn_expert(layer, x_sb, Bx, hT[:, :, :] if not last else hT, Bh[5 - len(tiles):], ffn_wg[jl], ffn_wu[jl], ffn_wd[jl], D_FF, tiles, slots)
                ffn_flush(slots)
                P.barrier()
                A.release(m1)
            else:
                stage_moe(layer, x_sb, Bx, hT, Bh[5 - len(tiles):], tiles)
            if layer == n_layers - 1:
                if layer == 3:
                    stage_final(x_sb, Bx)
                else:
                    for (t0, n, j) in TILES[1:]:
                        dma("sp", outT.rearrange("(c p) t -> p c t", p=128)[:, :, t0 - NCTX:t0 - NCTX + n], x_sb[:, :, t0:t0 + n], Bx[TG[t0]], [B_out])
                    if "ctx" in tap_out:
                        dma("sp", tap_out["ctx"].rearrange("(c p) t -> p c t", p=128), x_sb[:, :, 0:NCTX], Bx[0], [B_out])
            else:
                for (t0, n, j) in TILES:
                    dma("sp", x_dram.rearrange("(c p) t -> p c t", p=128)[:, :, t0:t0 + n], x_sb[:, :, t0:t0 + n], Bx[TG[t0]], [B_xd])
            P.barrier()
            A.release(m0)
        P.barrier()
        P.emit()
    return nc


def _fm(vec, nch):
    return np.ascontiguousarray(np.asarray(vec, np.float32).reshape(nch, 128).T)


def prep_shared(inp):
    sh = {}
    sh["cst"] = host_consts()
    sh["ada_w"] = np.ascontiguousarray(inp["ada_w"], dtype=np.float32)
    sh["ada_b_fm"] = np.ascontiguousarray(np.stack([_fm(inp["ada_b"][i], 48) for i in range(4)], axis=1))
    sh["ngm_fm"] = np.ascontiguousarray(np.stack([_fm(inp["norm_mix_g"][i], 8) for i in range(4)], axis=1))
    sh["ngf_fm"] = np.ascontiguousarray(np.stack([_fm(inp["norm_ffn_g"][i], 8) for i in range(4)], axis=1))
    sh["fg_fm"] = _fm(inp["final_g"], 8)
    sh["ret_w_in"] = np.ascontiguousarray(inp["ret_w_in"], dtype=np.float32)
    sh["ret_ld"] = np.ascontiguousarray(np.asarray(inp["ret_log_decay"], np.float32).reshape(2, 8))
    sh["ret_gnw_fm"] = np.ascontiguousarray(np.stack([_fm(inp["ret_gn_w"][i], 16) for i in range(2)], axis=1))
    sh["ret_gnb_fm"] = np.ascontiguousarray(np.stack([_fm(inp["ret_gn_b"][i], 16) for i in range(2)], axis=1))
    sh["ret_w_out"] = np.ascontiguousarray(inp["ret_w_out"], dtype=np.float32)
    sh["gla_w_in"] = np.ascontiguousarray(inp["gla_w_in"], dtype=np.float32)
    sh["gla_w_gate_up"] = np.ascontiguousarray(inp["gla_w_gate_up"], dtype=np.float32)
    bg = np.asarray(inp["gla_b_gate"], np.float32)
    sh["gla_bg_fm"] = np.ascontiguousarray(bg.reshape(2, 2, 4, 128).transpose(3, 0, 1, 2))
    sh["gla_ng_fm"] = np.ascontiguousarray(np.stack([_fm(inp["gla_norm_g"][i], 8) for i in range(2)], axis=1))
    sh["gla_w_out"] = np.ascontiguousarray(inp["gla_w_out"], dtype=np.float32)
    for k in ("ffn_w_gate", "ffn_w_up", "ffn_w_down", "moe_w_router", "moe_w_gate", "moe_w_up", "moe_w_down"):
        sh[k] = np.ascontiguousarray(inp[k], dtype=np.float32)
    return sh


def prep_core(inp, b, shared):
    m = dict(shared)
    m["xT"] = np.ascontiguousarray(np.concatenate([inp["ctx"][b], inp["x"][b]], axis=0).T.astype(np.float32))
    m["cc"] = np.ascontiguousarray(np.stack([_fm(inp["c"][b], 8), _fm(inp["c_ctx"], 8)], axis=1))
    return m


_NC_CACHE = {}


def kernel(**inputs):
    inp = {k: np.asarray(v) for k, v in inputs.items()}
    if "full" not in _NC_CACHE:
        _NC_CACHE["full"] = build(4)
    nc = _NC_CACHE["full"]
    shared = prep_shared(inp)
    in_maps = [prep_core(inp, b, shared) for b in range(8)]
    res = run_bass_kernel_spmd(nc, in_maps, core_ids=list(range(8)))
    out = np.stack([np.ascontiguousarray(res.results[b]["outT"].T) for b in range(8)], axis=0)
    return out.astype(np.float32)
```

```python
import contextlib
import numpy as np
import concourse.bass as bass
import concourse.mybir as mybir
from concourse.bass_utils import run_bass_kernel_spmd

F32 = mybir.dt.float32
BF16 = mybir.dt.bfloat16
AF = mybir.ActivationFunctionType
ALU = mybir.AluOpType
AX = mybir.AxisListType

ENGS = ("pe", "act", "dve", "pool", "sp")
NSLOT = 8
QSLOT = {"pool": 5, "sp": 8, "act": 8, "pe": 8, "dve": 8}
SAME_ENGINE_SYNC = True
import os as _os
OPT = set(_os.environ.get("KOPT", "war,prefetch").split(","))

D = 1024
KC = 8
NCTX = 256
NLAT = 2048
T = NCTX + NLAT
NCH = T // 128
EPS = 1e-6
TILES = [(0, 256, 1)] + [(256 + 512 * i, 512, 0) for i in range(4)]
TG = {t[0]: i for i, t in enumerate(TILES)}
D_FF = 2816
D_FFE = 3584
NE = 8


class Buf:
    __slots__ = ("name", "w", "r", "rd")

    def __init__(self, name=""):
        self.name = name
        self.w = None
        self.r = {}
        self.rd = []


class Ins:
    __slots__ = ("eng", "fn", "deps", "sig", "sem", "semval", "dma")

    def __init__(self, eng, fn, dma):
        self.eng = eng
        self.fn = fn
        self.deps = []
        self.sig = False
        self.sem = None
        self.semval = 0
        self.dma = dma


class Prog:
    def __init__(self, nc):
        self.nc = nc
        self.q = {e: [] for e in ENGS}
        self.dma_cnt = {e: 0 for e in ENGS}
        self.slot_last = {e: [None] * NSLOT for e in ENGS}
        self.slot_uses = {e: [0] * NSLOT for e in ENGS}

    def op(self, eng, fn, reads=(), writes=(), dma=False):
        ins = Ins(eng, fn, dma)
        deps = {}

        def add(d, war):
            if d is None:
                return
            if d.eng == eng and not d.dma and not dma:
                if eng in ("pe", "sp"):
                    return
                if not SAME_ENGINE_SYNC:
                    return
                if war and "war" not in OPT:
                    return
            deps[id(d)] = d

        for b in reads:
            add(b.w, False)
        for b in writes:
            add(b.w, False)
            for d in b.r.values():
                add(d, True)
            for d in b.rd:
                add(d, True)
        if dma:
            k = self.dma_cnt[eng] % QSLOT[eng]
            self.dma_cnt[eng] += 1
            prev = self.slot_last[eng][k]
            if prev is not None:
                deps[id(prev)] = prev
            self.slot_last[eng][k] = ins
            self.slot_uses[eng][k] += 1
            ins.sem = ("dma", eng, k)
            ins.semval = 16 * self.slot_uses[eng][k]
            ins.sig = True
        ins.deps = list(deps.values())
        for d in ins.deps:
            d.sig = True
        for b in reads:
            if dma:
                b.rd.append(ins)
            else:
                b.r[eng] = ins
        for b in writes:
            b.w = ins
            b.r = {}
            b.rd = []
        self.q[eng].append(ins)
        return ins

    def barrier(self):
        lasts = []
        for e in ENGS:
            for ins in reversed(self.q[e]):
                if not ins.dma and ins.fn is not None:
                    lasts.append(ins)
                    break
            for k in range(NSLOT):
                if self.slot_last[e][k] is not None:
                    lasts.append(self.slot_last[e][k])
        for e in ENGS:
            ins = Ins(e, None, False)
            ins.deps = list(lasts)
            for d in ins.deps:
                d.sig = True
            self.q[e].append(ins)

    def emit(self):
        nc = self.nc
        with contextlib.ExitStack() as st:
            esem = {e: st.enter_context(nc.semaphore("s_" + e)) for e in ENGS}
            dsem = {}
            for e in ENGS:
                for k in range(NSLOT):
                    if self.slot_uses[e][k]:
                        dsem[("dma", e, k)] = st.enter_context(nc.semaphore("d_%s%d" % (e, k)))
            for e in ENGS:
                c = 0
                for ins in self.q[e]:
                    if ins.dma or ins.fn is None:
                        continue
                    if ins.sig:
                        c += 1
                        ins.sem = ("eng", e)
                        ins.semval = c

            def semof(key):
                return esem[key[1]] if key[0] == "eng" else dsem[key]

            block = st.enter_context(nc.Block())

            def run(ename, eng):
                waited = {}
                for ins in self.q[ename]:
                    for d in ins.deps:
                        if waited.get(d.sem, 0) >= d.semval:
                            continue
                        eng.wait_ge(semof(d.sem), d.semval)
                        waited[d.sem] = d.semval
                    if ins.fn is None:
                        continue
                    r = ins.fn(eng)
                    if ins.sig:
                        r.then_inc(semof(ins.sem), 16 if ins.dma else 1)

            @block.tensor
            def _(eng):
                run("pe", eng)

            @block.scalar
            def _(eng):
                run("act", eng)

            @block.vector
            def _(eng):
                run("dve", eng)

            @block.gpsimd
            def _(eng):
                run("pool", eng)

            @block.sync
            def _(eng):
                run("sp", eng)


class Arena:
    def __init__(self, big, nbytes):
        self.big = big
        self.nbytes = nbytes
        self.off = 0

    def alloc(self, shape, dt):
        size = 4 if dt == F32 else 2
        n = int(np.prod(shape))
        nb = (n * size + 63) // 64 * 64
        assert self.off + nb <= self.nbytes, ("SBUF arena overflow", self.off, nb, self.nbytes)
        ap = self.big[:, self.off // 2:(self.off + n * size) // 2]
        self.off += nb
        if dt == F32:
            ap = ap.bitcast(F32)
        if len(shape) == 2:
            ap = ap.rearrange("p (a b) -> p a b", a=shape[0])
        elif len(shape) == 3:
            ap = ap.rearrange("p (a b c) -> p a b c", a=shape[0], b=shape[1])
        elif len(shape) == 4:
            ap = ap.rearrange("p (a b c d) -> p a b c d", a=shape[0], b=shape[1], c=shape[2])
        return ap

    def mark(self):
        return self.off

    def release(self, m):
        self.off = m


C_ID, C_DPOS, C_DNEG, C_LO, C_UP, C_I1, C_IB, C_PF, C_PB, C_ROPE = 0, 128, 256, 384, 512, 640, 768, 896, 897, 898
C_ROPEK = 898 + 192
C_N = 898 + 384


def host_consts():
    c = np.zeros((128, C_N), np.float32)
    p = np.arange(128)[:, None].astype(np.float32)
    i = np.arange(128)[None, :].astype(np.float32)
    c[:, C_ID:C_ID + 128] = np.eye(128)
    c[:, C_DPOS:C_DPOS + 128] = np.maximum(i - p, 0)
    c[:, C_DNEG:C_DNEG + 128] = np.maximum(p - i, 0)
    c[:, C_LO:C_LO + 128] = (i >= p)
    c[:, C_UP:C_UP + 128] = (i <= p)
    c[:, C_I1:C_I1 + 128] = i + 1
    c[:, C_IB:C_IB + 128] = 128 - i
    c[:, C_PF] = 127 - p[:, 0]
    c[:, C_PB] = p[:, 0]
    half = 64
    freqs = (10000.0 ** (-np.arange(half, dtype=np.float32) / half)).astype(np.float32)
    fr = np.concatenate([freqs, freqs])[:, None]
    rows = np.arange(32, dtype=np.float32)[None, :]
    cols = np.arange(64, dtype=np.float32)[None, :]
    c[:, C_ROPE:C_ROPE + 32] = np.cos(rows * fr)
    sgn = np.where(np.arange(128) < 64, -1.0, 1.0).astype(np.float32)[:, None]
    c[:, C_ROPE + 32:C_ROPE + 64] = np.sin(rows * fr) * sgn
    c[:, C_ROPE + 64:C_ROPE + 128] = np.cos(cols * fr)
    c[:, C_ROPE + 128:C_ROPE + 192] = np.sin(cols * fr) * sgn
    c[:, C_ROPEK:C_ROPEK + 192] = c[:, C_ROPE:C_ROPE + 192] * np.float32(0.0625)
    return c


def build(n_layers=4, taps=()):
    nc = bass.Bass("TRN2", target_bir_lowering=False)
    P = Prog(nc)

    def din(name, shape, dt=F32):
        return nc.dram_tensor(name, list(shape), dt, kind="ExternalInput").ap()

    xT = din("xT", [D, T])
    cc = din("cc", [128, 2, 8])
    cst_d = din("cst", [128, C_N])
    ada_w = din("ada_w", [4, D, 6 * D])
    ada_b = din("ada_b_fm", [128, 4, 48])
    ngm_d = din("ngm_fm", [128, 4, 8])
    ngf_d = din("ngf_fm", [128, 4, 8])
    fg_d = din("fg_fm", [128, 8])
    ret_w_in = din("ret_w_in", [2, D, 6144])
    ret_ld = din("ret_ld", [2, 8])
    ret_gnw = din("ret_gnw_fm", [128, 2, 16])
    ret_gnb = din("ret_gnb_fm", [128, 2, 16])
    ret_w_out = din("ret_w_out", [2, 2048, D])
    gla_w_in = din("gla_w_in", [2, D, 3104])
    gla_wgu = din("gla_w_gate_up", [2, 2, 16, 512])
    gla_bg = din("gla_bg_fm", [128, 2, 2, 4])
    gla_ng = din("gla_ng_fm", [128, 2, 8])
    gla_w_out = din("gla_w_out", [2, D, D])
    ffn_wg = din("ffn_w_gate", [2, D, D_FF])
    ffn_wu = din("ffn_w_up", [2, D, D_FF])
    ffn_wd = din("ffn_w_down", [2, D_FF, D])
    moe_wr = din("moe_w_router", [2, D, NE])
    moe_wg = din("moe_w_gate", [2, NE, D, D_FFE])
    moe_wu = din("moe_w_up", [2, NE, D, D_FFE])
    moe_wd = din("moe_w_down", [2, NE, D_FFE, D])

    outT = nc.dram_tensor("outT", [D, NLAT], F32, kind="ExternalOutput").ap()
    x_dram = nc.dram_tensor("x_scr", [D, T], F32, kind="Internal").ap()
    yT_dram = nc.dram_tensor("yT_scr", [2048, T], BF16, kind="Internal").ap()
    sb_dram = nc.dram_tensor("sb_scr", [NCH, 128, 1024], BF16, kind="Internal").ap()
    tap_out = {}
    for (name, shape) in taps:
        tap_out[name] = nc.dram_tensor("tap_" + name, list(shape), F32, kind="ExternalOutput").ap()

    ARENA_BYTES = 207 * 1024
    with contextlib.ExitStack() as st:
        big = st.enter_context(nc.sbuf_tensor("big", [128, ARENA_BYTES // 2], BF16))
        psum = st.enter_context(nc.psum_tensor("psum", [128, 8, 512], F32))
        A = Arena(big, ARENA_BYTES)
        PB = [Buf("bank%d" % i) for i in range(8)]
        bank = [psum[:, i, :] for i in range(8)]
        B_xd = Buf("x_dram")
        B_yd = Buf("yT_dram")
        B_sbd = [Buf("sbd%d" % i) for i in range(NCH)]
        B_out = Buf("out")

        def dma(eng, out, in_, reads, writes):
            return P.op(eng, lambda e: e.dma_start(out=out, in_=in_), reads, writes, dma=True)

        def mm(out, lhsT, rhs, start, stop, reads, writes):
            return P.op("pe", lambda e: e.matmul(out, lhsT, rhs, start=start, stop=stop), reads, writes)

        def tr(out, in_, ident, reads, writes):
            return P.op("pe", lambda e: e.transpose(out, in_, ident), reads, writes)

        def act(out, in_, func, reads, writes, **kw):
            return P.op("act", lambda e: e.activation(out=out, in_=in_, func=func, **kw), reads, writes)

        def tt(eng, out, in0, in1, op, reads, writes):
            return P.op(eng, lambda e: e.tensor_tensor(out=out, in0=in0, in1=in1, op=op), reads, writes)

        def ts(eng, out, in0, s1, s2, op0, op1, reads, writes):
            if s2 is None:
                return P.op(eng, lambda e: e.tensor_scalar(out=out, in0=in0, scalar1=s1, scalar2=None, op0=op0), reads, writes)
            return P.op(eng, lambda e: e.tensor_scalar(out=out, in0=in0, scalar1=s1, scalar2=s2, op0=op0, op1=op1), reads, writes)

        def stt(out, in0, scalar, in1, op0, op1, reads, writes):
            return P.op("dve", lambda e: e.scalar_tensor_tensor(out=out, in0=in0, scalar=scalar, in1=in1, op0=op0, op1=op1), reads, writes)

        def cp(eng, out, in_, reads, writes):
            if eng == "act":
                return P.op("act", lambda e: e.copy(out=out, in_=in_), reads, writes)
            return P.op(eng, lambda e: e.tensor_copy(out=out, in_=in_), reads, writes)

        def tap(name, src, reads):
            if name in tap_out:
                dma("sp", tap_out[name], src, reads, [B_out])

        cst = A.alloc([C_N], F32); B_cst = Buf("cst")
        identb = A.alloc([128], BF16)
        onesb = A.alloc([128], BF16)
        onesf = A.alloc([128], F32)
        epsc = A.alloc([1], F32)
        modt = A.alloc([4, 48, 2], F32); B_mod = Buf("mod")
        gm1 = A.alloc([4, 8, 2], F32)
        gm2 = A.alloc([4, 8, 2], F32)
        adab = A.alloc([4, 48], F32)
        ngm = A.alloc([4, 8], F32)
        ngf = A.alloc([4, 8], F32)
        fgt = A.alloc([8], F32)
        B_small = Buf("small")
        dma("sp", cst, cst_d, [], [B_cst])
        dma("sp", adab, ada_b, [], [B_small])
        dma("sp", ngm, ngm_d, [], [B_small])
        dma("sp", ngf, ngf_d, [], [B_small])
        dma("sp", fgt, fg_d, [], [B_small])
        cp("dve", identb, cst[:, C_ID:C_ID + 128], [B_cst], [B_small])
        P.op("dve", lambda e: e.memset(onesb, 1.0), [], [B_small])
        P.op("dve", lambda e: e.memset(onesf, 1.0), [], [B_small])
        P.op("dve", lambda e: e.memset(epsc, EPS), [], [B_small])
        P.barrier()
        base_mark = A.mark()

        def stage_adaln():
            m = A.mark()
            cs = A.alloc([2, 8], F32)
            csb = A.alloc([8, 2], BF16)
            wsl = [A.alloc([8, 1536], BF16) for _ in range(2)]
            Bw = [Buf("aw0"), Buf("aw1")]
            Bc = Buf("cs")
            tmpm = A.alloc([8, 2], F32)
            dma("sp", cs, cc, [], [Bc])
            act(csb, cs.rearrange("p j c -> p c j"), AF.Silu, [Bc], [Bc])
            for i in range(n_layers):
                pb = i % 2
                for cb in range(4):
                    s = (i * 4 + cb) % 2
                    dma("pool", wsl[s], ada_w[i].rearrange("(kc p) n -> p kc n", p=128)[:, :, cb * 1536:(cb + 1) * 1536], [], [Bw[s]])
                    for j in range(12):
                        col = (cb * 12 + j) * 2
                        for kc in range(KC):
                            mm(bank[pb][:, col:col + 2], wsl[s][:, kc, j * 128:(j + 1) * 128], csb[:, kc, :], kc == 0, kc == KC - 1, [Bw[s], Bc], [PB[pb]])
                tt("dve", modt[:, i], bank[pb][:, 0:96].rearrange("p (q j) -> p q j", j=2), adab[:, i, :].unsqueeze(2).to_broadcast([128, 48, 2]), ALU.add, [PB[pb], B_small], [B_mod])
                for (gm, ng, off) in ((gm1, ngm, 8), (gm2, ngf, 32)):
                    ts("dve", tmpm, modt[:, i, off:off + 8, :], 1.0, None, ALU.add, None, [B_mod], [Bc])
                    tt("dve", gm[:, i], tmpm, ng[:, i, :].unsqueeze(2).to_broadcast([128, 8, 2]), ALU.mult, [Bc, B_small], [B_mod])
            P.barrier()
            A.release(m)

        def stage_norm(layer, gm, sh_off, hT, Bh, x_sb=None, Bx=None, xsrc=None, Bxs=None, tiles=TILES, hf_cb=None):
            m = A.mark()
            stg = None
            if x_sb is None:
                stg = [A.alloc([8, 512], F32) for _ in range(2)]
                Bst = [Buf(), Buf()]
            sq = [A.alloc([8, 512], BF16) for _ in range(2)]
            Bsq = [Buf(), Buf()]
            rs = [A.alloc([512], F32) for _ in range(2)]
            Brs = [Buf(), Buf()]
            tmp = [A.alloc([8, 512], F32) for _ in range(2)]
            Btmp = [Buf(), Buf()]
            if hf_cb is not None:
                hf = [A.alloc([8, 512], F32) for _ in range(2)]
                Bhf = [Buf(), Buf()]
            info = {}

            def front(ti):
                (t0, n, j) = tiles[ti]
                s = ti % 2
                if x_sb is None:
                    dma("sp", stg[s][:, :, 0:n], xsrc.rearrange("(c p) t -> p c t", p=128)[:, :, t0:t0 + n], [Bxs], [Bst[s]])
                    xt = stg[s][:, :, 0:n]
                    Bxl = [Bst[s]]
                else:
                    xt = x_sb[:, :, t0:t0 + n]
                    Bxl = Bx[TG[t0]]
                info[ti] = (xt, Bxl)
                act(sq[s][:, :, 0:n], xt, AF.Square, Bxl, [Bsq[s]])
                pb = 6 + s
                for c in range(KC):
                    mm(bank[pb][:, 0:n], onesb, sq[s][:, c, 0:n], c == 0, c == KC - 1, [Bsq[s], B_small], [PB[pb]])

            def frontB(ti):
                (t0, n, j) = tiles[ti]
                s = ti % 2
                pb = 6 + s
                act(rs[s][:, 0:n], bank[pb][:, 0:n], AF.Ln, [PB[pb]], [Brs[s]], scale=1.0 / D, bias=epsc[:, 0:1])
                act(rs[s][:, 0:n], rs[s][:, 0:n], AF.Exp, [Brs[s]], [Brs[s]], scale=-0.5)

            def back(ti):
                (t0, n, j) = tiles[ti]
                s = ti % 2
                (xt, Bxl) = info[ti]
                tt("dve", tmp[s][:, :, 0:n], xt, rs[s][:, 0:n].unsqueeze(1).to_broadcast([128, 8, n]), ALU.mult, Bxl + [Brs[s]], [Btmp[s]])
                for c in range(KC):
                    sc_ap = gm[:, layer, c, j:j + 1]
                    bi_ap = modt[:, layer, sh_off + c, j:j + 1]
                    if hf_cb is None:
                        if c < 4:
                            act(hT[:, c, t0:t0 + n], tmp[s][:, c, 0:n], AF.Identity, [Btmp[s], B_mod], [Bh[ti]], scale=sc_ap, bias=bi_ap)
                        else:
                            ts("pool", hT[:, c, t0:t0 + n], tmp[s][:, c, 0:n], sc_ap, bi_ap, ALU.mult, ALU.add, [Btmp[s], B_mod], [Bh[ti]])
                    else:
                        act(hT[:, c, t0:t0 + n], tmp[s][:, c, 0:n], AF.Identity, [Btmp[s], B_mod], [Bh[ti]], scale=sc_ap, bias=bi_ap)
                        ts("dve" if c < 4 else "pool", hf[s][:, c, 0:n], tmp[s][:, c, 0:n], sc_ap, bi_ap, ALU.mult, ALU.add, [Btmp[s], B_mod], [Bhf[s]])
                if hf_cb is not None:
                    hf_cb(ti, t0, n, hf[s], Bhf[s])

            front(0)
            frontB(0)
            for ti in range(len(tiles)):
                if ti + 1 < len(tiles):
                    front(ti + 1)
                back(ti)
                if ti + 1 < len(tiles):
                    frontB(ti + 1)
            P.barrier()
            A.release(m)

        def stage_retention(layer, hT, Bh):
            jl = layer // 2
            m = A.mark()
            ld = A.alloc([8], F32); lg = A.alloc([8], F32); cdec = A.alloc([8], F32)
            maskT = A.alloc([4, 128], F32)
            decf = A.alloc([4, 128], F32); decb = A.alloc([4, 128], F32)
            kdecf = A.alloc([4], F32); kdecb = A.alloc([4], F32)
            mt = A.alloc([128], F32)
            gnw = A.alloc([16], F32); gnb = A.alloc([16], F32)
            Bd = Buf("dec")
            dma("sp", ld, ret_ld[jl:jl + 1, :].partition_broadcast(128), [], [Bd])
            dma("sp", gnw, ret_gnw[:, jl, :], [], [Bd])
            dma("sp", gnb, ret_gnb[:, jl, :], [], [Bd])
            act(lg, ld, AF.Exp, [Bd], [Bd])
            ts("dve", lg, lg, -1.0, None, ALU.mult, None, [Bd], [Bd])
            ts("dve", cdec, lg, 128.0, None, ALU.mult, None, [Bd], [Bd])
            act(cdec, cdec, AF.Exp, [Bd], [Bd])
            for hd in range(4):
                f, b = hd, 4 + hd
                act(maskT[:, hd], cst[:, C_DPOS:C_DPOS + 128], AF.Exp, [Bd, B_cst], [Bd], scale=lg[:, f:f + 1])
                tt("dve", maskT[:, hd], maskT[:, hd], cst[:, C_LO:C_LO + 128], ALU.mult, [Bd], [Bd])
                act(mt, cst[:, C_DNEG:C_DNEG + 128], AF.Exp, [Bd, B_cst], [Bd], scale=lg[:, b:b + 1])
                tt("dve", mt, mt, cst[:, C_UP:C_UP + 128], ALU.mult, [Bd], [Bd])
                tt("dve", maskT[:, hd], maskT[:, hd], mt, ALU.add, [Bd], [Bd])
                act(decf[:, hd], cst[:, C_I1:C_I1 + 128], AF.Exp, [Bd, B_cst], [Bd], scale=lg[:, f:f + 1])
                act(decb[:, hd], cst[:, C_IB:C_IB + 128], AF.Exp, [Bd, B_cst], [Bd], scale=lg[:, b:b + 1])
                act(kdecf[:, hd:hd + 1], cst[:, C_PF:C_PF + 1], AF.Exp, [Bd, B_cst], [Bd], scale=lg[:, f:f + 1])
                act(kdecb[:, hd:hd + 1], cst[:, C_PB:C_PB + 1], AF.Exp, [Bd, B_cst], [Bd], scale=lg[:, b:b + 1])
            wq = A.alloc([8, 256], BF16); wqr = A.alloc([8, 256], BF16)
            wk = A.alloc([8, 256], BF16); wkr = A.alloc([8, 256], BF16)
            wv = A.alloc([8, 512], BF16); wg = A.alloc([8, 512], BF16)
            Bwq, Bwk, Bwv, Bwg = Buf("wq"), Buf("wk"), Buf("wv"), Buf("wg")
            qT = A.alloc([2, T], BF16)
            qfc = [A.alloc([2, 128], BF16) for _ in range(2)]; qbc = [A.alloc([2, 128], BF16) for _ in range(2)]; Bqc = [Buf(), Buf()]
            kT = A.alloc([2, T], BF16)
            kdf = A.alloc([NCH, 256], BF16); kdb = A.alloc([NCH, 256], BF16)
            v = A.alloc([NCH, 512], BF16)
            sg = A.alloc([4, T], BF16)
            Bq, Bk, Bkd, Bv, Bsg = Buf("q"), Buf("k"), Buf("kd"), Buf("v"), Buf("sg")
            t1 = [A.alloc([512], F32)] * 2
            t2 = [A.alloc([512], F32)] * 2
            qr = [A.alloc([512], F32) for _ in range(2)]
            Bt1, Bt2, Bqr = [Buf()] * 2, [Buf()] * 2, [Buf(), Buf()]
            Sf = A.alloc([2, 512], F32); Sb = A.alloc([2, 512], F32)
            Sfb = [A.alloc([2, 512], BF16) for _ in range(2)]; Sbb = [A.alloc([2, 512], BF16) for _ in range(2)]
            Sbin = [A.alloc([2, 512], BF16) for _ in range(2)]
            BSf, BSb, BSfb = Buf("Sf"), Buf("Sb"), [Buf("Sfb0"), Buf("Sfb1")]
            BSbb, BSbin = [Buf(), Buf()], [Buf(), Buf()]
            PT = [A.alloc([128], BF16) for _ in range(2)]; BPT = [Buf(), Buf()]
            osb = [A.alloc([512], F32) for _ in range(2)]; Bosb = [Buf(), Buf()]
            junk = t1[0]; Bjunk = Bt1[0]
            stat = [A.alloc([8], F32) for _ in range(2)]; Bstat = [Buf(), Buf()]
            onb = [A.alloc([512], BF16) for _ in range(2)]; Bonb = [Buf(), Buf()]
            tmpn = [A.alloc([4, 128], F32) for _ in range(2)]; Btn = [Buf(), Buf()]; Btnv = [[Buf() for _ in range(4)] for _ in range(2)]
            ystg = [A.alloc([4, 512], BF16) for _ in range(2)]; Bys = [Buf(), Buf()]
            w_in = ret_w_in[jl].rearrange("(kc p) n -> p kc n", p=128)
            psT = [psum[:, 4 + i, :].bitcast(BF16) for i in range(2)]
            psT2 = [psum[:, 6 + i, :].bitcast(BF16) for i in range(2)]

            def rot_weights(w, wr_, Bw):
                w4 = w.rearrange("p k (b h x) -> p (k b) h x", b=2, h=2)
                r4 = wr_.rearrange("p k (b h x) -> p (k b) h x", b=2, h=2)
                cp("dve", r4[:, :, 0, :], w4[:, :, 1, :], [Bw], [Bw])
                cp("dve", r4[:, :, 1, :], w4[:, :, 0, :], [Bw], [Bw])

            def rope_evac(dst_list, b_main, b_rot, dc, t0, n, j, s, Bdst, hd, with_decay, RB=C_ROPE, csc=1.0):
                if j == 0:
                    r0 = (t0 - NCTX) // 64
                    nr = n // 64
                    if dc == 0:
                        cosv = cst[:, RB + r0:RB + r0 + nr].unsqueeze(2).to_broadcast([128, nr, 64])
                        sinv = cst[:, RB + 32 + r0:RB + 32 + r0 + nr].unsqueeze(2).to_broadcast([128, nr, 64])
                    else:
                        cosv = cst[:, RB + 64:RB + 128].unsqueeze(1).to_broadcast([128, nr, 64])
                        sinv = cst[:, RB + 128:RB + 192].unsqueeze(1).to_broadcast([128, nr, 64])
                    v3 = lambda ap: ap.rearrange("p (r c) -> p r c", c=64)
                    tt("dve", v3(t1[s][:, 0:n]), v3(bank[b_main][:, 0:n]), cosv, ALU.mult, [PB[b_main], B_cst], [Bt1[s]])
                    tt("dve", v3(t2[s][:, 0:n]), v3(bank[b_rot][:, 0:n]), sinv, ALU.mult, [PB[b_rot], B_cst], [Bt2[s]])
                    tt("pool", qr[s][:, 0:n], t1[s][:, 0:n], t2[s][:, 0:n], ALU.add, [Bt1[s], Bt2[s]], [Bqr[s]])
                else:
                    act(qr[s][:, 0:n], bank[b_main][:, 0:n], AF.Copy, [PB[b_main]], [Bqr[s]], scale=csc)
                cp("act", dst_list[0][:, dc, t0:t0 + n], qr[s][:, 0:n], [Bqr[s]], [Bdst])
                if with_decay:
                    nck = n // 128
                    q3 = qr[s][:, 0:n].rearrange("p (c i) -> p c i", i=128)
                    tt("pool", dst_list[1][:, dc, t0:t0 + n].rearrange("p (c i) -> p c i", i=128), q3,
                       decf[:, hd].unsqueeze(1).to_broadcast([128, nck, 128]), ALU.mult, [Bqr[s], Bd], [Bdst])
                    tt("dve", dst_list[2][:, dc, t0:t0 + n].rearrange("p (c i) -> p c i", i=128), q3,
                       decb[:, hd].unsqueeze(1).to_broadcast([128, nck, 128]), ALU.mult, [Bqr[s], Bd], [Bdst])

            def load_head_weights(hd):
                dma("pool", wq, w_in[:, :, hd * 256:(hd + 1) * 256], [], [Bwq])
                rot_weights(wq, wqr, Bwq)
                dma("pool", wk, w_in[:, :, 3072 + hd * 256:3072 + (hd + 1) * 256], [], [Bwk])
                rot_weights(wk, wkr, Bwk)
                dma("pool", wv, w_in[:, :, 4096 + hd * 512:4096 + (hd + 1) * 512], [], [Bwv])
                dma("pool", wg, w_in[:, :, 1024 + hd * 512:1024 + (hd + 1) * 512], [], [Bwg])

            load_head_weights(0)
            for hd in range(4):
                if hd > 0 and "prefetch" not in OPT:
                    load_head_weights(hd)
                cnt = 0
                for ti, (t0, n, j) in enumerate(TILES):
                    for (w, wr_, Bw, dsts, Bdst, wd, RB, csc) in ((wq, wqr, Bwq, (qT,), Bq, False, C_ROPE, 1.0), (wk, wkr, Bwk, (kT,), Bk, False, C_ROPEK, 0.0625)):
                        for dc in range(2):
                            bm, br = 0 + (cnt % 2) * 2, 1 + (cnt % 2) * 2
                            for kc in range(KC):
                                mm(bank[bm][:, 0:n], w[:, kc, dc * 128:(dc + 1) * 128], hT[:, kc, t0:t0 + n], kc == 0, kc == KC - 1, [Bw, Bh[ti]], [PB[bm]])
                            if j == 0:
                                for kc in range(KC):
                                    mm(bank[br][:, 0:n], wr_[:, kc, dc * 128:(dc + 1) * 128], hT[:, kc, t0:t0 + n], kc == 0, kc == KC - 1, [Bw, Bh[ti]], [PB[br]])
                            rope_evac(dsts, bm, br, dc, t0, n, j, cnt % 2, Bdst, hd, wd, RB, csc)
                            cnt += 1
                    for vc in range(4):
                        pb = 6 + (vc % 2)
                        for kc in range(KC):
                            mm(bank[pb][:, 0:n], wg[:, kc, vc * 128:(vc + 1) * 128], hT[:, kc, t0:t0 + n], kc == 0, kc == KC - 1, [Bwg, Bh[ti]], [PB[pb]])
                        act(sg[:, vc, t0:t0 + n], bank[pb][:, 0:n], AF.Silu, [PB[pb]], [Bsg])
                    for sub in range(n // 128):
                        c = t0 // 128 + sub
                        pb = 6 + (sub % 2)
                        for kc in range(KC):
                            mm(bank[pb], hT[:, kc, c * 128:(c + 1) * 128], wv[:, kc, :], kc == 0, kc == KC - 1, [Bwv, Bh[ti]], [PB[pb]])
                        cp("act", v[:, c, :], bank[pb], [PB[pb]], [Bv])
                if hd < 3 and "prefetch" in OPT:
                    load_head_weights(hd + 1)
                for c in range(NCH):
                    s = c % 2
                    for dc in range(2):
                        tr(psT[s][:, dc * 128:(dc + 1) * 128], kT[:, dc, c * 128:(c + 1) * 128], identb, [Bk, B_small], [PB[4 + s]])
                    ts("dve", kdf[:, c, :], psT[s][:, 0:256], kdecf[:, hd:hd + 1], None, ALU.mult, None, [PB[4 + s], Bd], [Bkd])
                    ts("dve", kdb[:, c, :], psT[s][:, 0:256], kdecb[:, hd:hd + 1], None, ALU.mult, None, [PB[4 + s], Bd], [Bkd])
                BSbd = [Buf(), Buf()]
                P.op("dve", lambda e: e.memset(Sb, 0.0), [], [BSb])
                orderB = [1, 0] + list(range(NCH - 1, 1, -1))
                for idx, c in enumerate(orderB):
                    s = idx % 2
                    for dc in range(2):
                        cp("act", Sbb[s][:, dc, :], Sb[:, dc, :], [BSb, BSbd[dc]], [BSbb[s]])
                    dma("sp", sb_dram[c].rearrange("p (a b) -> p a b", a=2), Sbb[s], [BSbb[s]], [B_sbd[c]])
                    for dc in range(2):
                        pb = 0 + dc
                        mm(bank[pb], kdb[:, c, dc * 128:(dc + 1) * 128], v[:, c, :], True, True, [Bkd, Bv], [PB[pb]])
                        stt(Sb[:, dc, :], Sb[:, dc, :], cdec[:, 4 + hd:5 + hd], bank[pb], ALU.mult, ALU.add, [BSb, BSbd[dc], PB[pb], Bd], [BSbd[dc]])
                P.op("dve", lambda e: e.memset(Sf, 0.0), [], [BSf])
                P.op("pool", lambda e: e.memset(Sfb[0], 0.0), [], [BSfb[0]])
                gstate = [0]

                def stA(c, hd=hd):
                    s = c % 2
                    ck = slice(c * 128, (c + 1) * 128)
                    dma("sp", Sbin[s], sb_dram[c].rearrange("p (a b) -> p a b", a=2), [B_sbd[c]], [BSbin[s]])
                    for dc in range(2):
                        mm(bank[2][:, 0:128], kT[:, dc, ck], qT[:, dc, ck], dc == 0, dc == 1, [Bk, Bq], [PB[2]])
                    for dc in range(2):
                        mm(bank[dc], kdf[:, c, dc * 128:(dc + 1) * 128], v[:, c, :], True, True, [Bkd, Bv], [PB[dc]])
                    tt("dve", PT[s], bank[2][:, 0:128], maskT[:, hd], ALU.mult, [PB[2], Bd], [BPT[s]])
                    tt("pool", qfc[s], qT[:, :, ck], decf[:, hd].unsqueeze(1).to_broadcast([128, 2, 128]), ALU.mult, [Bq, Bd], [Bqc[s]])
                    tt("pool", qbc[s], qT[:, :, ck], decb[:, hd].unsqueeze(1).to_broadcast([128, 2, 128]), ALU.mult, [Bq, Bd], [Bqc[s]])
                    mm(bank[3], PT[s], v[:, c, :], True, False, [BPT[s], Bv], [PB[3]])
                    for dc in range(2):
                        mm(bank[3], qfc[s][:, dc, :], Sfb[s][:, dc, :], False, False, [Bqc[s], BSfb[s]], [PB[3]])
                    for dc in range(2):
                        mm(bank[3], qbc[s][:, dc, :], Sbin[s][:, dc, :], False, dc == 1, [Bqc[s], BSbin[s]], [PB[3]])
                    for dc in range(2):
                        stt(Sf[:, dc, :], Sf[:, dc, :], cdec[:, hd:hd + 1], bank[dc], ALU.mult, ALU.add, [BSf, PB[dc], Bd], [BSf])
                    cp("dve", Sfb[1 - s], Sf, [BSf], [BSfb[1 - s]])
                    act(osb[s], bank[3], AF.Identity, [PB[3]], [Bosb[s], Bstat[s]], accum_out=stat[s][:, 0:1])
                    act(junk, osb[s], AF.Square, [Bosb[s]], [Bjunk, Bstat[s]], accum_out=stat[s][:, 1:2])

                def stB(c, hd=hd):
                    s = c % 2
                    sv = stat[s]
                    ts("dve", sv[:, 2:3], sv[:, 0:1], 1.0 / 512, None, ALU.mult, None, [Bstat[s]], [Bstat[s]])
                    tt("dve", sv[:, 3:4], sv[:, 2:3], sv[:, 2:3], ALU.mult, [Bstat[s]], [Bstat[s]])
                    stt(sv[:, 4:5], sv[:, 1:2], 1.0 / 512, sv[:, 3:4], ALU.mult, ALU.subtract, [Bstat[s]], [Bstat[s]])
                    ts("dve", sv[:, 4:5], sv[:, 4:5], EPS, None, ALU.add, None, [Bstat[s]], [Bstat[s]])
                    act(sv[:, 4:5], sv[:, 4:5], AF.Sqrt, [Bstat[s]], [Bstat[s]])
                    P.op("dve", lambda e, sv=sv: e.reciprocal(out=sv[:, 5:6], in_=sv[:, 4:5]), [Bstat[s]], [Bstat[s]])
                    stt(sv[:, 6:7], sv[:, 2:3], -1.0, sv[:, 5:6], ALU.mult, ALU.mult, [Bstat[s]], [Bstat[s]])
                    act(onb[s], osb[s], AF.Identity, [Bosb[s], Bstat[s]], [Bonb[s]], scale=sv[:, 5:6], bias=sv[:, 6:7])
                    for vc in range(4):
                        if vc < 2:
                            tr(psT[s][:, vc * 128:(vc + 1) * 128], onb[s][:, vc * 128:(vc + 1) * 128], identb, [Bonb[s], B_small], [PB[4 + s]])
                        else:
                            tr(psT2[s][:, vc * 128:(vc + 1) * 128], onb[s][:, vc * 128:(vc + 1) * 128], identb, [Bonb[s], B_small], [PB[6 + s]])

                def stC(c, hd=hd):
                    s = c % 2
                    ck = slice(c * 128, (c + 1) * 128)
                    for vc in range(4):
                        if vc < 2:
                            act(tmpn[s][:, vc, :], psT[s][:, vc * 128:(vc + 1) * 128], AF.Identity, [PB[4 + s], Bd], [Btnv[s][vc]],
                                scale=gnw[:, hd * 4 + vc:hd * 4 + vc + 1], bias=gnb[:, hd * 4 + vc:hd * 4 + vc + 1])
                        else:
                            ts("dve", tmpn[s][:, vc, :], psT2[s][:, vc * 128:(vc + 1) * 128], gnw[:, hd * 4 + vc:hd * 4 + vc + 1], gnb[:, hd * 4 + vc:hd * 4 + vc + 1],
                               ALU.mult, ALU.add, [PB[6 + s], Bd], [Btnv[s][vc]])
                    if c < 2:
                        g0, gn = 0, 2
                    else:
                        g0, gn = 2 + ((c - 2) // 4) * 4, 4
                    gs = gstate[0] % 2
                    ci = c - g0
                    tt("pool", ystg[gs][:, :, ci * 128:(ci + 1) * 128], tmpn[s], sg[:, :, ck], ALU.mult, Btnv[s] + [Bsg], [Bys[gs]])
                    if ci == gn - 1:
                        dma("sp", yT_dram.rearrange("(h vc p) t -> p h vc t", p=128, vc=4)[:, hd, :, g0 * 128:(g0 + gn) * 128],
                            ystg[gs][:, :, 0:gn * 128], [Bys[gs]], [B_yd])
                        gstate[0] += 1

                for it in range(NCH + 2):
                    if it < NCH:
                        stA(it)
                    if 0 <= it - 1 < NCH:
                        stB(it - 1)
                    if 0 <= it - 2 < NCH:
                        stC(it - 2)
            P.barrier()
            A.release(m)

        def stage_outproj(layer, x_sb, Bx, w_out_ap, kco, tiles):
            m = A.mark()
            wo = A.alloc([kco, 1024], BF16); Bwo = Buf("wo")
            yt = [A.alloc([kco, 512], BF16) for _ in range(2)]; Byt = [Buf(), Buf()]
            dma("pool", wo, w_out_ap.rearrange("(kc p) n -> p kc n", p=128), [], [Bwo])
            cnt = 0
            for ti, (t0, n, j) in enumerate(tiles):
                s = ti % 2
                dma("sp", yt[s][:, :, 0:n], yT_dram.rearrange("(kc p) t -> p kc t", p=128)[:, 0:kco, t0:t0 + n], [B_yd], [Byt[s]])
                for dcn in range(8):
                    pb = cnt % 4
                    cnt += 1
                    for kc in range(kco):
                        mm(bank[pb][:, 0:n], wo[:, kc, dcn * 128:(dcn + 1) * 128], yt[s][:, kc, 0:n], kc == 0, kc == kco - 1, [Bwo, Byt[s]], [PB[pb]])
                    stt(x_sb[:, dcn, t0:t0 + n], bank[pb][:, 0:n], modt[:, layer, 16 + dcn, j:j + 1], x_sb[:, dcn, t0:t0 + n], ALU.mult, ALU.add, [PB[pb], B_mod, Bx[TG[t0]][dcn]], [Bx[TG[t0]][dcn]])
            P.barrier()
            A.release(m)

        def ffn_expert(layer, x_sb, Bx, hT, Bh, wg_ap, wu_ap, wd_ap, F, tiles, slots, Ge=None, BGe=None):
            (wgs, wus, wds, Bws, sgt, Bsgt, actT, Bact, tmp2, Btmp2, cnts, GS) = slots
            NW = len(wgs)
            NFC = GS // 128
            w_g = wg_ap.rearrange("(kc p) n -> p kc n", p=128)
            w_u = wu_ap.rearrange("(kc p) n -> p kc n", p=128)
            w_d = wd_ap.rearrange("(fc p) n -> p fc n", p=128)
            assert F % GS == 0
            for grp in range(F // GS):
                ws = cnts[0] % NW
                cnts[0] += 1
                dma("pool", wgs[ws], w_g[:, :, grp * GS:(grp + 1) * GS], [], [Bws[ws]])
                dma("pool", wus[ws], w_u[:, :, grp * GS:(grp + 1) * GS], [], [Bws[ws]])
                dma("pool", wds[ws], w_d[:, grp * NFC:(grp + 1) * NFC, :], [], [Bws[ws]])
                for ti, (t0, n, j) in enumerate(tiles):
                    a_s = cnts[1] % 2
                    cnts[1] += 1
                    for fc in range(NFC):
                        f2 = fc % 2
                        pg = 0 + f2
                        pu = 2 + f2
                        for kc in range(KC):
                            mm(bank[pg][:, 0:n], wgs[ws][:, kc, fc * 128:(fc + 1) * 128], hT[:, kc, t0:t0 + n], kc == 0, kc == KC - 1, [Bws[ws], Bh[ti]], [PB[pg]])
                        for kc in range(KC):
                            mm(bank[pu][:, 0:n], wus[ws][:, kc, fc * 128:(fc + 1) * 128], hT[:, kc, t0:t0 + n], kc == 0, kc == KC - 1, [Bws[ws], Bh[ti]], [PB[pu]])
                        act(sgt[f2][:, 0:n], bank[pg][:, 0:n], AF.Silu, [PB[pg]], [Bsgt[f2]])
                        if Ge is None:
                            tt("dve", actT[a_s][:, fc, 0:n], bank[pu][:, 0:n], sgt[f2][:, 0:n], ALU.mult, [PB[pu], Bsgt[f2]], [Bact[a_s][fc]])
                        else:
                            tt("dve", tmp2[f2][:, 0:n], bank[pu][:, 0:n], sgt[f2][:, 0:n], ALU.mult, [PB[pu], Bsgt[f2]], [Btmp2[f2]])
                            tt("dve", actT[a_s][:, fc, 0:n], tmp2[f2][:, 0:n], Ge[:, t0:t0 + n], ALU.mult, [Btmp2[f2], BGe], [Bact[a_s][fc]])
                    if cnts[3] is not None:
                        cnts[3]()

                    def down(ws=ws, a_s=a_s, t0=t0, n=n, j=j):
                        for dcn in range(8):
                            pb = 4 + (cnts[2] % 4)
                            cnts[2] += 1
                            for fc in range(NFC):
                                mm(bank[pb][:, 0:n], wds[ws][:, fc, dcn * 128:(dcn + 1) * 128], actT[a_s][:, fc, 0:n], fc == 0, fc == NFC - 1, [Bws[ws], Bact[a_s][fc]], [PB[pb]])
                            bx = Bx[TG[t0]][dcn]
                            stt(x_sb[:, dcn, t0:t0 + n], bank[pb][:, 0:n], modt[:, layer, 40 + dcn, j:j + 1], x_sb[:, dcn, t0:t0 + n], ALU.mult, ALU.add, [PB[pb], B_mod, bx], [bx])
                    cnts[3] = down

        def ffn_flush(slots):
            cnts = slots[-2]
            if cnts[3] is not None:
                cnts[3]()
                cnts[3] = None

        def alloc_ffn_slots(moe):
            GS = 512 if moe else 256
            NFC = GS // 128
            NW = 2 if moe else 3
            wgs = [A.alloc([8, GS], BF16) for _ in range(NW)]
            wus = [A.alloc([8, GS], BF16) for _ in range(NW)]
            wds = [A.alloc([NFC, 1024], BF16) for _ in range(NW)]
            Bws = [Buf() for _ in range(NW)]
            sgt = [A.alloc([512], BF16) for _ in range(2)]; Bsgt = [Buf(), Buf()]
            actT = [A.alloc([NFC, 512], BF16) for _ in range(2)]; Bact = [[Buf() for _ in range(NFC)] for _ in range(2)]
            tmp2 = [A.alloc([512], F32) for _ in range(2)] if moe else None
            Btmp2 = [Buf(), Buf()]
            return (wgs, wus, wds, Bws, sgt, Bsgt, actT, Bact, tmp2, Btmp2, [0, 0, 0, None], GS)

        def stage_gla(layer, hT, Bh, last):
            jl = layer // 2
            m = A.mark()
            w_in = gla_w_in[jl].rearrange("(kc p) n -> p kc n", p=128)
            bg = A.alloc([2, 4], F32); nbg = A.alloc([2, 4], F32); ng = A.alloc([8], F32)
            Bc = Buf("glac")
            dma("sp", bg, gla_bg[:, jl], [], [Bc])
            dma("sp", ng, gla_ng[:, jl, :], [], [Bc])
            ts("dve", nbg, bg, -1.0, None, ALU.mult, None, [Bc], [Bc])
            wa = A.alloc([8, 32], BF16); Bwa = Buf("wa")
            wup = A.alloc([2, 512], BF16)
            aT = A.alloc([2, T], BF16); BaT = Buf("aT")
            dma("pool", wa, w_in[:, :, 3072:3104], [], [Bwa])
            dma("pool", wup[0:16], gla_wgu[jl].rearrange("d r f -> r d f"), [], [Bwa])
            cnt = 0
            for ti, (t0, n, j) in enumerate(TILES):
                for dr in range(2):
                    pb = cnt % 2
                    cnt += 1
                    for kc in range(KC):
                        mm(bank[pb][0:16, 0:n], wa[:, kc, dr * 16:(dr + 1) * 16], hT[:, kc, t0:t0 + n], kc == 0, kc == KC - 1, [Bwa, Bh[ti]], [PB[pb]])
                    cp("act", aT[0:16, dr, t0:t0 + n], bank[pb][0:16, 0:n], [PB[pb]], [BaT])
            wq = A.alloc([8, 128], BF16); wk = A.alloc([8, 128], BF16)
            wv = A.alloc([8, 256], BF16); wr_ = A.alloc([8, 256], BF16)
            Bwq, Bwk, Bwv, Bwr = Buf(), Buf(), Buf(), Buf()
            qs = A.alloc([T], F32); ks = A.alloc([T], F32); Bqs, Bks = Buf(), Buf()
            la = A.alloc([T], F32); Bla = Buf()
            Pp = A.alloc([T + 1], F32); BPp = Buf()
            fq = A.alloc([T], F32); fk = A.alloc([T], F32); fs = A.alloc([T], F32); Bfq, Bfk, Bfs = Buf(), Buf(), Buf()
            ex = [A.alloc([512], F32) for _ in range(2)]; Bex = [Buf(), Buf()]
            qt = [A.alloc([T], BF16) for _ in range(2)]; kt = [A.alloc([T], BF16) for _ in range(2)]
            kp = A.alloc([T], BF16)
            Bqt, Bkt, Bkp = [Buf(), Buf()], [Buf(), Buf()], Buf()
            kptm = [A.alloc([NCH, 128], BF16) for _ in range(2)]; Bkptm = [Buf(), Buf()]
            ed = [A.alloc([NCH], F32) for _ in range(2)]; Bed = [Buf(), Buf()]
            v = A.alloc([NCH, 256], BF16); Bv = Buf()
            sr = A.alloc([2, T], BF16); Bsr = Buf()
            Sf = A.alloc([256], F32); Sb = A.alloc([256], F32); BSf, BSb = Buf(), Buf()
            Sfb = [A.alloc([256], BF16) for _ in range(2)]; BSfb = [Buf(), Buf()]
            Sbb = [A.alloc([256], BF16) for _ in range(2)]; BSbb = [Buf(), Buf()]
            Sbin = [A.alloc([256], BF16) for _ in range(2)]; BSbin = [Buf(), Buf()]
            PTf = [A.alloc([128], BF16) for _ in range(2)]; PTb = [A.alloc([128], BF16) for _ in range(2)]; BPT = [Buf(), Buf()]
            junk = A.alloc([256], F32); Bjunk = Buf()
            stat = [A.alloc([4], F32) for _ in range(2)]; Bstat = [Buf(), Buf()]
            onb = [A.alloc([256], BF16) for _ in range(2)]; Bonb = [Buf(), Buf()]
            tmpn = [A.alloc([2, 128], F32) for _ in range(2)]; Btn = [Buf(), Buf()]
            ystg = [A.alloc([2, 512], BF16) for _ in range(2)]; Bys = [Buf(), Buf()]
            psT = [psum[:, 4 + i, :].bitcast(BF16) for i in range(2)]
            P.op("dve", lambda e: e.memset(Pp[:, 0:1], 0.0), [], [BPp])
            QB = float(np.log(128.0 ** -0.5))
            pv = lambda ap: ap.rearrange("p (c i) -> p c i", i=128)
            gstate = [0]
            def gla_load(hd):
                dma("pool", wq, w_in[:, :, hd * 128:(hd + 1) * 128], [], [Bwq])
                dma("pool", wk, w_in[:, :, 1536 + hd * 128:1536 + (hd + 1) * 128], [], [Bwk])
                dma("pool", wv, w_in[:, :, 2048 + hd * 256:2048 + (hd + 1) * 256], [], [Bwv])
                dma("pool", wr_, w_in[:, :, 512 + hd * 256:512 + (hd + 1) * 256], [], [Bwr])

            def gla_gates(dr, hd):
                for ti, (t0, n, j) in enumerate(TILES):
                    s = ti % 2
                    pb = 2 + s
                    mm(bank[pb][:, 0:n], wup[0:16, dr, hd * 128:(hd + 1) * 128], aT[0:16, dr, t0:t0 + n], True, True, [Bwa, BaT], [PB[pb]])
                    act(ex[s][:, 0:n], bank[pb][:, 0:n], AF.Exp, [PB[pb], Bc], [Bex[s]], scale=-1.0, bias=nbg[:, dr, hd:hd + 1])
                    act(la[:, t0:t0 + n], ex[s][:, 0:n], AF.Ln, [Bex[s]], [Bla], bias=1.0)
                P.op("dve", lambda e: e.tensor_tensor_scan(out=Pp[:, 1:T + 1], data0=la, data1=la, initial=0.0, op0=ALU.add, op1=ALU.add), [Bla], [BPp])
                c0v = pv(Pp[:, 0:T])[:, :, 0:1].to_broadcast([128, NCH, 128])
                cev = pv(Pp[:, 1:T + 1])[:, :, 127:128].to_broadcast([128, NCH, 128])
                if dr == 0:
                    tt("dve", pv(fq), pv(Pp[:, 1:T + 1]), c0v, ALU.subtract, [BPp], [Bfq])
                    tt("dve", pv(fs), cev, pv(Pp[:, 1:T + 1]), ALU.subtract, [BPp], [Bfs])
                else:
                    tt("dve", pv(fq), cev, pv(Pp[:, 0:T]), ALU.subtract, [BPp], [Bfq])
                    tt("dve", pv(fs), pv(Pp[:, 0:T]), c0v, ALU.subtract, [BPp], [Bfs])
                tt("dve", ed[dr], pv(Pp[:, 1:T + 1])[:, :, 127], pv(Pp[:, 0:T])[:, :, 0], ALU.subtract, [BPp], [Bed[dr]])
                act(ed[dr], ed[dr], AF.Exp, [Bed[dr]], [Bed[dr]], scale=-1.0 / 32)
                act(fk, fq, AF.Exp, [Bfq], [Bfk], scale=1.0 / 32)
                act(fq, fq, AF.Exp, [Bfq], [Bfq], scale=-1.0 / 32, bias=QB)
                act(fs, fs, AF.Exp, [Bfs], [Bfs], scale=-1.0 / 32)

            def gla_products(dr):
                tt("pool", qt[dr], qs, fq, ALU.mult, [Bqs, Bfq], [Bqt[dr]])
                tt("dve", kt[dr], ks, fk, ALU.mult, [Bks, Bfk], [Bkt[dr]])
                tt("pool", kp, ks, fs, ALU.mult, [Bks, Bfs], [Bkp])
                for c in range(NCH):
                    s = c % 2
                    tr(psT[s][:, 0:128], kp[:, c * 128:(c + 1) * 128], identb, [Bkp, B_small], [PB[4 + s]])
                    cp("act", kptm[dr][:, c, :], psT[s][:, 0:128], [PB[4 + s]], [Bkptm[dr]])

            gla_load(0)
            for hd in range(4):
                gla_gates(0, hd)
                for ti, (t0, n, j) in enumerate(TILES):
                    for (w, Bw, dst, Bdst, pb) in ((wq, Bwq, qs, Bqs, 0), (wk, Bwk, ks, Bks, 1)):
                        for kc in range(KC):
                            mm(bank[pb][:, 0:n], w[:, kc, :], hT[:, kc, t0:t0 + n], kc == 0, kc == KC - 1, [Bw, Bh[ti]], [PB[pb]])
                        cp("act", dst[:, t0:t0 + n], bank[pb][:, 0:n], [PB[pb]], [Bdst])
                    for vc in range(2):
                        pb = 2 + vc
                        for kc in range(KC):
                            mm(bank[pb][:, 0:n], wr_[:, kc, vc * 128:(vc + 1) * 128], hT[:, kc, t0:t0 + n], kc == 0, kc == KC - 1, [Bwr, Bh[ti]], [PB[pb]])
                        act(sr[:, vc, t0:t0 + n], bank[pb][:, 0:n], AF.Silu, [PB[pb]], [Bsr])
                    for sub in range(n // 128):
                        c = t0 // 128 + sub
                        pb = 6 + (sub % 2)
                        for kc in range(KC):
                            mm(bank[pb][:, 0:256], hT[:, kc, c * 128:(c + 1) * 128], wv[:, kc, :], kc == 0, kc == KC - 1, [Bwv, Bh[ti]], [PB[pb]])
                        cp("dve", v[:, c, :], bank[pb][:, 0:256], [PB[pb]], [Bv])
                if hd < 3:
                    gla_load(hd + 1)
                gla_products(0)
                gla_gates(1, hd)
                gla_products(1)
                P.op("dve", lambda e: e.memset(Sb, 0.0), [], [BSb])
                orderB = [1, 0] + list(range(NCH - 1, 1, -1))
                for idx, c in enumerate(orderB):
                    s = idx % 2
                    cp("act", Sbb[s], Sb, [BSb], [BSbb[s]])
                    dma("sp", sb_dram[c][:, 0:256], Sbb[s], [BSbb[s]], [B_sbd[c]])
                    mm(bank[0][:, 0:256], kptm[1][:, c, :], v[:, c, :], True, True, [Bkptm[1], Bv], [PB[0]])
                    stt(Sb, Sb, ed[1][:, c:c + 1], bank[0][:, 0:256], ALU.mult, ALU.add, [BSb, PB[0], Bed[1]], [BSb])
                P.op("dve", lambda e: e.memset(Sf, 0.0), [], [BSf])
                P.op("pool", lambda e: e.memset(Sfb[0], 0.0), [], [BSfb[0]])
                OB = (3, 1)

                def gA(c, hd=hd):
                    s = c % 2
                    ck = slice(c * 128, (c + 1) * 128)
                    ob = OB[s]
                    need_out = not (last and c < 2)
                    mm(bank[0][:, 0:256], kptm[0][:, c, :], v[:, c, :], True, True, [Bkptm[0], Bv], [PB[0]])
                    if need_out:
                        dma("sp", Sbin[s], sb_dram[c][:, 0:256], [B_sbd[c]], [BSbin[s]])
                        mm(bank[2][:, 0:128], kt[0][:, ck], qt[0][:, ck], True, True, [Bkt[0], Bqt[0]], [PB[2]])
                        mm(bank[2][:, 128:256], kt[1][:, ck], qt[1][:, ck], True, True, [Bkt[1], Bqt[1]], [PB[2]])
                        tt("dve", PTf[s], bank[2][:, 0:128], cst[:, C_LO:C_LO + 128], ALU.mult, [PB[2], B_cst], [BPT[s]])
                        tt("dve", PTb[s], bank[2][:, 128:256], cst[:, C_UP:C_UP + 128], ALU.mult, [PB[2], B_cst], [BPT[s]])
                        mm(bank[ob][:, 0:256], PTf[s], v[:, c, :], True, False, [BPT[s], Bv], [PB[ob]])
                        mm(bank[ob][:, 0:256], PTb[s], v[:, c, :], False, False, [BPT[s], Bv], [PB[ob]])
                        mm(bank[ob][:, 0:256], qt[0][:, ck], Sfb[s], False, False, [Bqt[0], BSfb[s]], [PB[ob]])
                        mm(bank[ob][:, 0:256], qt[1][:, ck], Sbin[s], False, True, [Bqt[1], BSbin[s]], [PB[ob]])
                    stt(Sf, Sf, ed[0][:, c:c + 1], bank[0][:, 0:256], ALU.mult, ALU.add, [BSf, PB[0], Bed[0]], [BSf])
                    cp("dve", Sfb[1 - s], Sf, [BSf], [BSfb[1 - s]])
                    if need_out:
                        act(junk, bank[ob][:, 0:256], AF.Square, [PB[ob]], [Bjunk, Bstat[s]], accum_out=stat[s][:, 0:1])

                def gB(c, hd=hd):
                    s = c % 2
                    ob = OB[s]
                    if last and c < 2:
                        return
                    sv = stat[s]
                    ts("dve", sv[:, 1:2], sv[:, 0:1], 1.0 / 256, EPS, ALU.mult, ALU.add, [Bstat[s]], [Bstat[s]])
                    act(sv[:, 1:2], sv[:, 1:2], AF.Sqrt, [Bstat[s]], [Bstat[s]])
                    P.op("dve", lambda e, sv=sv: e.reciprocal(out=sv[:, 2:3], in_=sv[:, 1:2]), [Bstat[s]], [Bstat[s]])
                    act(onb[s], bank[ob][:, 0:256], AF.Copy, [PB[ob], Bstat[s]], [Bonb[s]], scale=sv[:, 2:3])
                    for vc in range(2):
                        tr(psT[s][:, vc * 128:(vc + 1) * 128], onb[s][:, vc * 128:(vc + 1) * 128], identb, [Bonb[s], B_small], [PB[4 + s]])

                def gC(c, hd=hd):
                    s = c % 2
                    ck = slice(c * 128, (c + 1) * 128)
                    if last and c < 2:
                        return
                    for vc in range(2):
                        ts("dve", tmpn[s][:, vc, :], psT[s][:, vc * 128:(vc + 1) * 128], ng[:, hd * 2 + vc:hd * 2 + vc + 1], None, ALU.mult, None, [PB[4 + s], Bc], [Btn[s]])
                    if c < 2:
                        g0, gn = 0, 2
                    else:
                        g0, gn = 2 + ((c - 2) // 4) * 4, 4
                    gs = gstate[0] % 2
                    ci = c - g0
                    tt("pool", ystg[gs][:, :, ci * 128:(ci + 1) * 128], tmpn[s], sr[:, :, ck], ALU.mult, [Btn[s], Bsr], [Bys[gs]])
                    if ci == gn - 1:
                        dma("sp", yT_dram[0:1024].rearrange("(h vc p) t -> p h vc t", p=128, vc=2)[:, hd, :, g0 * 128:(g0 + gn) * 128],
                            ystg[gs][:, :, 0:gn * 128], [Bys[gs]], [B_yd])
                        gstate[0] += 1

                for it in range(NCH + 2):
                    if it < NCH:
                        gA(it)
                    if 0 <= it - 1 < NCH:
                        gB(it - 1)
                    if 0 <= it - 2 < NCH:
                        gC(it - 2)
            P.barrier()
            A.release(m)

        def stage_moe(layer, x_sb, Bx, hT, Bh, tiles):
            jl = layer // 2
            m = A.mark()
            wr = A.alloc([8, 8], F32); Bwr = Buf("wr")
            dma("sp", wr, moe_wr[jl].rearrange("(kc p) e -> p kc e", p=128), [], [Bwr])

            def hf_cb(ti, t0, n, hf, Bhf):
                for sub in range(n // 128):
                    c = t0 // 128 + sub
                    for kc in range(KC):
                        mm(bank[5][:, c * 8:(c + 1) * 8], hf[:, kc, sub * 128:(sub + 1) * 128], wr[:, kc, :], kc == 0, kc == KC - 1, [Bhf, Bwr], [PB[5]])

            stage_norm(layer, gm2, 24, hT, Bh, x_sb=x_sb, Bx=Bx, tiles=tiles, hf_cb=hf_cb)
            c_lo = tiles[0][0] // 128
            ncu = NCH - c_lo
            L = A.alloc([NCH, 8], F32); L2 = A.alloc([NCH, 8], F32); eq1 = A.alloc([NCH, 8], F32); eq2 = A.alloc([NCH, 8], F32)
            gates = A.alloc([NCH, 8], F32)
            m1 = A.alloc([NCH], F32); m2 = A.alloc([NCH], F32); w1 = A.alloc([NCH], F32); w2 = A.alloc([NCH], F32)
            Bg = Buf("gate")
            sl = slice(c_lo, NCH)
            cp("dve", L[:, sl], bank[5][:, c_lo * 8:NCH * 8].rearrange("p (c e) -> p c e", e=8), [PB[5]], [Bg])
            P.op("dve", lambda e: e.tensor_reduce(out=m1[:, sl], in_=L[:, sl], axis=AX.X, op=ALU.max), [Bg], [Bg])
            tt("dve", eq1[:, sl], L[:, sl], m1[:, sl].unsqueeze(2).to_broadcast([128, ncu, 8]), ALU.is_equal, [Bg], [Bg])
            stt(L2[:, sl], eq1[:, sl], -1e30, L[:, sl], ALU.mult, ALU.add, [Bg], [Bg])
            P.op("dve", lambda e: e.tensor_reduce(out=m2[:, sl], in_=L2[:, sl], axis=AX.X, op=ALU.max), [Bg], [Bg])
            tt("dve", eq2[:, sl], L2[:, sl], m2[:, sl].unsqueeze(2).to_broadcast([128, ncu, 8]), ALU.is_equal, [Bg], [Bg])
            tt("dve", w2[:, sl], m2[:, sl], m1[:, sl], ALU.subtract, [Bg], [Bg])
            act(w2[:, sl], w2[:, sl], AF.Exp, [Bg], [Bg])
            ts("dve", w1[:, sl], w2[:, sl], 1.0, None, ALU.add, None, [Bg], [Bg])
            P.op("dve", lambda e: e.reciprocal(out=w1[:, sl], in_=w1[:, sl]), [Bg], [Bg])
            tt("dve", w2[:, sl], w2[:, sl], w1[:, sl], ALU.mult, [Bg], [Bg])
            tt("dve", gates[:, sl], eq1[:, sl], w1[:, sl].unsqueeze(2).to_broadcast([128, ncu, 8]), ALU.mult, [Bg], [Bg])
            tt("dve", eq2[:, sl], eq2[:, sl], w2[:, sl].unsqueeze(2).to_broadcast([128, ncu, 8]), ALU.mult, [Bg], [Bg])
            tt("dve", gates[:, sl], gates[:, sl], eq2[:, sl], ALU.add, [Bg], [Bg])
            Ge = [A.alloc([T], F32) for _ in range(2)]; BGe = [Buf(), Buf()]
            diag = [A.alloc([128], F32) for _ in range(2)]; Bdg = [Buf(), Buf()]
            slots = alloc_ffn_slots(True)
            dcnt = 0
            for e_ in range(NE):
                gs = e_ % 2
                for (t0, n, j) in tiles:
                    for sub in range(n // 128):
                        c = t0 // 128 + sub
                        ds_ = dcnt % 2
                        dcnt += 1
                        ts("dve", diag[ds_], cst[:, C_ID:C_ID + 128], gates[:, c, e_:e_ + 1], None, ALU.mult, None, [Bg, B_cst], [Bdg[ds_]])
                        mm(bank[4][:, sub * 128:(sub + 1) * 128], onesf, diag[ds_], True, True, [Bdg[ds_], B_small], [PB[4]])
                    cp("act", Ge[gs][:, t0:t0 + n], bank[4][:, 0:n], [PB[4]], [BGe[gs]])
                ffn_expert(layer, x_sb, Bx, hT, Bh, moe_wg[jl, e_], moe_wu[jl, e_], moe_wd[jl, e_], D_FFE, tiles, slots, Ge=Ge[gs], BGe=BGe[gs])
            ffn_flush(slots)
            P.barrier()
            A.release(m)

        def stage_final(x_sb, Bx):
            m = A.mark()
            sq = [A.alloc([8, 512], BF16) for _ in range(2)]; Bsq = [Buf(), Buf()]
            rs = [A.alloc([512], F32) for _ in range(2)]; Brs = [Buf(), Buf()]
            tmp = [A.alloc([8, 512], F32) for _ in range(2)]; Btmp = [Buf(), Buf()]
            for ti, (t0, n, j) in enumerate(TILES[1:]):
                s = ti % 2
                xt = x_sb[:, :, t0:t0 + n]
                act(sq[s], xt, AF.Square, Bx[TG[t0]], [Bsq[s]])
                pb = 6 + s
                for c in range(KC):
                    mm(bank[pb], onesb, sq[s][:, c, :], c == 0, c == KC - 1, [Bsq[s], B_small], [PB[pb]])
                ts("dve", rs[s], bank[pb], 1.0 / D, EPS, ALU.mult, ALU.add, [PB[pb]], [Brs[s]])
                act(rs[s], rs[s], AF.Sqrt, [Brs[s]], [Brs[s]])
                P.op("dve", lambda e, s=s: e.reciprocal(out=rs[s], in_=rs[s]), [Brs[s]], [Brs[s]])
                tt("dve", tmp[s], xt, rs[s].unsqueeze(1).to_broadcast([128, 8, 512]), ALU.mult, Bx[TG[t0]] + [Brs[s]], [Btmp[s]])
                tt("pool", tmp[s], tmp[s], fgt.unsqueeze(2).to_broadcast([128, 8, 512]), ALU.mult, [Btmp[s], B_small], [Btmp[s]])
                dma("sp", outT.rearrange("(c p) t -> p c t", p=128)[:, :, t0 - NCTX:t0 - NCTX + n], tmp[s], [Btmp[s]], [B_out])
            P.barrier()
            A.release(m)

        stage_adaln()
        for layer in range(n_layers):
            last = layer == 3
            jl = layer // 2
            is_ret = layer % 2 == 0
            xsrc = xT if layer == 0 else x_dram
            Bxs = Buf("xT") if layer == 0 else B_xd
            m0 = A.mark()
            hT = A.alloc([8, T], BF16)
            Bh = [Buf("h%d" % i) for i in range(5)]
            stage_norm(layer, gm1, 0, hT, Bh, xsrc=xsrc, Bxs=Bxs)
            if is_ret:
                stage_retention(layer, hT, Bh)
                kco, w_out_ap = 16, ret_w_out[jl]
            else:
                stage_gla(layer, hT, Bh, last)
                kco, w_out_ap = 8, gla_w_out[jl]
            A.release(m0)
            tiles = TILES[1:] if last else TILES
            x_sb = A.alloc([8, T], F32); Bx = [[Buf("x%d_%d" % (a, b_)) for b_ in range(8)] for a in range(5)]
            for tg, (t0, n, j) in enumerate(TILES):
                dma("sp", x_sb[:, :, t0:t0 + n], xsrc.rearrange("(c p) t -> p c t", p=128)[:, :, t0:t0 + n], [Bxs], Bx[tg])
            stage_outproj(layer, x_sb, Bx, w_out_ap, kco, tiles)
            hT = A.alloc([8, T], BF16)
            Bh = [Buf("h%d" % i) for i in range(5)]
            if is_ret:
                stage_norm(layer, gm2, 24, hT, Bh[5 - len(tiles):], x_sb=x_sb, Bx=Bx, tiles=tiles)
                m1 = A.mark()
                slots = alloc_ffn_slots(False)
                ffn_expert(layer, x_sb, Bx, hT[:, :, :] if not last else hT, Bh[5 - len(tiles):], ffn_wg[jl], ffn_wu[jl], ffn_wd[jl], D_FF, tiles, slots)
                ffn_flush(slots)
                P.barrier()
                A.release(m1)
            else:
                stage_moe(layer, x_sb, Bx, hT, Bh[5 - len(tiles):], tiles)
            if layer == n_layers - 1:
                if layer == 3:
                    stage_final(x_sb, Bx)
                else:
                    for (t0, n, j) in TILES[1:]:
                        dma("sp", outT.rearrange("(c p) t -> p c t", p=128)[:, :, t0 - NCTX:t0 - NCTX + n], x_sb[:, :, t0:t0 + n], Bx[TG[t0]], [B_out])
                    if "ctx" in tap_out:
                        dma("sp", tap_out["ctx"].rearrange("(c p) t -> p c t", p=128), x_sb[:, :, 0:NCTX], Bx[0], [B_out])
            else:
                for (t0, n, j) in TILES:
                    dma("sp", x_dram.rearrange("(c p) t -> p c t", p=128)[:, :, t0:t0 + n], x_sb[:, :, t0:t0 + n], Bx[TG[t0]], [B_xd])
            P.barrier()
            A.release(m0)
        P.barrier()
        P.emit()
    return nc


def _fm(vec, nch):
    return np.ascontiguousarray(np.asarray(vec, np.float32).reshape(nch, 128).T)


def prep_shared(inp):
    sh = {}
    sh["cst"] = host_consts()
    sh["ada_w"] = np.ascontiguousarray(inp["ada_w"], dtype=np.float32)
    sh["ada_b_fm"] = np.ascontiguousarray(np.stack([_fm(inp["ada_b"][i], 48) for i in range(4)], axis=1))
    sh["ngm_fm"] = np.ascontiguousarray(np.stack([_fm(inp["norm_mix_g"][i], 8) for i in range(4)], axis=1))
    sh["ngf_fm"] = np.ascontiguousarray(np.stack([_fm(inp["norm_ffn_g"][i], 8) for i in range(4)], axis=1))
    sh["fg_fm"] = _fm(inp["final_g"], 8)
    sh["ret_w_in"] = np.ascontiguousarray(inp["ret_w_in"], dtype=np.float32)
    sh["ret_ld"] = np.ascontiguousarray(np.asarray(inp["ret_log_decay"], np.float32).reshape(2, 8))
    sh["ret_gnw_fm"] = np.ascontiguousarray(np.stack([_fm(inp["ret_gn_w"][i], 16) for i in range(2)], axis=1))
    sh["ret_gnb_fm"] = np.ascontiguousarray(np.stack([_fm(inp["ret_gn_b"][i], 16) for i in range(2)], axis=1))
    sh["ret_w_out"] = np.ascontiguousarray(inp["ret_w_out"], dtype=np.float32)
    sh["gla_w_in"] = np.ascontiguousarray(inp["gla_w_in"], dtype=np.float32)
    sh["gla_w_gate_up"] = np.ascontiguousarray(inp["gla_w_gate_up"], dtype=np.float32)
    bg = np.asarray(inp["gla_b_gate"], np.float32)
    sh["gla_bg_fm"] = np.ascontiguousarray(bg.reshape(2, 2, 4, 128).transpose(3, 0, 1, 2))
    sh["gla_ng_fm"] = np.ascontiguousarray(np.stack([_fm(inp["gla_norm_g"][i], 8) for i in range(2)], axis=1))
    sh["gla_w_out"] = np.ascontiguousarray(inp["gla_w_out"], dtype=np.float32)
    for k in ("ffn_w_gate", "ffn_w_up", "ffn_w_down", "moe_w_router", "moe_w_gate", "moe_w_up", "moe_w_down"):
        sh[k] = np.ascontiguousarray(inp[k], dtype=np.float32)
    return sh


def prep_core(inp, b, shared):
    m = dict(shared)
    m["xT"] = np.ascontiguousarray(np.concatenate([inp["ctx"][b], inp["x"][b]], axis=0).T.astype(np.float32))
    m["cc"] = np.ascontiguousarray(np.stack([_fm(inp["c"][b], 8), _fm(inp["c_ctx"], 8)], axis=1))
    return m


_NC_CACHE = {}


def kernel(**inputs):
    inp = {k: np.asarray(v) for k, v in inputs.items()}
    if "full" not in _NC_CACHE:
        _NC_CACHE["full"] = build(4)
    nc = _NC_CACHE["full"]
    shared = prep_shared(inp)
    in_maps = [prep_core(inp, b, shared) for b in range(8)]
    res = run_bass_kernel_spmd(nc, in_maps, core_ids=list(range(8)))
    out = np.stack([np.ascontiguousarray(res.results[b]["outT"].T) for b in range(8)], axis=0)
    return out.astype(np.float32)
```

```python
import contextlib
import numpy as np
import concourse.bass as bass
import concourse.mybir as mybir
from concourse.bass_utils import run_bass_kernel_spmd

F32 = mybir.dt.float32
BF16 = mybir.dt.bfloat16
AF = mybir.ActivationFunctionType
ALU = mybir.AluOpType
AX = mybir.AxisListType

ENGS = ("pe", "act", "dve", "pool", "sp")
NSLOT = 8
QSLOT = {"pool": 5, "sp": 8, "act": 8, "pe": 8, "dve": 8}
SAME_ENGINE_SYNC = True
import os as _os
OPT = set(_os.environ.get("KOPT", "war,prefetch").split(","))

D = 1024
KC = 8
NCTX = 256
NLAT = 2048
T = NCTX + NLAT
NCH = T // 128
EPS = 1e-6
TILES = [(0, 256, 1)] + [(256 + 512 * i, 512, 0) for i in range(4)]
TG = {t[0]: i for i, t in enumerate(TILES)}
D_FF = 2816
D_FFE = 3584
NE = 8


class Buf:
    __slots__ = ("name", "w", "r", "rd")

    def __init__(self, name=""):
        self.name = name
        self.w = None
        self.r = {}
        self.rd = []


class Ins:
    __slots__ = ("eng", "fn", "deps", "sig", "sem", "semval", "dma")

    def __init__(self, eng, fn, dma):
        self.eng = eng
        self.fn = fn
        self.deps = []
        self.sig = False
        self.sem = None
        self.semval = 0
        self.dma = dma


class Prog:
    def __init__(self, nc):
        self.nc = nc
        self.q = {e: [] for e in ENGS}
        self.dma_cnt = {e: 0 for e in ENGS}
        self.slot_last = {e: [None] * NSLOT for e in ENGS}
        self.slot_uses = {e: [0] * NSLOT for e in ENGS}

    def op(self, eng, fn, reads=(), writes=(), dma=False):
        ins = Ins(eng, fn, dma)
        deps = {}

        def add(d, war):
            if d is None:
                return
            if d.eng == eng and not d.dma and not dma:
                if eng in ("pe", "sp"):
                    return
                if not SAME_ENGINE_SYNC:
                    return
                if war and "war" not in OPT:
                    return
            deps[id(d)] = d

        for b in reads:
            add(b.w, False)
        for b in writes:
            add(b.w, False)
            for d in b.r.values():
                add(d, True)
            for d in b.rd:
                add(d, True)
        if dma:
            k = self.dma_cnt[eng] % QSLOT[eng]
            self.dma_cnt[eng] += 1
            prev = self.slot_last[eng][k]
            if prev is not None:
                deps[id(prev)] = prev
            self.slot_last[eng][k] = ins
            self.slot_uses[eng][k] += 1
            ins.sem = ("dma", eng, k)
            ins.semval = 16 * self.slot_uses[eng][k]
            ins.sig = True
        ins.deps = list(deps.values())
        for d in ins.deps:
            d.sig = True
        for b in reads:
            if dma:
                b.rd.append(ins)
            else:
                b.r[eng] = ins
        for b in writes:
            b.w = ins
            b.r = {}
            b.rd = []
        self.q[eng].append(ins)
        return ins

    def barrier(self):
        lasts = []
        for e in ENGS:
            for ins in reversed(self.q[e]):
                if not ins.dma and ins.fn is not None:
                    lasts.append(ins)
                    break
            for k in range(NSLOT):
                if self.slot_last[e][k] is not None:
                    lasts.append(self.slot_last[e][k])
        for e in ENGS:
            ins = Ins(e, None, False)
            ins.deps = list(lasts)
            for d in ins.deps:
                d.sig = True
            self.q[e].append(ins)

    def emit(self):
        nc = self.nc
        with contextlib.ExitStack() as st:
            esem = {e: st.enter_context(nc.semaphore("s_" + e)) for e in ENGS}
            dsem = {}
            for e in ENGS:
                for k in range(NSLOT):
                    if self.slot_uses[e][k]:
                        dsem[("dma", e, k)] = st.enter_context(nc.semaphore("d_%s%d" % (e, k)))
            for e in ENGS:
                c = 0
                for ins in self.q[e]:
                    if ins.dma or ins.fn is None:
                        continue
                    if ins.sig:
                        c += 1
                        ins.sem = ("eng", e)
                        ins.semval = c

            def semof(key):
                return esem[key[1]] if key[0] == "eng" else dsem[key]

            block = st.enter_context(nc.Block())

            def run(ename, eng):
                waited = {}
                for ins in self.q[ename]:
                    for d in ins.deps:
                        if waited.get(d.sem, 0) >= d.semval:
                            continue
                        eng.wait_ge(semof(d.sem), d.semval)
                        waited[d.sem] = d.semval
                    if ins.fn is None:
                        continue
                    r = ins.fn(eng)
                    if ins.sig:
                        r.then_inc(semof(ins.sem), 16 if ins.dma else 1)

            @block.tensor
            def _(eng):
                run("pe", eng)

            @block.scalar
            def _(eng):
                run("act", eng)

            @block.vector
            def _(eng):
                run("dve", eng)

            @block.gpsimd
            def _(eng):
                run("pool", eng)

            @block.sync
            def _(eng):
                run("sp", eng)


class Arena:
    def __init__(self, big, nbytes):
        self.big = big
        self.nbytes = nbytes
        self.off = 0

    def alloc(self, shape, dt):
        size = 4 if dt == F32 else 2
        n = int(np.prod(shape))
        nb = (n * size + 63) // 64 * 64
        assert self.off + nb <= self.nbytes, ("SBUF arena overflow", self.off, nb, self.nbytes)
        ap = self.big[:, self.off // 2:(self.off + n * size) // 2]
        self.off += nb
        if dt == F32:
            ap = ap.bitcast(F32)
        if len(shape) == 2:
            ap = ap.rearrange("p (a b) -> p a b", a=shape[0])
        elif len(shape) == 3:
            ap = ap.rearrange("p (a b c) -> p a b c", a=shape[0], b=shape[1])
        elif len(shape) == 4:
            ap = ap.rearrange("p (a b c d) -> p a b c d", a=shape[0], b=shape[1], c=shape[2])
        return ap

    def mark(self):
        return self.off

    def release(self, m):
        self.off = m


C_ID, C_DPOS, C_DNEG, C_LO, C_UP, C_I1, C_IB, C_PF, C_PB, C_ROPE = 0, 128, 256, 384, 512, 640, 768, 896, 897, 898
C_ROPEK = 898 + 192
C_N = 898 + 384


def host_consts():
    c = np.zeros((128, C_N), np.float32)
    p = np.arange(128)[:, None].astype(np.float32)
    i = np.arange(128)[None, :].astype(np.float32)
    c[:, C_ID:C_ID + 128] = np.eye(128)
    c[:, C_DPOS:C_DPOS + 128] = np.maximum(i - p, 0)
    c[:, C_DNEG:C_DNEG + 128] = np.maximum(p - i, 0)
    c[:, C_LO:C_LO + 128] = (i >= p)
    c[:, C_UP:C_UP + 128] = (i <= p)
    c[:, C_I1:C_I1 + 128] = i + 1
    c[:, C_IB:C_IB + 128] = 128 - i
    c[:, C_PF] = 127 - p[:, 0]
    c[:, C_PB] = p[:, 0]
    half = 64
    freqs = (10000.0 ** (-np.arange(half, dtype=np.float32) / half)).astype(np.float32)
    fr = np.concatenate([freqs, freqs])[:, None]
    rows = np.arange(32, dtype=np.float32)[None, :]
    cols = np.arange(64, dtype=np.float32)[None, :]
    c[:, C_ROPE:C_ROPE + 32] = np.cos(rows * fr)
    sgn = np.where(np.arange(128) < 64, -1.0, 1.0).astype(np.float32)[:, None]
    c[:, C_ROPE + 32:C_ROPE + 64] = np.sin(rows * fr) * sgn
    c[:, C_ROPE + 64:C_ROPE + 128] = np.cos(cols * fr)
    c[:, C_ROPE + 128:C_ROPE + 192] = np.sin(cols * fr) * sgn
    c[:, C_ROPEK:C_ROPEK + 192] = c[:, C_ROPE:C_ROPE + 192] * np.float32(0.0625)
    return c


def build(n_layers=4, taps=()):
    nc = bass.Bass("TRN2", target_bir_lowering=False)
    P = Prog(nc)

    def din(name, shape, dt=F32):
        return nc.dram_tensor(name, list(shape), dt, kind="ExternalInput").ap()

    xT = din("xT", [D, T])
    cc = din("cc", [128, 2, 8])
    cst_d = din("cst", [128, C_N])
    ada_w = din("ada_w", [4, D, 6 * D])
    ada_b = din("ada_b_fm", [128, 4, 48])
    ngm_d = din("ngm_fm", [128, 4, 8])
    ngf_d = din("ngf_fm", [128, 4, 8])
    fg_d = din("fg_fm", [128, 8])
    ret_w_in = din("ret_w_in", [2, D, 6144])
    ret_ld = din("ret_ld", [2, 8])
    ret_gnw = din("ret_gnw_fm", [128, 2, 16])
    ret_gnb = din("ret_gnb_fm", [128, 2, 16])
    ret_w_out = din("ret_w_out", [2, 2048, D])
    gla_w_in = din("gla_w_in", [2, D, 3104])
    gla_wgu = din("gla_w_gate_up", [2, 2, 16, 512])
    gla_bg = din("gla_bg_fm", [128, 2, 2, 4])
    gla_ng = din("gla_ng_fm", [128, 2, 8])
    gla_w_out = din("gla_w_out", [2, D, D])
    ffn_wg = din("ffn_w_gate", [2, D, D_FF])
    ffn_wu = din("ffn_w_up", [2, D, D_FF])
    ffn_wd = din("ffn_w_down", [2, D_FF, D])
    moe_wr = din("moe_w_router", [2, D, NE])
    moe_wg = din("moe_w_gate", [2, NE, D, D_FFE])
    moe_wu = din("moe_w_up", [2, NE, D, D_FFE])
    moe_wd = din("moe_w_down", [2, NE, D_FFE, D])

    outT = nc.dram_tensor("outT", [D, NLAT], F32, kind="ExternalOutput").ap()
    x_dram = nc.dram_tensor("x_scr", [D, T], F32, kind="Internal").ap()
    yT_dram = nc.dram_tensor("yT_scr", [2048, T], BF16, kind="Internal").ap()
    sb_dram = nc.dram_tensor("sb_scr", [NCH, 128, 1024], BF16, kind="Internal").ap()
    tap_out = {}
    for (name, shape) in taps:
        tap_out[name] = nc.dram_tensor("tap_" + name, list(shape), F32, kind="ExternalOutput").ap()

    ARENA_BYTES = 207 * 1024
    with contextlib.ExitStack() as st:
        big = st.enter_context(nc.sbuf_tensor("big", [128, ARENA_BYTES // 2], BF16))
        psum = st.enter_context(nc.psum_tensor("psum", [128, 8, 512], F32))
        A = Arena(big, ARENA_BYTES)
        PB = [Buf("bank%d" % i) for i in range(8)]
        bank = [psum[:, i, :] for i in range(8)]
        B_xd = Buf("x_dram")
        B_yd = Buf("yT_dram")
        B_sbd = [Buf("sbd%d" % i) for i in range(NCH)]
        B_out = Buf("out")

        def dma(eng, out, in_, reads, writes):
            return P.op(eng, lambda e: e.dma_start(out=out, in_=in_), reads, writes, dma=True)

        def mm(out, lhsT, rhs, start, stop, reads, writes):
            return P.op("pe", lambda e: e.matmul(out, lhsT, rhs, start=start, stop=stop), reads, writes)

        def tr(out, in_, ident, reads, writes):
            return P.op("pe", lambda e: e.transpose(out, in_, ident), reads, writes)

        def act(out, in_, func, reads, writes, **kw):
            return P.op("act", lambda e: e.activation(out=out, in_=in_, func=func, **kw), reads, writes)

        def tt(eng, out, in0, in1, op, reads, writes):
            return P.op(eng, lambda e: e.tensor_tensor(out=out, in0=in0, in1=in1, op=op), reads, writes)

        def ts(eng, out, in0, s1, s2, op0, op1, reads, writes):
            if s2 is None:
                return P.op(eng, lambda e: e.tensor_scalar(out=out, in0=in0, scalar1=s1, scalar2=None, op0=op0), reads, writes)
            return P.op(eng, lambda e: e.tensor_scalar(out=out, in0=in0, scalar1=s1, scalar2=s2, op0=op0, op1=op1), reads, writes)

        def stt(out, in0, scalar, in1, op0, op1, reads, writes):
            return P.op("dve", lambda e: e.scalar_tensor_tensor(out=out, in0=in0, scalar=scalar, in1=in1, op0=op0, op1=op1), reads, writes)

        def cp(eng, out, in_, reads, writes):
            if eng == "act":
                return P.op("act", lambda e: e.copy(out=out, in_=in_), reads, writes)
            return P.op(eng, lambda e: e.tensor_copy(out=out, in_=in_), reads, writes)

        def tap(name, src, reads):
            if name in tap_out:
                dma("sp", tap_out[name], src, reads, [B_out])

        cst = A.alloc([C_N], F32); B_cst = Buf("cst")
        identb = A.alloc([128], BF16)
        onesb = A.alloc([128], BF16)
        onesf = A.alloc([128], F32)
        epsc = A.alloc([1], F32)
        modt = A.alloc([4, 48, 2], F32); B_mod = Buf("mod")
        gm1 = A.alloc([4, 8, 2], F32)
        gm2 = A.alloc([4, 8, 2], F32)
        adab = A.alloc([4, 48], F32)
        ngm = A.alloc([4, 8], F32)
        ngf = A.alloc([4, 8], F32)
        fgt = A.alloc([8], F32)
        B_small = Buf("small")
        dma("sp", cst, cst_d, [], [B_cst])
        dma("sp", adab, ada_b, [], [B_small])
        dma("sp", ngm, ngm_d, [], [B_small])
        dma("sp", ngf, ngf_d, [], [B_small])
        dma("sp", fgt, fg_d, [], [B_small])
        cp("dve", identb, cst[:, C_ID:C_ID + 128], [B_cst], [B_small])
        P.op("dve", lambda e: e.memset(onesb, 1.0), [], [B_small])
        P.op("dve", lambda e: e.memset(onesf, 1.0), [], [B_small])
        P.op("dve", lambda e: e.memset(epsc, EPS), [], [B_small])
        P.barrier()
        base_mark = A.mark()

        def stage_adaln():
            m = A.mark()
            cs = A.alloc([2, 8], F32)
            csb = A.alloc([8, 2], BF16)
            wsl = [A.alloc([8, 1536], BF16) for _ in range(2)]
            Bw = [Buf("aw0"), Buf("aw1")]
            Bc = Buf("cs")
            tmpm = A.alloc([8, 2], F32)
            dma("sp", cs, cc, [], [Bc])
            act(csb, cs.rearrange("p j c -> p c j"), AF.Silu, [Bc], [Bc])
            for i in range(n_layers):
                pb = i % 2
                for cb in range(4):
                    s = (i * 4 + cb) % 2
                    dma("pool", wsl[s], ada_w[i].rearrange("(kc p) n -> p kc n", p=128)[:, :, cb * 1536:(cb + 1) * 1536], [], [Bw[s]])
                    for j in range(12):
                        col = (cb * 12 + j) * 2
                        for kc in range(KC):
                            mm(bank[pb][:, col:col + 2], wsl[s][:, kc, j * 128:(j + 1) * 128], csb[:, kc, :], kc == 0, kc == KC - 1, [Bw[s], Bc], [PB[pb]])
                tt("dve", modt[:, i], bank[pb][:, 0:96].rearrange("p (q j) -> p q j", j=2), adab[:, i, :].unsqueeze(2).to_broadcast([128, 48, 2]), ALU.add, [PB[pb], B_small], [B_mod])
                for (gm, ng, off) in ((gm1, ngm, 8), (gm2, ngf, 32)):
                    ts("dve", tmpm, modt[:, i, off:off + 8, :], 1.0, None, ALU.add, None, [B_mod], [Bc])
                    tt("dve", gm[:, i], tmpm, ng[:, i, :].unsqueeze(2).to_broadcast([128, 8, 2]), ALU.mult, [Bc, B_small], [B_mod])
            P.barrier()
            A.release(m)

        def stage_norm(layer, gm, sh_off, hT, Bh, x_sb=None, Bx=None, xsrc=None, Bxs=None, tiles=TILES, hf_cb=None):
            m = A.mark()
            stg = None
            if x_sb is None:
                stg = [A.alloc([8, 512], F32) for _ in range(2)]
                Bst = [Buf(), Buf()]
            sq = [A.alloc([8, 512], BF16) for _ in range(2)]
            Bsq = [Buf(), Buf()]
            rs = [A.alloc([512], F32) for _ in range(2)]
            Brs = [Buf(), Buf()]
            tmp = [A.alloc([8, 512], F32) for _ in range(2)]
            Btmp = [Buf(), Buf()]
            if hf_cb is not None:
                hf = [A.alloc([8, 512], F32) for _ in range(2)]
                Bhf = [Buf(), Buf()]
            info = {}

            def front(ti):
                (t0, n, j) = tiles[ti]
                s = ti % 2
                if x_sb is None:
                    dma("sp", stg[s][:, :, 0:n], xsrc.rearrange("(c p) t -> p c t", p=128)[:, :, t0:t0 + n], [Bxs], [Bst[s]])
                    xt = stg[s][:, :, 0:n]
                    Bxl = [Bst[s]]
                else:
                    xt = x_sb[:, :, t0:t0 + n]
                    Bxl = Bx[TG[t0]]
                info[ti] = (xt, Bxl)
                act(sq[s][:, :, 0:n], xt, AF.Square, Bxl, [Bsq[s]])
                pb = 6 + s
                for c in range(KC):
                    mm(bank[pb][:, 0:n], onesb, sq[s][:, c, 0:n], c == 0, c == KC - 1, [Bsq[s], B_small], [PB[pb]])

            def frontB(ti):
                (t0, n, j) = tiles[ti]
                s = ti % 2
                pb = 6 + s
                act(rs[s][:, 0:n], bank[pb][:, 0:n], AF.Ln, [PB[pb]], [Brs[s]], scale=1.0 / D, bias=epsc[:, 0:1])
                act(rs[s][:, 0:n], rs[s][:, 0:n], AF.Exp, [Brs[s]], [Brs[s]], scale=-0.5)

            def back(ti):
                (t0, n, j) = tiles[ti]
                s = ti % 2
                (xt, Bxl) = info[ti]
                tt("dve", tmp[s][:, :, 0:n], xt, rs[s][:, 0:n].unsqueeze(1).to_broadcast([128, 8, n]), ALU.mult, Bxl + [Brs[s]], [Btmp[s]])
                for c in range(KC):
                    sc_ap = gm[:, layer, c, j:j + 1]
                    bi_ap = modt[:, layer, sh_off + c, j:j + 1]
                    if hf_cb is None:
                        if c < 4:
                            act(hT[:, c, t0:t0 + n], tmp[s][:, c, 0:n], AF.Identity, [Btmp[s], B_mod], [Bh[ti]], scale=sc_ap, bias=bi_ap)
                        else:
                            ts("pool", hT[:, c, t0:t0 + n], tmp[s][:, c, 0:n], sc_ap, bi_ap, ALU.mult, ALU.add, [Btmp[s], B_mod], [Bh[ti]])
                    else:
                        act(hT[:, c, t0:t0 + n], tmp[s][:, c, 0:n], AF.Identity, [Btmp[s], B_mod], [Bh[ti]], scale=sc_ap, bias=bi_ap)
                        ts("dve" if c < 4 else "pool", hf[s][:, c, 0:n], tmp[s][:, c, 0:n], sc_ap, bi_ap, ALU.mult, ALU.add, [Btmp[s], B_mod], [Bhf[s]])
                if hf_cb is not None:
                    hf_cb(ti, t0, n, hf[s], Bhf[s])

            front(0)
            frontB(0)
            for ti in range(len(tiles)):
                if ti + 1 < len(tiles):
                    front(ti + 1)
                back(ti)
                if ti + 1 < len(tiles):
                    frontB(ti + 1)
            P.barrier()
            A.release(m)

        def stage_retention(layer, hT, Bh):
            jl = layer // 2
            m = A.mark()
            ld = A.alloc([8], F32); lg = A.alloc([8], F32); cdec = A.alloc([8], F32)
            maskT = A.alloc([4, 128], F32)
            decf = A.alloc([4, 128], F32); decb = A.alloc([4, 128], F32)
            kdecf = A.alloc([4], F32); kdecb = A.alloc([4], F32)
            mt = A.alloc([128], F32)
            gnw = A.alloc([16], F32); gnb = A.alloc([16], F32)
            Bd = Buf("dec")
            dma("sp", ld, ret_ld[jl:jl + 1, :].partition_broadcast(128), [], [Bd])
            dma("sp", gnw, ret_gnw[:, jl, :], [], [Bd])
            dma("sp", gnb, ret_gnb[:, jl, :], [], [Bd])
            act(lg, ld, AF.Exp, [Bd], [Bd])
            ts("dve", lg, lg, -1.0, None, ALU.mult, None, [Bd], [Bd])
            ts("dve", cdec, lg, 128.0, None, ALU.mult, None, [Bd], [Bd])
            act(cdec, cdec, AF.Exp, [Bd], [Bd])
            for hd in range(4):
                f, b = hd, 4 + hd
                act(maskT[:, hd], cst[:, C_DPOS:C_DPOS + 128], AF.Exp, [Bd, B_cst], [Bd], scale=lg[:, f:f + 1])
                tt("dve", maskT[:, hd], maskT[:, hd], cst[:, C_LO:C_LO + 128], ALU.mult, [Bd], [Bd])
                act(mt, cst[:, C_DNEG:C_DNEG + 128], AF.Exp, [Bd, B_cst], [Bd], scale=lg[:, b:b + 1])
                tt("dve", mt, mt, cst[:, C_UP:C_UP + 128], ALU.mult, [Bd], [Bd])
                tt("dve", maskT[:, hd], maskT[:, hd], mt, ALU.add, [Bd], [Bd])
                act(decf[:, hd], cst[:, C_I1:C_I1 + 128], AF.Exp, [Bd, B_cst], [Bd], scale=lg[:, f:f + 1])
                act(decb[:, hd], cst[:, C_IB:C_IB + 128], AF.Exp, [Bd, B_cst], [Bd], scale=lg[:, b:b + 1])
                act(kdecf[:, hd:hd + 1], cst[:, C_PF:C_PF + 1], AF.Exp, [Bd, B_cst], [Bd], scale=lg[:, f:f + 1])
                act(kdecb[:, hd:hd + 1], cst[:, C_PB:C_PB + 1], AF.Exp, [Bd, B_cst], [Bd], scale=lg[:, b:b + 1])
            wq = A.alloc([8, 256], BF16); wqr = A.alloc([8, 256], BF16)
            wk = A.alloc([8, 256], BF16); wkr = A.alloc([8, 256], BF16)
            wv = A.alloc([8, 512], BF16); wg = A.alloc([8, 512], BF16)
            Bwq, Bwk, Bwv, Bwg = Buf("wq"), Buf("wk"), Buf("wv"), Buf("wg")
            qT = A.alloc([2, T], BF16)
            qfc = [A.alloc([2, 128], BF16) for _ in range(2)]; qbc = [A.alloc([2, 128], BF16) for _ in range(2)]; Bqc = [Buf(), Buf()]
            kT = A.alloc([2, T], BF16)
            kdf = A.alloc([NCH, 256], BF16); kdb = A.alloc([NCH, 256], BF16)
            v = A.alloc([NCH, 512], BF16)
            sg = A.alloc([4, T], BF16)
            Bq, Bk, Bkd, Bv, Bsg = Buf("q"), Buf("k"), Buf("kd"), Buf("v"), Buf("sg")
            t1 = [A.alloc([512], F32)] * 2
            t2 = [A.alloc([512], F32)] * 2
            qr = [A.alloc([512], F32) for _ in range(2)]
            Bt1, Bt2, Bqr = [Buf()] * 2, [Buf()] * 2, [Buf(), Buf()]
            Sf = A.alloc([2, 512], F32); Sb = A.alloc([2, 512], F32)
            Sfb = [A.alloc([2, 512], BF16) for _ in range(2)]; Sbb = [A.alloc([2, 512], BF16) for _ in range(2)]
            Sbin = [A.alloc([2, 512], BF16) for _ in range(2)]
            BSf, BSb, BSfb = Buf("Sf"), Buf("Sb"), [Buf("Sfb0"), Buf("Sfb1")]
            BSbb, BSbin = [Buf(), Buf()], [Buf(), Buf()]
            PT = [A.alloc([128], BF16) for _ in range(2)]; BPT = [Buf(), Buf()]
            osb = [A.alloc([512], F32) for _ in range(2)]; Bosb = [Buf(), Buf()]
            junk = t1[0]; Bjunk = Bt1[0]
            stat = [A.alloc([8], F32) for _ in range(2)]; Bstat = [Buf(), Buf()]
            onb = [A.alloc([512], BF16) for _ in range(2)]; Bonb = [Buf(), Buf()]
            tmpn = [A.alloc([4, 128], F32) for _ in range(2)]; Btn = [Buf(), Buf()]; Btnv = [[Buf() for _ in range(4)] for _ in range(2)]
            ystg = [A.alloc([4, 512], BF16) for _ in range(2)]; Bys = [Buf(), Buf()]
            w_in = ret_w_in[jl].rearrange("(kc p) n -> p kc n", p=128)
            psT = [psum[:, 4 + i, :].bitcast(BF16) for i in range(2)]
            psT2 = [psum[:, 6 + i, :].bitcast(BF16) for i in range(2)]

            def rot_weights(w, wr_, Bw):
                w4 = w.rearrange("p k (b h x) -> p (k b) h x", b=2, h=2)
                r4 = wr_.rearrange("p k (b h x) -> p (k b) h x", b=2, h=2)
                cp("dve", r4[:, :, 0, :], w4[:, :, 1, :], [Bw], [Bw])
                cp("dve", r4[:, :, 1, :], w4[:, :, 0, :], [Bw], [Bw])

            def rope_evac(dst_list, b_main, b_rot, dc, t0, n, j, s, Bdst, hd, with_decay, RB=C_ROPE, csc=1.0):
                if j == 0:
                    r0 = (t0 - NCTX) // 64
                    nr = n // 64
                    if dc == 0:
                        cosv = cst[:, RB + r0:RB + r0 + nr].unsqueeze(2).to_broadcast([128, nr, 64])
                        sinv = cst[:, RB + 32 + r0:RB + 32 + r0 + nr].unsqueeze(2).to_broadcast([128, nr, 64])
                    else:
                        cosv = cst[:, RB + 64:RB + 128].unsqueeze(1).to_broadcast([128, nr, 64])
                        sinv = cst[:, RB + 128:RB + 192].unsqueeze(1).to_broadcast([128, nr, 64])
                    v3 = lambda ap: ap.rearrange("p (r c) -> p r c", c=64)
                    tt("dve", v3(t1[s][:, 0:n]), v3(bank[b_main][:, 0:n]), cosv, ALU.mult, [PB[b_main], B_cst], [Bt1[s]])
                    tt("dve", v3(t2[s][:, 0:n]), v3(bank[b_rot][:, 0:n]), sinv, ALU.mult, [PB[b_rot], B_cst], [Bt2[s]])
                    tt("pool", qr[s][:, 0:n], t1[s][:, 0:n], t2[s][:, 0:n], ALU.add, [Bt1[s], Bt2[s]], [Bqr[s]])
                else:
                    act(qr[s][:, 0:n], bank[b_main][:, 0:n], AF.Copy, [PB[b_main]], [Bqr[s]], scale=csc)
                cp("act", dst_list[0][:, dc, t0:t0 + n], qr[s][:, 0:n], [Bqr[s]], [Bdst])
                if with_decay:
                    nck = n // 128
                    q3 = qr[s][:, 0:n].rearrange("p (c i) -> p c i", i=128)
                    tt("pool", dst_list[1][:, dc, t0:t0 + n].rearrange("p (c i) -> p c i", i=128), q3,
                       decf[:, hd].unsqueeze(1).to_broadcast([128, nck, 128]), ALU.mult, [Bqr[s], Bd], [Bdst])
                    tt("dve", dst_list[2][:, dc, t0:t0 + n].rearrange("p (c i) -> p c i", i=128), q3,
                       decb[:, hd].unsqueeze(1).to_broadcast([128, nck, 128]), ALU.mult, [Bqr[s], Bd], [Bdst])

            def load_head_weights(hd):
                dma("pool", wq, w_in[:, :, hd * 256:(hd + 1) * 256], [], [Bwq])
                rot_weights(wq, wqr, Bwq)
                dma("pool", wk, w_in[:, :, 3072 + hd * 256:3072 + (hd + 1) * 256], [], [Bwk])
                rot_weights(wk, wkr, Bwk)
                dma("pool", wv, w_in[:, :, 4096 + hd * 512:4096 + (hd + 1) * 512], [], [Bwv])
                dma("pool", wg, w_in[:, :, 1024 + hd * 512:1024 + (hd + 1) * 512], [], [Bwg])

            load_head_weights(0)
            for hd in range(4):
                if hd > 0 and "prefetch" not in OPT:
                    load_head_weights(hd)
                cnt = 0
                for ti, (t0, n, j) in enumerate(TILES):
                    for (w, wr_, Bw, dsts, Bdst, wd, RB, csc) in ((wq, wqr, Bwq, (qT,), Bq, False, C_ROPE, 1.0), (wk, wkr, Bwk, (kT,), Bk, False, C_ROPEK, 0.0625)):
                        for dc in range(2):
                            bm, br = 0 + (cnt % 2) * 2, 1 + (cnt % 2) * 2
                            for kc in range(KC):
                                mm(bank[bm][:, 0:n], w[:, kc, dc * 128:(dc + 1) * 128], hT[:, kc, t0:t0 + n], kc == 0, kc == KC - 1, [Bw, Bh[ti]], [PB[bm]])
                            if j == 0:
                                for kc in range(KC):
                                    mm(bank[br][:, 0:n], wr_[:, kc, dc * 128:(dc + 1) * 128], hT[:, kc, t0:t0 + n], kc == 0, kc == KC - 1, [Bw, Bh[ti]], [PB[br]])
                            rope_evac(dsts, bm, br, dc, t0, n, j, cnt % 2, Bdst, hd, wd, RB, csc)
                            cnt += 1
                    for vc in range(4):
                        pb = 6 + (vc % 2)
                        for kc in range(KC):
                            mm(bank[pb][:, 0:n], wg[:, kc, vc * 128:(vc + 1) * 128], hT[:, kc, t0:t0 + n], kc == 0, kc == KC - 1, [Bwg, Bh[ti]], [PB[pb]])
                        act(sg[:, vc, t0:t0 + n], bank[pb][:, 0:n], AF.Silu, [PB[pb]], [Bsg])
                    for sub in range(n // 128):
                        c = t0 // 128 + sub
                        pb = 6 + (sub % 2)
                        for kc in range(KC):
                            mm(bank[pb], hT[:, kc, c * 128:(c + 1) * 128], wv[:, kc, :], kc == 0, kc == KC - 1, [Bwv, Bh[ti]], [PB[pb]])
                        cp("act", v[:, c, :], bank[pb], [PB[pb]], [Bv])
                if hd < 3 and "prefetch" in OPT:
                    load_head_weights(hd + 1)
                for c in range(NCH):
                    s = c % 2
                    for dc in range(2):
                        tr(psT[s][:, dc * 128:(dc + 1) * 128], kT[:, dc, c * 128:(c + 1) * 128], identb, [Bk, B_small], [PB[4 + s]])
                    ts("dve", kdf[:, c, :], psT[s][:, 0:256], kdecf[:, hd:hd + 1], None, ALU.mult, None, [PB[4 + s], Bd], [Bkd])
                    ts("dve", kdb[:, c, :], psT[s][:, 0:256], kdecb[:, hd:hd + 1], None, ALU.mult, None, [PB[4 + s], Bd], [Bkd])
                BSbd = [Buf(), Buf()]
                P.op("dve", lambda e: e.memset(Sb, 0.0), [], [BSb])
                orderB = [1, 0] + list(range(NCH - 1, 1, -1))
                for idx, c in enumerate(orderB):
                    s = idx % 2
                    for dc in range(2):
                        cp("act", Sbb[s][:, dc, :], Sb[:, dc, :], [BSb, BSbd[dc]], [BSbb[s]])
                    dma("sp", sb_dram[c].rearrange("p (a b) -> p a b", a=2), Sbb[s], [BSbb[s]], [B_sbd[c]])
                    for dc in range(2):
                        pb = 0 + dc
                        mm(bank[pb], kdb[:, c, dc * 128:(dc + 1) * 128], v[:, c, :], True, True, [Bkd, Bv], [PB[pb]])
                        stt(Sb[:, dc, :], Sb[:, dc, :], cdec[:, 4 + hd:5 + hd], bank[pb], ALU.mult, ALU.add, [BSb, BSbd[dc], PB[pb], Bd], [BSbd[dc]])
                P.op("dve", lambda e: e.memset(Sf, 0.0), [], [BSf])
                P.op("pool", lambda e: e.memset(Sfb[0], 0.0), [], [BSfb[0]])
                gstate = [0]

                def stA(c, hd=hd):
                    s = c % 2
                    ck = slice(c * 128, (c + 1) * 128)
                    dma("sp", Sbin[s], sb_dram[c].rearrange("p (a b) -> p a b", a=2), [B_sbd[c]], [BSbin[s]])
                    for dc in range(2):
                        mm(bank[2][:, 0:128], kT[:, dc, ck], qT[:, dc, ck], dc == 0, dc == 1, [Bk, Bq], [PB[2]])
                    for dc in range(2):
                        mm(bank[dc], kdf[:, c, dc * 128:(dc + 1) * 128], v[:, c, :], True, True, [Bkd, Bv], [PB[dc]])
                    tt("dve", PT[s], bank[2][:, 0:128], maskT[:, hd], ALU.mult, [PB[2], Bd], [BPT[s]])
                    tt("pool", qfc[s], qT[:, :, ck], decf[:, hd].unsqueeze(1).to_broadcast([128, 2, 128]), ALU.mult, [Bq, Bd], [Bqc[s]])
                    tt("pool", qbc[s], qT[:, :, ck], decb[:, hd].unsqueeze(1).to_broadcast([128, 2, 128]), ALU.mult, [Bq, Bd], [Bqc[s]])
                    mm(bank[3], PT[s], v[:, c, :], True, False, [BPT[s], Bv], [PB[3]])
                    for dc in range(2):
                        mm(bank[3], qfc[s][:, dc, :], Sfb[s][:, dc, :], False, False, [Bqc[s], BSfb[s]], [PB[3]])
                    for dc in range(2):
                        mm(bank[3], qbc[s][:, dc, :], Sbin[s][:, dc, :], False, dc == 1, [Bqc[s], BSbin[s]], [PB[3]])
                    for dc in range(2):
                        stt(Sf[:, dc, :], Sf[:, dc, :], cdec[:, hd:hd + 1], bank[dc], ALU.mult, ALU.add, [BSf, PB[dc], Bd], [BSf])
                    cp("dve", Sfb[1 - s], Sf, [BSf], [BSfb[1 - s]])
                    act(osb[s], bank[3], AF.Identity, [PB[3]], [Bosb[s], Bstat[s]], accum_out=stat[s][:, 0:1])
                    act(junk, osb[s], AF.Square, [Bosb[s]], [Bjunk, Bstat[s]], accum_out=stat[s][:, 1:2])

                def stB(c, hd=hd):
                    s = c % 2
                    sv = stat[s]
                    ts("dve", sv[:, 2:3], sv[:, 0:1], 1.0 / 512, None, ALU.mult, None, [Bstat[s]], [Bstat[s]])
                    tt("dve", sv[:, 3:4], sv[:, 2:3], sv[:, 2:3], ALU.mult, [Bstat[s]], [Bstat[s]])
                    stt(sv[:, 4:5], sv[:, 1:2], 1.0 / 512, sv[:, 3:4], ALU.mult, ALU.subtract, [Bstat[s]], [Bstat[s]])
                    ts("dve", sv[:, 4:5], sv[:, 4:5], EPS, None, ALU.add, None, [Bstat[s]], [Bstat[s]])
                    act(sv[:, 4:5], sv[:, 4:5], AF.Sqrt, [Bstat[s]], [Bstat[s]])
                    P.op("dve", lambda e, sv=sv: e.reciprocal(out=sv[:, 5:6], in_=sv[:, 4:5]), [Bstat[s]], [Bstat[s]])
                    stt(sv[:, 6:7], sv[:, 2:3], -1.0, sv[:, 5:6], ALU.mult, ALU.mult, [Bstat[s]], [Bstat[s]])
                    act(onb[s], osb[s], AF.Identity, [Bosb[s], Bstat[s]], [Bonb[s]], scale=sv[:, 5:6], bias=sv[:, 6:7])
                    for vc in range(4):
                        if vc < 2:
                            tr(psT[s][:, vc * 128:(vc + 1) * 128], onb[s][:, vc * 128:(vc + 1) * 128], identb, [Bonb[s], B_small], [PB[4 + s]])
                        else:
                            tr(psT2[s][:, vc * 128:(vc + 1) * 128], onb[s][:, vc * 128:(vc + 1) * 128], identb, [Bonb[s], B_small], [PB[6 + s]])

                def stC(c, hd=hd):
                    s = c % 2
                    ck = slice(c * 128, (c + 1) * 128)
                    for vc in range(4):
                        if vc < 2:
                            act(tmpn[s][:, vc, :], psT[s][:, vc * 128:(vc + 1) * 128], AF.Identity, [PB[4 + s], Bd], [Btnv[s][vc]],
                                scale=gnw[:, hd * 4 + vc:hd * 4 + vc + 1], bias=gnb[:, hd * 4 + vc:hd * 4 + vc + 1])
                        else:
                            ts("dve", tmpn[s][:, vc, :], psT2[s][:, vc * 128:(vc + 1) * 128], gnw[:, hd * 4 + vc:hd * 4 + vc + 1], gnb[:, hd * 4 + vc:hd * 4 + vc + 1],
                               ALU.mult, ALU.add, [PB[6 + s], Bd], [Btnv[s][vc]])
                    if c < 2:
                        g0, gn = 0, 2
                    else:
                        g0, gn = 2 + ((c - 2) // 4) * 4, 4
                    gs = gstate[0] % 2
                    ci = c - g0
                    tt("pool", ystg[gs][:, :, ci * 128:(ci + 1) * 128], tmpn[s], sg[:, :, ck], ALU.mult, Btnv[s] + [Bsg], [Bys[gs]])
                    if ci == gn - 1:
                        dma("sp", yT_dram.rearrange("(h vc p) t -> p h vc t", p=128, vc=4)[:, hd, :, g0 * 128:(g0 + gn) * 128],
                            ystg[gs][:, :, 0:gn * 128], [Bys[gs]], [B_yd])
                        gstate[0] += 1

                for it in range(NCH + 2):
                    if it < NCH:
                        stA(it)
                    if 0 <= it - 1 < NCH:
                        stB(it - 1)
                    if 0 <= it - 2 < NCH:
                        stC(it - 2)
            P.barrier()
            A.release(m)

        def stage_outproj(layer, x_sb, Bx, w_out_ap, kco, tiles, xsrc, Bxs):
            m = A.mark()
            wo = A.alloc([kco, 1024], BF16); Bwo = Buf("wo")
            yt = [A.alloc([kco, 512], BF16) for _ in range(2)]; Byt = [Buf(), Buf()]
            dma("pool", wo, w_out_ap.rearrange("(kc p) n -> p kc n", p=128), [], [Bwo])
            cnt = 0
            for ti, (t0, n, j) in enumerate(tiles):
                s = ti % 2
                dma("sp", x_sb[:, :, t0:t0 + n], xsrc.rearrange("(c p) t -> p c t", p=128)[:, :, t0:t0 + n], [Bxs], Bx[TG[t0]])
                dma("sp", yt[s][:, :, 0:n], yT_dram.rearrange("(kc p) t -> p kc t", p=128)[:, 0:kco, t0:t0 + n], [B_yd], [Byt[s]])
                for dcn in range(8):
                    pb = cnt % 4
                    cnt += 1
                    for kc in range(kco):
                        mm(bank[pb][:, 0:n], wo[:, kc, dcn * 128:(dcn + 1) * 128], yt[s][:, kc, 0:n], kc == 0, kc == kco - 1, [Bwo, Byt[s]], [PB[pb]])
                    stt(x_sb[:, dcn, t0:t0 + n], bank[pb][:, 0:n], modt[:, layer, 16 + dcn, j:j + 1], x_sb[:, dcn, t0:t0 + n], ALU.mult, ALU.add, [PB[pb], B_mod, Bx[TG[t0]][dcn]], [Bx[TG[t0]][dcn]])
            P.barrier()
            A.release(m)

        def ffn_expert(layer, x_sb, Bx, hT, Bh, wg_ap, wu_ap, wd_ap, F, tiles, slots, Ge=None, BGe=None):
            (wgs, wus, wds, Bws, sgt, Bsgt, actT, Bact, tmp2, Btmp2, cnts, GS) = slots
            NW = len(wgs)
            NFC = GS // 128
            w_g = wg_ap.rearrange("(kc p) n -> p kc n", p=128)
            w_u = wu_ap.rearrange("(kc p) n -> p kc n", p=128)
            w_d = wd_ap.rearrange("(fc p) n -> p fc n", p=128)
            assert F % GS == 0
            for grp in range(F // GS):
                ws = cnts[0] % NW
                cnts[0] += 1
                dma("pool", wgs[ws], w_g[:, :, grp * GS:(grp + 1) * GS], [], [Bws[ws]])
                dma("pool", wus[ws], w_u[:, :, grp * GS:(grp + 1) * GS], [], [Bws[ws]])
                dma("pool", wds[ws], w_d[:, grp * NFC:(grp + 1) * NFC, :], [], [Bws[ws]])
                for ti, (t0, n, j) in enumerate(tiles):
                    a_s = cnts[1] % 2
                    cnts[1] += 1
                    for fc in range(NFC):
                        f2 = fc % 2
                        pg = 0 + f2
                        pu = 2 + f2
                        for kc in range(KC):
                            mm(bank[pg][:, 0:n], wgs[ws][:, kc, fc * 128:(fc + 1) * 128], hT[:, kc, t0:t0 + n], kc == 0, kc == KC - 1, [Bws[ws], Bh[ti]], [PB[pg]])
                        for kc in range(KC):
                            mm(bank[pu][:, 0:n], wus[ws][:, kc, fc * 128:(fc + 1) * 128], hT[:, kc, t0:t0 + n], kc == 0, kc == KC - 1, [Bws[ws], Bh[ti]], [PB[pu]])
                        act(sgt[f2][:, 0:n], bank[pg][:, 0:n], AF.Silu, [PB[pg]], [Bsgt[f2]])
                        if Ge is None:
                            tt("dve", actT[a_s][:, fc, 0:n], bank[pu][:, 0:n], sgt[f2][:, 0:n], ALU.mult, [PB[pu], Bsgt[f2]], [Bact[a_s][fc]])
                        else:
                            tt("dve", tmp2[f2][:, 0:n], bank[pu][:, 0:n], sgt[f2][:, 0:n], ALU.mult, [PB[pu], Bsgt[f2]], [Btmp2[f2]])
                            tt("dve", actT[a_s][:, fc, 0:n], tmp2[f2][:, 0:n], Ge[:, t0:t0 + n], ALU.mult, [Btmp2[f2], BGe], [Bact[a_s][fc]])
                    if cnts[3] is not None:
                        cnts[3]()

                    def down(ws=ws, a_s=a_s, t0=t0, n=n, j=j):
                        for dcn in range(8):
                            pb = 4 + (cnts[2] % 4)
                            cnts[2] += 1
                            for fc in range(NFC):
                                mm(bank[pb][:, 0:n], wds[ws][:, fc, dcn * 128:(dcn + 1) * 128], actT[a_s][:, fc, 0:n], fc == 0, fc == NFC - 1, [Bws[ws], Bact[a_s][fc]], [PB[pb]])
                            bx = Bx[TG[t0]][dcn]
                            stt(x_sb[:, dcn, t0:t0 + n], bank[pb][:, 0:n], modt[:, layer, 40 + dcn, j:j + 1], x_sb[:, dcn, t0:t0 + n], ALU.mult, ALU.add, [PB[pb], B_mod, bx], [bx])
                    cnts[3] = down

        def ffn_flush(slots):
            cnts = slots[-2]
            if cnts[3] is not None:
                cnts[3]()
                cnts[3] = None

        def alloc_ffn_slots(moe):
            GS = 512 if moe else 256
            NFC = GS // 128
            NW = 2 if moe else 3
            wgs = [A.alloc([8, GS], BF16) for _ in range(NW)]
            wus = [A.alloc([8, GS], BF16) for _ in range(NW)]
            wds = [A.alloc([NFC, 1024], BF16) for _ in range(NW)]
            Bws = [Buf() for _ in range(NW)]
            sgt = [A.alloc([512], BF16) for _ in range(2)]; Bsgt = [Buf(), Buf()]
            actT = [A.alloc([NFC, 512], BF16) for _ in range(2)]; Bact = [[Buf() for _ in range(NFC)] for _ in range(2)]
            tmp2 = [A.alloc([512], F32) for _ in range(2)] if moe else None
            Btmp2 = [Buf(), Buf()]
            return (wgs, wus, wds, Bws, sgt, Bsgt, actT, Bact, tmp2, Btmp2, [0, 0, 0, None], GS)

        def stage_gla(layer, hT, Bh, last):
            jl = layer // 2
            m = A.mark()
            w_in = gla_w_in[jl].rearrange("(kc p) n -> p kc n", p=128)
            bg = A.alloc([2, 4], F32); nbg = A.alloc([2, 4], F32); ng = A.alloc([8], F32)
            Bc = Buf("glac")
            dma("sp", bg, gla_bg[:, jl], [], [Bc])
            dma("sp", ng, gla_ng[:, jl, :], [], [Bc])
            ts("dve", nbg, bg, -1.0, None, ALU.mult, None, [Bc], [Bc])
            wa = A.alloc([8, 32], BF16); Bwa = Buf("wa")
            wup = A.alloc([2, 512], BF16)
            aT = A.alloc([2, T], BF16); BaT = Buf("aT")
            dma("pool", wa, w_in[:, :, 3072:3104], [], [Bwa])
            dma("pool", wup[0:16], gla_wgu[jl].rearrange("d r f -> r d f"), [], [Bwa])
            cnt = 0
            for ti, (t0, n, j) in enumerate(TILES):
                for dr in range(2):
                    pb = cnt % 2
                    cnt += 1
                    for kc in range(KC):
                        mm(bank[pb][0:16, 0:n], wa[:, kc, dr * 16:(dr + 1) * 16], hT[:, kc, t0:t0 + n], kc == 0, kc == KC - 1, [Bwa, Bh[ti]], [PB[pb]])
                    cp("act", aT[0:16, dr, t0:t0 + n], bank[pb][0:16, 0:n], [PB[pb]], [BaT])
            wq = A.alloc([8, 128], BF16); wk = A.alloc([8, 128], BF16)
            wv = A.alloc([8, 256], BF16); wr_ = A.alloc([8, 256], BF16)
            Bwq, Bwk, Bwv, Bwr = Buf(), Buf(), Buf(), Buf()
            qs = A.alloc([T], F32); ks = A.alloc([T], F32); Bqs, Bks = Buf(), Buf()
            la = A.alloc([T], F32); Bla = Buf()
            Pp = A.alloc([T + 1], F32); BPp = Buf()
            fq = A.alloc([T], F32); fk = A.alloc([T], F32); fs = A.alloc([T], F32); Bfq, Bfk, Bfs = Buf(), Buf(), Buf()
            ex = [A.alloc([512], F32) for _ in range(2)]; Bex = [Buf(), Buf()]
            qt = [A.alloc([T], BF16) for _ in range(2)]; kt = [A.alloc([T], BF16) for _ in range(2)]
            kp = A.alloc([T], BF16)
            Bqt, Bkt, Bkp = [Buf(), Buf()], [Buf(), Buf()], Buf()
            kptm = [A.alloc([NCH, 128], BF16) for _ in range(2)]; Bkptm = [Buf(), Buf()]
            ed = [A.alloc([NCH], F32) for _ in range(2)]; Bed = [Buf(), Buf()]
            v = A.alloc([NCH, 256], BF16); Bv = Buf()
            sr = A.alloc([2, T], BF16); Bsr = Buf()
            Sf = A.alloc([256], F32); Sb = A.alloc([256], F32); BSf, BSb = Buf(), Buf()
            Sfb = [A.alloc([256], BF16) for _ in range(2)]; BSfb = [Buf(), Buf()]
            Sbb = [A.alloc([256], BF16) for _ in range(2)]; BSbb = [Buf(), Buf()]
            Sbin = [A.alloc([256], BF16) for _ in range(2)]; BSbin = [Buf(), Buf()]
            PTf = [A.alloc([128], BF16) for _ in range(2)]; PTb = [A.alloc([128], BF16) for _ in range(2)]; BPT = [Buf(), Buf()]
            junk = A.alloc([256], F32); Bjunk = Buf()
            stat = [A.alloc([4], F32) for _ in range(2)]; Bstat = [Buf(), Buf()]
            onb = [A.alloc([256], BF16) for _ in range(2)]; Bonb = [Buf(), Buf()]
            tmpn = [A.alloc([2, 128], F32) for _ in range(2)]; Btn = [Buf(), Buf()]
            ystg = [A.alloc([2, 512], BF16) for _ in range(2)]; Bys = [Buf(), Buf()]
            psT = [psum[:, 4 + i, :].bitcast(BF16) for i in range(2)]
            P.op("dve", lambda e: e.memset(Pp[:, 0:1], 0.0), [], [BPp])
            QB = float(np.log(128.0 ** -0.5))
            pv = lambda ap: ap.rearrange("p (c i) -> p c i", i=128)
            gstate = [0]
            def gla_load(hd):
                dma("pool", wq, w_in[:, :, hd * 128:(hd + 1) * 128], [], [Bwq])
                dma("pool", wk, w_in[:, :, 1536 + hd * 128:1536 + (hd + 1) * 128], [], [Bwk])
                dma("pool", wv, w_in[:, :, 2048 + hd * 256:2048 + (hd + 1) * 256], [], [Bwv])
                dma("pool", wr_, w_in[:, :, 512 + hd * 256:512 + (hd + 1) * 256], [], [Bwr])

            def gla_gates(dr, hd):
                for ti, (t0, n, j) in enumerate(TILES):
                    s = ti % 2
                    pb = 2 + s
                    mm(bank[pb][:, 0:n], wup[0:16, dr, hd * 128:(hd + 1) * 128], aT[0:16, dr, t0:t0 + n], True, True, [Bwa, BaT], [PB[pb]])
                    act(ex[s][:, 0:n], bank[pb][:, 0:n], AF.Exp, [PB[pb], Bc], [Bex[s]], scale=-1.0, bias=nbg[:, dr, hd:hd + 1])
                    act(la[:, t0:t0 + n], ex[s][:, 0:n], AF.Ln, [Bex[s]], [Bla], bias=1.0)
                P.op("dve", lambda e: e.tensor_tensor_scan(out=Pp[:, 1:T + 1], data0=la, data1=la, initial=0.0, op0=ALU.add, op1=ALU.add), [Bla], [BPp])
                c0v = pv(Pp[:, 0:T])[:, :, 0:1].to_broadcast([128, NCH, 128])
                cev = pv(Pp[:, 1:T + 1])[:, :, 127:128].to_broadcast([128, NCH, 128])
                if dr == 0:
                    tt("dve", pv(fq), pv(Pp[:, 1:T + 1]), c0v, ALU.subtract, [BPp], [Bfq])
                    tt("dve", pv(fs), cev, pv(Pp[:, 1:T + 1]), ALU.subtract, [BPp], [Bfs])
                else:
                    tt("dve", pv(fq), cev, pv(Pp[:, 0:T]), ALU.subtract, [BPp], [Bfq])
                    tt("dve", pv(fs), pv(Pp[:, 0:T]), c0v, ALU.subtract, [BPp], [Bfs])
                tt("dve", ed[dr], pv(Pp[:, 1:T + 1])[:, :, 127], pv(Pp[:, 0:T])[:, :, 0], ALU.subtract, [BPp], [Bed[dr]])
                act(ed[dr], ed[dr], AF.Exp, [Bed[dr]], [Bed[dr]], scale=-1.0 / 32)
                act(fk, fq, AF.Exp, [Bfq], [Bfk], scale=1.0 / 32)
                act(fq, fq, AF.Exp, [Bfq], [Bfq], scale=-1.0 / 32, bias=QB)
                act(fs, fs, AF.Exp, [Bfs], [Bfs], scale=-1.0 / 32)

            def gla_products(dr):
                tt("pool", qt[dr], qs, fq, ALU.mult, [Bqs, Bfq], [Bqt[dr]])
                tt("dve", kt[dr], ks, fk, ALU.mult, [Bks, Bfk], [Bkt[dr]])
                tt("pool", kp, ks, fs, ALU.mult, [Bks, Bfs], [Bkp])
                for c in range(NCH):
                    s = c % 2
                    tr(psT[s][:, 0:128], kp[:, c * 128:(c + 1) * 128], identb, [Bkp, B_small], [PB[4 + s]])
                    cp("act", kptm[dr][:, c, :], psT[s][:, 0:128], [PB[4 + s]], [Bkptm[dr]])

            gla_load(0)
            for hd in range(4):
                gla_gates(0, hd)
                for ti, (t0, n, j) in enumerate(TILES):
                    for (w, Bw, dst, Bdst, pb) in ((wq, Bwq, qs, Bqs, 0), (wk, Bwk, ks, Bks, 1)):
                        for kc in range(KC):
                            mm(bank[pb][:, 0:n], w[:, kc, :], hT[:, kc, t0:t0 + n], kc == 0, kc == KC - 1, [Bw, Bh[ti]], [PB[pb]])
                        cp("act", dst[:, t0:t0 + n], bank[pb][:, 0:n], [PB[pb]], [Bdst])
                    for vc in range(2):
                        pb = 2 + vc
                        for kc in range(KC):
                            mm(bank[pb][:, 0:n], wr_[:, kc, vc * 128:(vc + 1) * 128], hT[:, kc, t0:t0 + n], kc == 0, kc == KC - 1, [Bwr, Bh[ti]], [PB[pb]])
                        act(sr[:, vc, t0:t0 + n], bank[pb][:, 0:n], AF.Silu, [PB[pb]], [Bsr])
                    for sub in range(n // 128):
                        c = t0 // 128 + sub
                        pb = 6 + (sub % 2)
                        for kc in range(KC):
                            mm(bank[pb][:, 0:256], hT[:, kc, c * 128:(c + 1) * 128], wv[:, kc, :], kc == 0, kc == KC - 1, [Bwv, Bh[ti]], [PB[pb]])
                        cp("dve", v[:, c, :], bank[pb][:, 0:256], [PB[pb]], [Bv])
                if hd < 3:
                    gla_load(hd + 1)
                gla_products(0)
                gla_gates(1, hd)
                gla_products(1)
                P.op("dve", lambda e: e.memset(Sb, 0.0), [], [BSb])
                orderB = [1, 0] + list(range(NCH - 1, 1, -1))
                for idx, c in enumerate(orderB):
                    s = idx % 2
                    cp("act", Sbb[s], Sb, [BSb], [BSbb[s]])
                    dma("sp", sb_dram[c][:, 0:256], Sbb[s], [BSbb[s]], [B_sbd[c]])
                    mm(bank[0][:, 0:256], kptm[1][:, c, :], v[:, c, :], True, True, [Bkptm[1], Bv], [PB[0]])
                    stt(Sb, Sb, ed[1][:, c:c + 1], bank[0][:, 0:256], ALU.mult, ALU.add, [BSb, PB[0], Bed[1]], [BSb])
                P.op("dve", lambda e: e.memset(Sf, 0.0), [], [BSf])
                P.op("pool", lambda e: e.memset(Sfb[0], 0.0), [], [BSfb[0]])
                OB = (3, 1)

                def gA(c, hd=hd):
                    s = c % 2
                    ck = slice(c * 128, (c + 1) * 128)
                    ob = OB[s]
                    need_out = not (last and c < 2)
                    mm(bank[0][:, 0:256], kptm[0][:, c, :], v[:, c, :], True, True, [Bkptm[0], Bv], [PB[0]])
                    if need_out:
                        dma("sp", Sbin[s], sb_dram[c][:, 0:256], [B_sbd[c]], [BSbin[s]])
                        mm(bank[2][:, 0:128], kt[0][:, ck], qt[0][:, ck], True, True, [Bkt[0], Bqt[0]], [PB[2]])
                        mm(bank[2][:, 128:256], kt[1][:, ck], qt[1][:, ck], True, True, [Bkt[1], Bqt[1]], [PB[2]])
                        tt("dve", PTf[s], bank[2][:, 0:128], cst[:, C_LO:C_LO + 128], ALU.mult, [PB[2], B_cst], [BPT[s]])
                        tt("dve", PTb[s], bank[2][:, 128:256], cst[:, C_UP:C_UP + 128], ALU.mult, [PB[2], B_cst], [BPT[s]])
                        mm(bank[ob][:, 0:256], PTf[s], v[:, c, :], True, False, [BPT[s], Bv], [PB[ob]])
                        mm(bank[ob][:, 0:256], PTb[s], v[:, c, :], False, False, [BPT[s], Bv], [PB[ob]])
                        mm(bank[ob][:, 0:256], qt[0][:, ck], Sfb[s], False, False, [Bqt[0], BSfb[s]], [PB[ob]])
                        mm(bank[ob][:, 0:256], qt[1][:, ck], Sbin[s], False, True, [Bqt[1], BSbin[s]], [PB[ob]])
                    stt(Sf, Sf, ed[0][:, c:c + 1], bank[0][:, 0:256], ALU.mult, ALU.add, [BSf, PB[0], Bed[0]], [BSf])
                    cp("dve", Sfb[1 - s], Sf, [BSf], [BSfb[1 - s]])
                    if need_out:
                        act(junk, bank[ob][:, 0:256], AF.Square, [PB[ob]], [Bjunk, Bstat[s]], accum_out=stat[s][:, 0:1])

                def gB(c, hd=hd):
                    s = c % 2
                    ob = OB[s]
                    if last and c < 2:
                        return
                    sv = stat[s]
                    ts("dve", sv[:, 1:2], sv[:, 0:1], 1.0 / 256, EPS, ALU.mult, ALU.add, [Bstat[s]], [Bstat[s]])
                    act(sv[:, 1:2], sv[:, 1:2], AF.Sqrt, [Bstat[s]], [Bstat[s]])
                    P.op("dve", lambda e, sv=sv: e.reciprocal(out=sv[:, 2:3], in_=sv[:, 1:2]), [Bstat[s]], [Bstat[s]])
                    act(onb[s], bank[ob][:, 0:256], AF.Copy, [PB[ob], Bstat[s]], [Bonb[s]], scale=sv[:, 2:3])
                    for vc in range(2):
                        tr(psT[s][:, vc * 128:(vc + 1) * 128], onb[s][:, vc * 128:(vc + 1) * 128], identb, [Bonb[s], B_small], [PB[4 + s]])

                def gC(c, hd=hd):
                    s = c % 2
                    ck = slice(c * 128, (c + 1) * 128)
                    if last and c < 2:
                        return
                    for vc in range(2):
                        ts("dve", tmpn[s][:, vc, :], psT[s][:, vc * 128:(vc + 1) * 128], ng[:, hd * 2 + vc:hd * 2 + vc + 1], None, ALU.mult, None, [PB[4 + s], Bc], [Btn[s]])
                    if c < 2:
                        g0, gn = 0, 2
                    else:
                        g0, gn = 2 + ((c - 2) // 4) * 4, 4
                    gs = gstate[0] % 2
                    ci = c - g0
                    tt("pool", ystg[gs][:, :, ci * 128:(ci + 1) * 128], tmpn[s], sr[:, :, ck], ALU.mult, [Btn[s], Bsr], [Bys[gs]])
                    if ci == gn - 1:
                        dma("sp", yT_dram[0:1024].rearrange("(h vc p) t -> p h vc t", p=128, vc=2)[:, hd, :, g0 * 128:(g0 + gn) * 128],
                            ystg[gs][:, :, 0:gn * 128], [Bys[gs]], [B_yd])
                        gstate[0] += 1

                for it in range(NCH + 2):
                    if it < NCH:
                        gA(it)
                    if 0 <= it - 1 < NCH:
                        gB(it - 1)
                    if 0 <= it - 2 < NCH:
                        gC(it - 2)
            P.barrier()
            A.release(m)

        def stage_moe(layer, x_sb, Bx, hT, Bh, tiles):
            jl = layer // 2
            m = A.mark()
            wr = A.alloc([8, 8], F32); Bwr = Buf("wr")
            dma("sp", wr, moe_wr[jl].rearrange("(kc p) e -> p kc e", p=128), [], [Bwr])

            def hf_cb(ti, t0, n, hf, Bhf):
                for sub in range(n // 128):
                    c = t0 // 128 + sub
                    for kc in range(KC):
                        mm(bank[5][:, c * 8:(c + 1) * 8], hf[:, kc, sub * 128:(sub + 1) * 128], wr[:, kc, :], kc == 0, kc == KC - 1, [Bhf, Bwr], [PB[5]])

            stage_norm(layer, gm2, 24, hT, Bh, x_sb=x_sb, Bx=Bx, tiles=tiles, hf_cb=hf_cb)
            c_lo = tiles[0][0] // 128
            ncu = NCH - c_lo
            L = A.alloc([NCH, 8], F32); L2 = A.alloc([NCH, 8], F32); eq1 = A.alloc([NCH, 8], F32); eq2 = A.alloc([NCH, 8], F32)
            gates = A.alloc([NCH, 8], F32)
            m1 = A.alloc([NCH], F32); m2 = A.alloc([NCH], F32); w1 = A.alloc([NCH], F32); w2 = A.alloc([NCH], F32)
            Bg = Buf("gate")
            sl = slice(c_lo, NCH)
            cp("dve", L[:, sl], bank[5][:, c_lo * 8:NCH * 8].rearrange("p (c e) -> p c e", e=8), [PB[5]], [Bg])
            P.op("dve", lambda e: e.tensor_reduce(out=m1[:, sl], in_=L[:, sl], axis=AX.X, op=ALU.max), [Bg], [Bg])
            tt("dve", eq1[:, sl], L[:, sl], m1[:, sl].unsqueeze(2).to_broadcast([128, ncu, 8]), ALU.is_equal, [Bg], [Bg])
            stt(L2[:, sl], eq1[:, sl], -1e30, L[:, sl], ALU.mult, ALU.add, [Bg], [Bg])
            P.op("dve", lambda e: e.tensor_reduce(out=m2[:, sl], in_=L2[:, sl], axis=AX.X, op=ALU.max), [Bg], [Bg])
            tt("dve", eq2[:, sl], L2[:, sl], m2[:, sl].unsqueeze(2).to_broadcast([128, ncu, 8]), ALU.is_equal, [Bg], [Bg])
            tt("dve", w2[:, sl], m2[:, sl], m1[:, sl], ALU.subtract, [Bg], [Bg])
            act(w2[:, sl], w2[:, sl], AF.Exp, [Bg], [Bg])
            ts("dve", w1[:, sl], w2[:, sl], 1.0, None, ALU.add, None, [Bg], [Bg])
            P.op("dve", lambda e: e.reciprocal(out=w1[:, sl], in_=w1[:, sl]), [Bg], [Bg])
            tt("dve", w2[:, sl], w2[:, sl], w1[:, sl], ALU.mult, [Bg], [Bg])
            tt("dve", gates[:, sl], eq1[:, sl], w1[:, sl].unsqueeze(2).to_broadcast([128, ncu, 8]), ALU.mult, [Bg], [Bg])
            tt("dve", eq2[:, sl], eq2[:, sl], w2[:, sl].unsqueeze(2).to_broadcast([128, ncu, 8]), ALU.mult, [Bg], [Bg])
            tt("dve", gates[:, sl], gates[:, sl], eq2[:, sl], ALU.add, [Bg], [Bg])
            Ge = [A.alloc([T], F32) for _ in range(2)]; BGe = [Buf(), Buf()]
            diag = [A.alloc([128], F32) for _ in range(2)]; Bdg = [Buf(), Buf()]
            slots = alloc_ffn_slots(True)
            dcnt = 0
            for e_ in range(NE):
                gs = e_ % 2
                for (t0, n, j) in tiles:
                    for sub in range(n // 128):
                        c = t0 // 128 + sub
                        ds_ = dcnt % 2
                        dcnt += 1
                        ts("dve", diag[ds_], cst[:, C_ID:C_ID + 128], gates[:, c, e_:e_ + 1], None, ALU.mult, None, [Bg, B_cst], [Bdg[ds_]])
                        mm(bank[4][:, sub * 128:(sub + 1) * 128], onesf, diag[ds_], True, True, [Bdg[ds_], B_small], [PB[4]])
                    cp("act", Ge[gs][:, t0:t0 + n], bank[4][:, 0:n], [PB[4]], [BGe[gs]])
                ffn_expert(layer, x_sb, Bx, hT, Bh, moe_wg[jl, e_], moe_wu[jl, e_], moe_wd[jl, e_], D_FFE, tiles, slots, Ge=Ge[gs], BGe=BGe[gs])
            ffn_flush(slots)
            P.barrier()
            A.release(m)

        def stage_final(x_sb, Bx):
            m = A.mark()
            sq = [A.alloc([8, 512], BF16) for _ in range(2)]; Bsq = [Buf(), Buf()]
            rs = [A.alloc([512], F32) for _ in range(2)]; Brs = [Buf(), Buf()]
            tmp = [A.alloc([8, 512], F32) for _ in range(2)]; Btmp = [Buf(), Buf()]
            for ti, (t0, n, j) in enumerate(TILES[1:]):
                s = ti % 2
                xt = x_sb[:, :, t0:t0 + n]
                act(sq[s], xt, AF.Square, Bx[TG[t0]], [Bsq[s]])
                pb = 6 + s
                for c in range(KC):
                    mm(bank[pb], onesb, sq[s][:, c, :], c == 0, c == KC - 1, [Bsq[s], B_small], [PB[pb]])
                act(rs[s], bank[pb], AF.Ln, [PB[pb]], [Brs[s]], scale=1.0 / D, bias=epsc[:, 0:1])
                act(rs[s], rs[s], AF.Exp, [Brs[s]], [Brs[s]], scale=-0.5)
                tt("dve", tmp[s], xt, rs[s].unsqueeze(1).to_broadcast([128, 8, 512]), ALU.mult, Bx[TG[t0]] + [Brs[s]], [Btmp[s]])
                tt("pool", tmp[s], tmp[s], fgt.unsqueeze(2).to_broadcast([128, 8, 512]), ALU.mult, [Btmp[s], B_small], [Btmp[s]])
                dma("sp", outT.rearrange("(c p) t -> p c t", p=128)[:, :, t0 - NCTX:t0 - NCTX + n], tmp[s], [Btmp[s]], [B_out])
            P.barrier()
            A.release(m)

        stage_adaln()
        for layer in range(n_layers):
            last = layer == 3
            jl = layer // 2
            is_ret = layer % 2 == 0
            xsrc = xT if layer == 0 else x_dram
            Bxs = Buf("xT") if layer == 0 else B_xd
            m0 = A.mark()
            hT = A.alloc([8, T], BF16)
            Bh = [Buf("h%d" % i) for i in range(5)]
            stage_norm(layer, gm1, 0, hT, Bh, xsrc=xsrc, Bxs=Bxs)
            if is_ret:
                stage_retention(layer, hT, Bh)
                kco, w_out_ap = 16, ret_w_out[jl]
            else:
                stage_gla(layer, hT, Bh, last)
                kco, w_out_ap = 8, gla_w_out[jl]
            A.release(m0)
            tiles = TILES[1:] if last else TILES
            x_sb = A.alloc([8, T], F32); Bx = [[Buf("x%d_%d" % (a, b_)) for b_ in range(8)] for a in range(5)]
            stage_outproj(layer, x_sb, Bx, w_out_ap, kco, tiles, xsrc, Bxs)
            hT = A.alloc([8, T], BF16)
            Bh = [Buf("h%d" % i) for i in range(5)]
            if is_ret:
                stage_norm(layer, gm2, 24, hT, Bh[5 - len(tiles):], x_sb=x_sb, Bx=Bx, tiles=tiles)
                m1 = A.mark()
                slots = alloc_ffn_slots(False)
                ffn_expert(layer, x_sb, Bx, hT[:, :, :] if not last else hT, Bh[5 - len(tiles):], ffn_wg[jl], ffn_wu[jl], ffn_wd[jl], D_FF, tiles, slots)
                ffn_flush(slots)
                P.barrier()
                A.release(m1)
            else:
                stage_moe(layer, x_sb, Bx, hT, Bh[5 - len(tiles):], tiles)
            if layer == n_layers - 1:
                if layer == 3:
                    stage_final(x_sb, Bx)
                else:
                    for (t0, n, j) in TILES[1:]:
                        dma("sp", outT.rearrange("(c p) t -> p c t", p=128)[:, :, t0 - NCTX:t0 - NCTX + n], x_sb[:, :, t0:t0 + n], Bx[TG[t0]], [B_out])
                    if "ctx" in tap_out:
                        dma("sp", tap_out["ctx"].rearrange("(c p) t -> p c t", p=128), x_sb[:, :, 0:NCTX], Bx[0], [B_out])
            else:
                for (t0, n, j) in TILES:
                    dma("sp", x_dram.rearrange("(c p) t -> p c t", p=128)[:, :, t0:t0 + n], x_sb[:, :, t0:t0 + n], Bx[TG[t0]], [B_xd])
            P.barrier()
            A.release(m0)
        P.barrier()
        P.emit()
    return nc


def _fm(vec, nch):
    return np.ascontiguousarray(np.asarray(vec, np.float32).reshape(nch, 128).T)


def prep_shared(inp):
    sh = {}
    sh["cst"] = host_consts()
    sh["ada_w"] = np.ascontiguousarray(inp["ada_w"], dtype=np.float32)
    sh["ada_b_fm"] = np.ascontiguousarray(np.stack([_fm(inp["ada_b"][i], 48) for i in range(4)], axis=1))
    sh["ngm_fm"] = np.ascontiguousarray(np.stack([_fm(inp["norm_mix_g"][i], 8) for i in range(4)], axis=1))
    sh["ngf_fm"] = np.ascontiguousarray(np.stack([_fm(inp["norm_ffn_g"][i], 8) for i in range(4)], axis=1))
    sh["fg_fm"] = _fm(inp["final_g"], 8)
    sh["ret_w_in"] = np.ascontiguousarray(inp["ret_w_in"], dtype=np.float32)
    sh["ret_ld"] = np.ascontiguousarray(np.asarray(inp["ret_log_decay"], np.float32).reshape(2, 8))
    sh["ret_gnw_fm"] = np.ascontiguousarray(np.stack([_fm(inp["ret_gn_w"][i], 16) for i in range(2)], axis=1))
    sh["ret_gnb_fm"] = np.ascontiguousarray(np.stack([_fm(inp["ret_gn_b"][i], 16) for i in range(2)], axis=1))
    sh["ret_w_out"] = np.ascontiguousarray(inp["ret_w_out"], dtype=np.float32)
    sh["gla_w_in"] = np.ascontiguousarray(inp["gla_w_in"], dtype=np.float32)
    sh["gla_w_gate_up"] = np.ascontiguousarray(inp["gla_w_gate_up"], dtype=np.float32)
    bg = np.asarray(inp["gla_b_gate"], np.float32)
    sh["gla_bg_fm"] = np.ascontiguousarray(bg.reshape(2, 2, 4, 128).transpose(3, 0, 1, 2))
    sh["gla_ng_fm"] = np.ascontiguousarray(np.stack([_fm(inp["gla_norm_g"][i], 8) for i in range(2)], axis=1))
    sh["gla_w_out"] = np.ascontiguousarray(inp["gla_w_out"], dtype=np.float32)
    for k in ("ffn_w_gate", "ffn_w_up", "ffn_w_down", "moe_w_router", "moe_w_gate", "moe_w_up", "moe_w_down"):
        sh[k] = np.ascontiguousarray(inp[k], dtype=np.float32)
    return sh


def prep_core(inp, b, shared):
    m = dict(shared)
    m["xT"] = np.ascontiguousarray(np.concatenate([inp["ctx"][b], inp["x"][b]], axis=0).T.astype(np.float32))
    m["cc"] = np.ascontiguousarray(np.stack([_fm(inp["c"][b], 8), _fm(inp["c_ctx"], 8)], axis=1))
    return m


_NC_CACHE = {}


def kernel(**inputs):
    inp = {k: np.asarray(v) for k, v in inputs.items()}
    if "full" not in _NC_CACHE:
        _NC_CACHE["full"] = build(4)
    nc = _NC_CACHE["full"]
    shared = prep_shared(inp)
    in_maps = [prep_core(inp, b, shared) for b in range(8)]
    res = run_bass_kernel_spmd(nc, in_maps, core_ids=list(range(8)))
    out = np.stack([np.ascontiguousarray(res.results[b]["outT"].T) for b in range(8)], axis=0)
    return out.astype(np.float32)
```
